# Optimizing a Trainium2 kernel written in Bass

```python
import math
import jax, jax.numpy as jnp
from jax import lax
import numpy as np


D_MODEL = 1024
BATCH = 16
SEQ = 2048
DEPTH = 2

HEAD_DIM = 64
FOX_HEADS = 4
FOX_W = FOX_HEADS * HEAD_DIM
CONV_W = 256
CONV_K = 3
MLSTM_HEADS = 4
MLSTM_W = MLSTM_HEADS * HEAD_DIM
MLSTM_CHUNK = 64
SWA_Q_HEADS = 8
SWA_KV_HEADS = 2
SWA_W = SWA_Q_HEADS * HEAD_DIM
SWA_KV_W = SWA_KV_HEADS * HEAD_DIM
WINDOW = 128
Q_BLOCK = 128
REL_BUCKETS = 32
REL_MAX_DIST = 128
N_BRANCH = 4
D_FF = -(-8 * D_MODEL // (3 * 256)) * 256
EPS = 1e-6

SPLIT_SIZES = (FOX_W, FOX_W, FOX_W, FOX_HEADS,
               CONV_W, CONV_W, CONV_W,
               MLSTM_W, MLSTM_W, MLSTM_W, MLSTM_HEADS, MLSTM_HEADS, MLSTM_W,
               SWA_W, SWA_KV_W, SWA_KV_W,
               N_BRANCH * D_MODEL)
IN_COLS = sum(SPLIT_SIZES)

kernel_name = 'hybrid_fox_conv_mlstm_swa_block'


def rms_norm(x, gain):
    xf = x.astype(jnp.float32)
    y = xf * lax.rsqrt(jnp.mean(xf * xf, axis=-1, keepdims=True) + EPS)
    return (y * gain.astype(jnp.float32)).astype(x.dtype)


def t5_bucket(n):
    max_exact = REL_BUCKETS // 2
    nf = jnp.maximum(n, 1).astype(jnp.float32)
    large = max_exact + (jnp.log(nf / max_exact) / math.log(REL_MAX_DIST / max_exact)
                         * (REL_BUCKETS - max_exact)).astype(jnp.int32)
    large = jnp.minimum(large, REL_BUCKETS - 1)
    return jnp.where(n < max_exact, n, large)


def fox_attention(q, k, v, f_logit):
    B, S, H, d = q.shape
    nblk = S // Q_BLOCK
    c = jnp.cumsum(jax.nn.log_sigmoid(f_logit.astype(jnp.float32)), axis=1)
    c_t = c.transpose(0, 2, 1)
    qb = q.reshape(B, nblk, Q_BLOCK, H, d).transpose(1, 0, 2, 3, 4)
    cb = c.reshape(B, nblk, Q_BLOCK, H).transpose(1, 0, 3, 2)
    key_pos = jnp.arange(S)
    scale = d ** -0.5

    def block(args):
        qi, ci, bi = args
        s = jnp.einsum('bqhd,bkhd->bhqk', qi, k).astype(jnp.float32) * scale
        s = s + (ci[..., :, None] - c_t[..., None, :])
        q_pos = bi * Q_BLOCK + jnp.arange(Q_BLOCK)
        s = jnp.where(key_pos[None, :] <= q_pos[:, None], s, -jnp.inf)
        p = jax.nn.softmax(s, axis=-1).astype(v.dtype)
        return jnp.einsum('bhqk,bkhd->bqhd', p, v)

    out = lax.map(block, (qb, cb, jnp.arange(nblk)))
    return out.transpose(1, 0, 2, 3, 4).reshape(B, S, H * d)


def short_conv_mixer(u, b_gate, c_gate, w):
    z = c_gate * u
    y = lax.conv_general_dilated(z, w[:, None, :], window_strides=(1,),
                                 padding=[(CONV_K - 1, 0)],
                                 dimension_numbers=('NWC', 'WIO', 'NWC'),
                                 feature_group_count=z.shape[-1])
    return b_gate * y


def mlstm_chunkwise(q, k, v, i_pre, f_pre):
    out_dtype = q.dtype
    f32 = jnp.float32
    B, S, H, d = q.shape
    L = MLSTM_CHUNK
    nc = S // L
    q = q.astype(f32).reshape(B, nc, L, H, d) * d ** -0.5
    k = k.astype(f32).reshape(B, nc, L, H, d)
    v = v.astype(f32).reshape(B, nc, L, H, d)
    log_i = i_pre.astype(f32).reshape(B, nc, L, H)
    log_f = jax.nn.log_sigmoid(f_pre.astype(f32)).reshape(B, nc, L, H)
    b = jnp.cumsum(log_f, axis=2)
    b_last = b[:, :, -1]

    a = b_last[:, :, None] - b + log_i
    m_loc = a.max(axis=2)
    w_state = jnp.exp(a - m_loc[:, :, None])
    c_loc = jnp.einsum('bclh,bclhd,bclhe->bchde', w_state, k, v)
    n_loc = jnp.einsum('bclh,bclhd->bchd', w_state, k)

    def step(carry, xs):
        c_st, n_st, m_st = carry
        bl, ml, cl, nl = xs
        m_new = jnp.maximum(bl + m_st, ml)
        decay = jnp.exp(bl + m_st - m_new)
        wl = jnp.exp(ml - m_new)
        c_new = decay[..., None, None] * c_st + wl[..., None, None] * cl
        n_new = decay[..., None] * n_st + wl[..., None] * nl
        return (c_new, n_new, m_new), (c_st, n_st, m_st)

    init = (jnp.zeros((B, H, d, d), f32), jnp.zeros((B, H, d), f32), jnp.zeros((B, H), f32))
    xs = (jnp.moveaxis(b_last, 1, 0), jnp.moveaxis(m_loc, 1, 0),
          jnp.moveaxis(c_loc, 1, 0), jnp.moveaxis(n_loc, 1, 0))
    _, (c_prev, n_prev, m_prev) = lax.scan(step, init, xs)
    c_prev = jnp.moveaxis(c_prev, 0, 1)
    n_prev = jnp.moveaxis(n_prev, 0, 1)
    m_prev = jnp.moveaxis(m_prev, 0, 1)

    g = b + m_prev[:, :, None, :]
    dmat = b[:, :, :, None, :] - b[:, :, None, :, :] + log_i[:, :, None, :, :]
    causal = jnp.tril(jnp.ones((L, L), dtype=bool))
    dmat = jnp.where(causal[None, None, :, :, None], dmat, -jnp.inf)
    m_t = jnp.maximum(g, dmat.max(axis=3))
    s_w = jnp.einsum('bcthd,bcshd->bctsh', q, k) * jnp.exp(dmat - m_t[:, :, :, None, :])
    inter = jnp.exp(g - m_t)
    num = (jnp.einsum('bctsh,bcshe->bcthe', s_w, v)
           + inter[..., None] * jnp.einsum('bcthd,bchde->bcthe', q, c_prev))
    den = s_w.sum(axis=3) + inter * jnp.einsum('bcthd,bchd->bcth', q, n_prev)
    h = num / jnp.maximum(jnp.abs(den), jnp.exp(-m_t))[..., None]
    return h.reshape(B, S, H, d).astype(out_dtype)


def swa_attention(q, k, v, sinks, bias):
    B, S, Hq, d = q.shape
    Hkv = k.shape[2]
    G = Hq // Hkv
    W = WINDOW
    nb = S // W
    qb = q.reshape(B, nb, W, Hkv, G, d)

    def band(t):
        tp = jnp.pad(t, ((0, 0), (W, 0), (0, 0), (0, 0))).reshape(B, nb + 1, W, Hkv, d)
        return jnp.concatenate([tp[:, :-1], tp[:, 1:]], axis=2)

    kb, vb = band(k), band(v)
    s = jnp.einsum('bnqhgd,bnjhd->bnhgqj', qb, kb).astype(jnp.float32) * d ** -0.5
    s = s + bias.astype(jnp.float32).reshape(Hkv, G, W, 2 * W)
    qi = jnp.arange(W)[:, None]
    kj = jnp.arange(2 * W)[None, :]
    dist = qi + W - kj
    in_window = (dist >= 0) & (dist < WINDOW)
    key_pos = jnp.arange(nb)[:, None, None] * W - W + kj[None]
    valid = in_window[None] & (key_pos >= 0)
    s = jnp.where(valid[None, :, None, None], s, -jnp.inf)
    sink = jnp.broadcast_to(sinks.astype(jnp.float32).reshape(1, 1, Hkv, G, 1, 1),
                            s.shape[:-1] + (1,))
    p = jax.nn.softmax(jnp.concatenate([s, sink], axis=-1), axis=-1)[..., :-1]
    out = jnp.einsum('bnhgqj,bnjhd->bnqhgd', p.astype(v.dtype), vb)
    return out.reshape(B, S, Hq * d)


def setup_inputs(seed: int = 0) -> dict:
    key = jax.random.key(seed)
    ks = jax.random.split(key, 24)

    def nrm(k, shape, scale):
        return jax.random.normal(k, shape, jnp.float32) * scale

    return {
        'x': nrm(ks[0], (BATCH, SEQ, D_MODEL), 1.0),
        'rel_bias': nrm(ks[1], (REL_BUCKETS, SWA_Q_HEADS), 0.5),
        'attn_norm': 1.0 + nrm(ks[2], (DEPTH, D_MODEL), 0.05),
        'w_in': nrm(ks[3], (DEPTH, D_MODEL, IN_COLS), D_MODEL ** -0.5),
        'fox_f_bias': 3.0 + nrm(ks[4], (DEPTH, FOX_HEADS), 1.0),
        'fox_q_gain': 1.0 + nrm(ks[5], (DEPTH, HEAD_DIM), 0.05),
        'fox_k_gain': 1.0 + nrm(ks[6], (DEPTH, HEAD_DIM), 0.05),
        'conv_w': nrm(ks[7], (DEPTH, CONV_K, CONV_W), CONV_K ** -0.5),
        'mlstm_i_bias': nrm(ks[8], (DEPTH, MLSTM_HEADS), 0.1),
        'mlstm_f_bias': 4.0 + nrm(ks[9], (DEPTH, MLSTM_HEADS), 1.0),
        'mlstm_h_gain': 1.0 + nrm(ks[10], (DEPTH, MLSTM_W), 0.05),
        'swa_q_gain': 1.0 + nrm(ks[11], (DEPTH, HEAD_DIM), 0.05),
        'swa_k_gain': 1.0 + nrm(ks[12], (DEPTH, HEAD_DIM), 0.05),
        'swa_sinks': nrm(ks[13], (DEPTH, SWA_Q_HEADS), 1.0),
        'w_fox_out': nrm(ks[14], (DEPTH, FOX_W, D_MODEL), FOX_W ** -0.5),
        'w_conv_out': nrm(ks[15], (DEPTH, CONV_W, D_MODEL), CONV_W ** -0.5),
        'w_mlstm_out': nrm(ks[16], (DEPTH, MLSTM_W, D_MODEL), MLSTM_W ** -0.5),
        'w_swa_out': nrm(ks[17], (DEPTH, SWA_W, D_MODEL), SWA_W ** -0.5),
        'w_merge_out': nrm(ks[18], (DEPTH, D_MODEL, D_MODEL), D_MODEL ** -0.5),
        'ffn_norm': 1.0 + nrm(ks[19], (DEPTH, D_MODEL), 0.05),
        'w_gate': nrm(ks[20], (DEPTH, D_MODEL, D_FF), D_MODEL ** -0.5),
        'w_up': nrm(ks[21], (DEPTH, D_MODEL, D_FF), D_MODEL ** -0.5),
        'w_down': nrm(ks[22], (DEPTH, D_FF, D_MODEL), D_FF ** -0.5),
    }


def reference(x, rel_bias, attn_norm, w_in, fox_f_bias, fox_q_gain, fox_k_gain, conv_w,
              mlstm_i_bias, mlstm_f_bias, mlstm_h_gain, swa_q_gain, swa_k_gain, swa_sinks,
              w_fox_out, w_conv_out, w_mlstm_out, w_swa_out, w_merge_out, ffn_norm,
              w_gate, w_up, w_down):
    B, S, _ = x.shape
    split_points = np.cumsum(SPLIT_SIZES)[:-1].tolist()

    dist = jnp.arange(WINDOW)[:, None] + WINDOW - jnp.arange(2 * WINDOW)[None, :]
    swa_bias = rel_bias[t5_bucket(jnp.maximum(dist, 0))].transpose(2, 0, 1)

    for l in range(DEPTH):
        h = rms_norm(x, attn_norm[l])
        proj = jnp.einsum('bsd,dc->bsc', h, w_in[l])
        (fq, fk, fv, ff, cu, cb, cc, mq, mk, mv, mi, mf, mo, sq, sk, sv,
         gates) = jnp.split(proj, split_points, axis=-1)

        fq = rms_norm(fq.reshape(B, S, FOX_HEADS, HEAD_DIM), fox_q_gain[l])
        fk = rms_norm(fk.reshape(B, S, FOX_HEADS, HEAD_DIM), fox_k_gain[l])
        fv = fv.reshape(B, S, FOX_HEADS, HEAD_DIM)
        y_fox = fox_attention(fq, fk, fv, ff + fox_f_bias[l])

        y_conv = short_conv_mixer(cu, cb, cc, conv_w[l])

        hm = mlstm_chunkwise(mq.reshape(B, S, MLSTM_HEADS, HEAD_DIM),
                             mk.reshape(B, S, MLSTM_HEADS, HEAD_DIM),
                             mv.reshape(B, S, MLSTM_HEADS, HEAD_DIM),
                             mi + mlstm_i_bias[l], mf + mlstm_f_bias[l])
        hm = rms_norm(hm, mlstm_h_gain[l].reshape(MLSTM_HEADS, HEAD_DIM))
        y_mlstm = jax.nn.sigmoid(mo) * hm.reshape(B, S, MLSTM_W)

        sq = rms_norm(sq.reshape(B, S, SWA_Q_HEADS, HEAD_DIM), swa_q_gain[l])
        sk = rms_norm(sk.reshape(B, S, SWA_KV_HEADS, HEAD_DIM), swa_k_gain[l])
        sv = sv.reshape(B, S, SWA_KV_HEADS, HEAD_DIM)
        y_swa = swa_attention(sq, sk, sv, swa_sinks[l], swa_bias)

        g = jax.nn.sigmoid(gates.reshape(B, S, N_BRANCH, D_MODEL))
        merged = (g[:, :, 0] * jnp.einsum('bsw,wd->bsd', y_fox, w_fox_out[l])
                  + g[:, :, 1] * jnp.einsum('bsw,wd->bsd', y_conv, w_conv_out[l])
                  + g[:, :, 2] * jnp.einsum('bsw,wd->bsd', y_mlstm, w_mlstm_out[l])
                  + g[:, :, 3] * jnp.einsum('bsw,wd->bsd', y_swa, w_swa_out[l]))
        x = x + jnp.einsum('bsd,de->bse', merged, w_merge_out[l])

        h = rms_norm(x, ffn_norm[l])
        act = jax.nn.silu(jnp.einsum('bsd,df->bsf', h, w_gate[l])) * jnp.einsum('bsd,df->bsf', h, w_up[l])
        x = x + jnp.einsum('bsf,fd->bsd', act, w_down[l])
    return x
```

```python
import contextlib
import math
import numpy as np
import concourse.bass as bass
import concourse.mybir as mybir
from concourse.bass_utils import run_bass_kernel_spmd

F32 = mybir.dt.float32
BF16 = mybir.dt.bfloat16
ALU = mybir.AluOpType
AF = mybir.ActivationFunctionType
AX = mybir.AxisListType

D = 1024
SEQ = 2048
DEPTH = 2
DFF = 2816
INC = 7436
EPS = 1e-6
NEG = -30000.0
C_FOX, C_CONV, C_ML, C_SWA, C_GATE = 0, 772, 1540, 2572, 3340

COMPUTE = ("pe", "act", "dve", "pool")
ISSUERS = ("pe", "act", "dve", "pool", "sp")


class Op:
    __slots__ = ("eng", "fn", "waits", "marked", "idx", "vc", "dma_sem", "dma_val", "count")

    def __init__(self, eng, fn):
        self.eng = eng
        self.fn = fn
        self.waits = []
        self.marked = False
        self.idx = -1
        self.vc = None
        self.dma_sem = None
        self.dma_val = 0
        self.count = 0


class Prog:
    def __init__(self):
        self.ops = {e: [] for e in ISSUERS}
        self.last_w = {}
        self.readers = {}
        self.known = {e: {} for e in ISSUERS}
        self.known_dma = {e: {} for e in ISSUERS}
        self.dma_count = {}

    def _dep(self, op, prod):
        if prod is None or prod is op:
            return
        A = op.eng
        if prod.dma_sem is not None:
            val = self.dma_count[prod.dma_sem]
            if self.known_dma[A].get(prod.dma_sem, 0) >= val:
                return
            self.known_dma[A][prod.dma_sem] = val
            op.waits.append((prod, val))
        else:
            B = prod.eng
            if B == "pe" and A == "pe":
                return
            if self.known[A].get(B, -1) >= prod.idx:
                return
            op.waits.append((prod, None))
            prod.marked = True
        for b, n in prod.vc[0].items():
            if self.known[A].get(b, -1) < n:
                self.known[A][b] = n
        for s, v in prod.vc[1].items():
            if self.known_dma[A].get(s, 0) < v:
                self.known_dma[A][s] = v

    def last(self, eng):
        return self.ops[eng][-1] if self.ops[eng] else None

    def emit(self, eng, fn, reads=(), writes=(), dma_sem=None, after=()):
        op = Op(eng, fn)
        op.idx = len(self.ops[eng])
        for r in reads:
            self._dep(op, self.last_w.get(r))
        for w in writes:
            self._dep(op, self.last_w.get(w))
            for rd in self.readers.get(w, ()):
                self._dep(op, rd)
        for a in after:
            self._dep(op, a)
        if dma_sem is not None:
            op.dma_sem = dma_sem
            self.dma_count[dma_sem] = self.dma_count.get(dma_sem, 0) + 16
            op.dma_val = self.dma_count[dma_sem]
        kc = dict(self.known[eng])
        if dma_sem is None:
            kc[eng] = op.idx
            op.vc = (kc, dict(self.known_dma[eng]))
        else:
            kd = dict(self.known_dma[eng])
            kd[dma_sem] = op.dma_val
            op.vc = (kc, kd)
        for r in reads:
            self.readers.setdefault(r, []).append(op)
        for w in writes:
            self.last_w[w] = op
            self.readers[w] = []
        self.ops[eng].append(op)
        return op

    def finalize(self, nc):
        for e in ISSUERS:
            c = 0
            for op in self.ops[e]:
                if op.dma_sem is None and op.marked:
                    c += 1
                op.count = c
        with contextlib.ExitStack() as st:
            esem = {e: st.enter_context(nc.semaphore("s_" + e)) for e in COMPUTE}
            dsem = {k: st.enter_context(nc.semaphore("d_" + str(k))) for k in self.dma_count}
            block = st.enter_context(nc.Block())

            def run(e, engine):
                for op in self.ops[e]:
                    for p, val in op.waits:
                        if p.dma_sem is not None:
                            engine.wait_ge(dsem[p.dma_sem], val)
                        else:
                            engine.wait_ge(esem[p.eng], p.count)
                    ins = op.fn(engine)
                    if op.dma_sem is not None:
                        ins.then_inc(dsem[op.dma_sem], 16)
                    elif op.marked:
                        ins.then_inc(esem[e], 1)

            @block.tensor
            def _(eng):
                run("pe", eng)

            @block.scalar
            def _(eng):
                run("act", eng)

            @block.vector
            def _(eng):
                run("dve", eng)

            @block.gpsimd
            def _(eng):
                run("pool", eng)

            @block.sync
            def _(eng):
                run("sp", eng)


WEIGHTS = [
    ("w_in", D, INC), ("w_fox_out", 256, D), ("w_conv_out", 256, D), ("w_mlstm_out", 256, D),
    ("w_swa_out", 512, D), ("w_merge_out", D, D), ("w_gate", D, DFF), ("w_up", D, DFF),
    ("w_down", DFF, D),
]
ROWP = 532
RP_FQG, RP_FKG, RP_SQG, RP_SKG, RP_MHG, RP_FFB, RP_MIB, RP_MFB, RP_SNK = 0, 64, 128, 192, 256, 512, 516, 520, 524


class _Stop(Exception):
    pass


def build(n_seq=2, n_tiles=8, n_layers=2, NSUB=2, seq_len=SEQ, stop_at=99, do_cast=True):
    T = NSUB * 128
    nc = bass.Bass("TRN2", target_bir_lowering=False)
    NTOK = n_seq * seq_len
    x_d = nc.dram_tensor("x", [NTOK, D], F32, kind="ExternalInput").ap()
    out_d = nc.dram_tensor("out", [NTOK, D], F32, kind="ExternalOutput").ap()
    wf = {}
    wb = {}
    for name, r, c in WEIGHTS:
        wf[name] = nc.dram_tensor(name, [DEPTH, r, c], F32, kind="ExternalInput").ap()
        wb[name] = nc.dram_tensor(name + "_b", [DEPTH, r, c], BF16, kind="Internal").ap()
    cst_d = nc.dram_tensor("cst", [128, 6, 128], F32, kind="ExternalInput").ap()
    swab_d = nc.dram_tensor("swab", [128, 2, 8, 128], F32, kind="ExternalInput").ap()
    swam_d = nc.dram_tensor("swam", [128, 2, 128], F32, kind="ExternalInput").ap()
    gT_d = nc.dram_tensor("gT", [128, 32], F32, kind="ExternalInput").ap()
    rowp_d = nc.dram_tensor("rowp", [1, DEPTH * ROWP], F32, kind="ExternalInput").ap()
    convw_d = nc.dram_tensor("convw", [128, 12], F32, kind="ExternalInput").ap()

    P = Prog()
    st = contextlib.ExitStack()
    with st:
        def sb(name, shape, dt):
            return st.enter_context(nc.sbuf_tensor("s_" + name, shape, dt))

        def psum(name, shape, dt):
            return st.enter_context(nc.psum_tensor("p_" + name, shape, dt))

        def tt(eng, out, in0, in1, op, R, W):
            return P.emit(eng, lambda e: e.tensor_tensor(out=out, in0=in0, in1=in1, op=op), R, W)

        def ts(eng, out, in0, s1, s2, op0, op1, R, W):
            if s2 is None:
                return P.emit(eng, lambda e: e.tensor_scalar(out=out, in0=in0, scalar1=s1, scalar2=None, op0=op0), R, W)
            return P.emit(eng, lambda e: e.tensor_scalar(out=out, in0=in0, scalar1=s1, scalar2=s2, op0=op0, op1=op1), R, W)

        def stt(eng, out, in0, scalar, in1, op0, op1, R, W):
            return P.emit(eng, lambda e: e.scalar_tensor_tensor(out=out, in0=in0, scalar=scalar, in1=in1, op0=op0, op1=op1), R, W)

        def cp(eng, out, in_, R, W):
            if eng == "act":
                return P.emit("act", lambda e: e.copy(out=out, in_=in_), R, W)
            return P.emit(eng, lambda e: e.tensor_copy(out=out, in_=in_), R, W)

        def act(out, in_, func, R, W, bias=None, scale=1.0, accum=None):
            kw = {}
            if bias is not None:
                kw["bias"] = bias
            if accum is not None:
                kw["accum_out"] = accum
            return P.emit("act", lambda e: e.activation(out=out, in_=in_, func=func, scale=scale, **kw), R, W)

        def mm(out, lhsT, rhs, start, stop, R, W):
            return P.emit("pe", lambda e: e.matmul(out, lhsT=lhsT, rhs=rhs, start=start, stop=stop), R, W)

        def tr(out, in_, R, W):
            return P.emit("pe", lambda e: e.transpose(out=out, in_=in_, identity=identb[:]), list(R) + ["identb"], W)

        def mset(eng, ap, val, W):
            return P.emit(eng, lambda e: e.memset(ap, val), (), W)

        def recip(out, in_, R, W):
            return P.emit("dve", lambda e: e.reciprocal(out=out, in_=in_), R, W)

        def dma(eng, out, in_, R, W, sem):
            return P.emit(eng, lambda e: e.dma_start(out=out, in_=in_), R, W, dma_sem=sem)

        class Rot:
            def __init__(self, items):
                self.items = items
                self.i = 0

            def next(self):
                it = self.items[self.i % len(self.items)]
                self.i += 1
                return it

        mmR = Rot([(psum("mm%d" % i, [128, 512], F32), "mm%d" % i) for i in range(2)])
        tpR = Rot([(psum("tp%d" % i, [128, 8, 128], BF16), "tp%d" % i) for i in range(2)])
        scR = Rot([(psum("sc%d" % i, [128, 512], F32), "sc%d" % i) for i in range(2)])
        aoR = Rot([(psum("ao%d" % i, [128, 512], F32), "ao%d" % i) for i in range(2)])

        cst = sb("cst", [128, 6, 128], F32)
        identb = sb("identb", [128, 128], BF16)
        m01b = sb("m01b", [128, 128], BF16)
        chunkind = sb("chunkind", [128, 2, 64], F32)
        bm = sb("bm", [128, 2, 8, 128], F32)
        swam = sb("swam", [128, 2, 128], F32)
        gT = sb("gT", [128, 32], F32)
        rowp = sb("rowp", [128, DEPTH * ROWP], F32)
        convw = sb("convw", [128, 12], F32)
        expsink = sb("expsink", [128, DEPTH, 8], F32)
        tri, triblk, cmaskT, ones = cst[:, 1, :], cst[:, 2, :], cst[:, 3, :], cst[:, 4, :]

        dma("sp", cst[:], cst_d, (), ["cst"], "c0")
        dma("sp", bm[:], swab_d, (), ["bm"], "c1")
        dma("sp", swam[:], swam_d, (), ["swam"], "c2")
        dma("sp", gT[:], gT_d, (), ["gT"], "c3")
        dma("sp", rowp[:], rowp_d.partition_broadcast(128), (), ["rowp"], "c4")
        dma("sp", convw[:], convw_d, (), ["convw"], "c5")
        cp("dve", identb[:], cst[:, 0, :], ["cst"], ["identb"])
        cp("dve", m01b[:], cst[:, 5, :], ["cst"], ["m01b"])
        mset("pool", chunkind[:], 0.0, ["chunkind"])
        mset("pool", chunkind[0:64, 0, :], 1.0, ["chunkind"])
        mset("pool", chunkind[64:128, 1, :], 1.0, ["chunkind"])
        for blk in range(2):
            tt("dve", bm[:, blk, :, :], bm[:, blk, :, :], swam[:, blk, :].unsqueeze(1).to_broadcast([128, 8, 128]),
               ALU.add, ["bm", "swam"], ["bm"])
        for l in range(DEPTH):
            act(expsink[:, l, :], rowp[:, l * ROWP + RP_SNK: l * ROWP + RP_SNK + 8], AF.Exp, ["rowp"], ["expsink"])

        wbkeys = {}

        def cast_weight(name, l, rows, cols):
            blk = max(16, min(rows, (1 << 19) // cols // 16 * 16))
            r = 0
            keys = []
            while r < rows:
                rr = min(blk, rows - r)
                keys.append(("wb", name, l, r))
                dma("pool", wb[name][l, r:r + rr, :], wf[name][l, r:r + rr, :], (), [keys[-1]],
                    "wb_%s_%d" % (name, l))
                r += rr
            wbkeys[(name, l)] = keys

        for l in range(n_layers):
            for name, r, c in WEIGHTS:
                if do_cast:
                    cast_weight(name, l, r, c)
                else:
                    wbkeys[(name, l)] = []

        NSLOT = 4
        wslots = [sb("wslot%d" % i, [128, 8, 512], BF16) for i in range(NSLOT)]

        def layer_chunks(l):
            ch = []

            def win(c0, ncol):
                ch.append(("w_in", l, wb["w_in"][l, :, c0:c0 + ncol].rearrange("(k p) c -> p k c", p=128), 8, ncol))

            win(C_FOX, 512)
            win(C_FOX + 512, 260)
            win(C_SWA + 512, 256)
            win(C_SWA, 512)
            win(C_ML + 512, 264)
            win(C_ML + 776, 256)
            win(C_ML, 512)
            win(C_CONV, 512)
            win(C_CONV + 512, 256)
            for b, (nm, kc, ) in enumerate([("w_fox_out", 2), ("w_conv_out", 2), ("w_mlstm_out", 2), ("w_swa_out", 4)]):
                if kc == 2:
                    ch.append((nm, l, wb[nm][l].rearrange("(k p) c -> p k c", p=128), 2, 1024))
                else:
                    ch.append((nm, l, wb[nm][l].rearrange("(k p) c -> p k c", p=128), 4, 1024))
                win(C_GATE + b * 1024, 512)
                win(C_GATE + b * 1024 + 512, 512)
            for h in range(2):
                ch.append(("w_merge_out", l, wb["w_merge_out"][l, :, h * 512:(h + 1) * 512].rearrange("(k p) c -> p k c", p=128), 8, 512))
            for c in range(6):
                ncol = min(512, DFF - c * 512)
                ch.append(("w_gate", l, wb["w_gate"][l, :, c * 512:c * 512 + ncol].rearrange("(k p) c -> p k c", p=128), 8, ncol))
                ch.append(("w_up", l, wb["w_up"][l, :, c * 512:c * 512 + ncol].rearrange("(k p) c -> p k c", p=128), 8, ncol))
            for h in range(2):
                for c in range(3):
                    k0 = c * 8
                    nk = min(8, 22 - k0)
                    ch.append(("w_down", l, wb["w_down"][l, k0 * 128:(k0 + nk) * 128, h * 512:(h + 1) * 512].rearrange("(k p) c -> p k c", p=128), nk, 512))
            return ch

        sched = []
        for s in range(n_seq):
            for t in range(n_tiles):
                for l in range(n_layers):
                    sched.extend(layer_chunks(l))

        class WStream:
            def __init__(self):
                self.issued = 0
                self.taken = 0
                self.holds = {}

            def _issue(self, n):
                name, l, src, nk, ncol = sched[n]
                slot = n % NSLOT
                key = ("ws", slot)
                view = wslots[slot][:].rearrange("p k c -> p (k c)")[:, 0:nk * ncol].rearrange("p (k c) -> p k c", c=ncol)
                dma("sp", view, src, wbkeys[(name, l)], [key], "ws%d" % slot)

            def next(self, expect, hold=0):
                m = self.taken
                while self.issued < len(sched) and self.issued < m + NSLOT:
                    k = self.issued
                    prev = k - NSLOT
                    if prev >= 0 and not (prev + 1 + self.holds[prev] <= m):
                        break
                    self._issue(k)
                    self.issued += 1
                n = self.taken
                assert self.issued > n
                self.holds[n] = hold
                self.taken += 1
                name, l, src, nk, ncol = sched[n]
                assert name == expect, (name, expect)
                slot = n % NSLOT
                view = wslots[slot][:].rearrange("p k c -> p (k c)")[:, 0:nk * ncol].rearrange("p (k c) -> p k c", c=ncol)
                return view, ("ws", slot)

        WS = WStream()

        xt = sb("xt", [128, NSUB, D], F32)
        hT = sb("hT", [128, 8, T], BF16)
        hb = sb("hb", [128, D], BF16)
        junk = sb("junk", [128, D], BF16)
        small = sb("small", [128, 64], F32)
        stages = [sb("stage%d" % i, [128, 512], F32) for i in range(3)]
        stR = Rot([(stages[i], "stage%d" % i) for i in range(3)])
        sqtmp = sb("sqtmp", [128, 512], F32)
        nrm_ss = sb("nrm_ss", [128, 8], F32)
        qkn = sb("qkn", [128, 512], BF16)
        fqT = sb("fqT", [128, 2, T], BF16)
        fkT = [sb("fkT%d" % l, [128, 2, seq_len], BF16) for l in range(n_layers)]
        fV = [sb("fV%d" % l, [128, seq_len // 128, 4, 65], BF16) for l in range(n_layers)]
        fnegc = [sb("fnegc%d" % l, [128, seq_len // 128, 4], F32) for l in range(n_layers)]
        fncar = [sb("fncar%d" % l, [128, NSUB + 1, 4], F32) for l in range(n_layers)]
        fsp = sb("fsp", [128, NSUB, 4], F32)
        fxf = sb("fxf", [128, 4], F32)
        fD = sb("fD", [128, 128], F32)
        fcbc = [sb("fcbc%d" % i, [128, T], F32) for i in range(2)]
        atmp = [sb("atmp%d" % i, [128, 512], F32) for i in range(2)]
        apT = [sb("apT%d" % i, [128, 512], BF16) for i in range(2)]
        frec = sb("frec", [128, 8], F32)
        yfox = sb("yfox", [128, NSUB, 256], BF16)
        sqT = [sb("sqT%d" % i, [64, 8, 128], BF16) for i in range(2)]
        skT = [sb("skT%d" % l, [64, NSUB + 1, 2, 128], BF16) for l in range(n_layers)]
        sV = [sb("sV%d" % l, [128, NSUB + 1, 2, 65], BF16) for l in range(n_layers)]
        skst = sb("skst", [128, 128], F32)
        skn = sb("skn", [128, 128], BF16)
        yswa = [sb("yswa%d" % i, [128, 512], BF16) for i in range(2)]
        sden = sb("sden", [128, 4], F32)
        mV = sb("mV", [128, NSUB, 4, 65], BF16)
        mif = sb("mif", [128, NSUB, 8], F32)
        msp = sb("msp", [128, NSUB, 4], F32)
        mea = sb("mea", [128, NSUB, 4], F32)
        meb = sb("meb", [128, NSUB, 4], F32)
        mdec = sb("mdec", [64, NSUB, 2, 4], F32)
        mso = sb("mso", [128, NSUB, 256], F32)
        mkt = sb("mkt", [128, 256], BF16)
        mqb = sb("mqb", [128, 256], BF16)
        mqT = sb("mqT", [64, 4, 128], BF16)
        mq0T = sb("mq0T", [64, 4, 128], BF16)
        mq1T = sb("mq1T", [64, 4, 128], BF16)
        mkT = sb("mkT", [64, 4, 128], BF16)
        mPT = sb("mPT", [128, 4, 128], BF16)
        mCf = [sb("mCf%d" % l, [64, 4, 65], F32) for l in range(n_layers)]
        mCb = [[sb("mCb%d_%d" % (l, i), [64, 4, 65], BF16) for i in range(2)] for l in range(n_layers)]
        mt1 = sb("mt1", [128, 8], F32)
        mp2 = sb("mp2", [128, 16], F32)
        mhm = sb("mhm", [128, 256], F32)
        ymls = sb("ymls", [128, 256], BF16)
        cu = sb("cu", [128, 2, T], F32)
        cbt = sb("cbt", [128, 2, T], F32)
        cz = [sb("cz%d" % l, [128, 2, T + 2], F32) for l in range(n_layers)]
        cy = sb("cy", [128, T], F32)
        yT = sb("yT", [128, 10, T], BF16)
        sg = [sb("sg%d" % i, [128, 512], F32) for i in range(2)]
        mtmp = sb("mtmp", [128, 512], F32)
        merged = sb("merged", [128, NSUB, D], F32)
        actT = sb("actT", [128, 22, T], BF16)

        def rp(l, off, n):
            return rowp[:, l * ROWP + off: l * ROWP + off + n]

        mset("pool", mq0T[:], 0.0, ["mq0T"])
        mset("pool", mq1T[:], 0.0, ["mq1T"])
        mset("pool", mV[:], 1.0, ["mV"])
        for l in range(n_layers):
            mset("pool", fV[l][:], 1.0, [("fV", l)])
            mset("pool", sV[l][:], 1.0, [("sV", l)])

        def head_norm(src, nh, gain_views, out_bf, srcR, outW, out_scale=1.0):
            w = nh * 64
            tt("pool", sqtmp[:, 0:w], src, src, ALU.mult, srcR, ["sqtmp"])
            P.emit("dve", lambda e: e.tensor_reduce(out=nrm_ss[:, 0:nh], in_=sqtmp[:, 0:w].rearrange("p (h d) -> p h d", d=64),
                                                     axis=AX.X, op=ALU.add), ["sqtmp"], ["nrm_ss"])
            act(nrm_ss[:, 0:nh], nrm_ss[:, 0:nh], AF.Sqrt, ["nrm_ss"], ["nrm_ss"], bias=EPS, scale=1.0 / 64)
            recip(nrm_ss[:, 0:nh], nrm_ss[:, 0:nh], ["nrm_ss"], ["nrm_ss"])
            tt("dve", sqtmp[:, 0:w].rearrange("p (h d) -> p h d", d=64), src.rearrange("p (h d) -> p h d", d=64),
               nrm_ss[:, 0:nh].unsqueeze(2).to_broadcast([128, nh, 64]), ALU.mult, list(srcR) + ["nrm_ss"], ["sqtmp"])
            for h0, h1, g in gain_views:
                n = h1 - h0
                tt("pool", out_bf[:, h0 * 64:h1 * 64].rearrange("p (h d) -> p h d", d=64),
                   sqtmp[:, h0 * 64:h1 * 64].rearrange("p (h d) -> p h d", d=64),
                   g.unsqueeze(1).to_broadcast([128, n, 64]), ALU.mult, ["sqtmp", "rowp"], outW)

        def resid_norm(l, which):
            for a in range(NSUB):
                mset("pool", small[:, 0:1], 0.0, ["small0"])
                act(junk[:], xt[:, a, :], AF.Square, [("xt", a), "small0"], ["junk", "small0"], accum=small[:, 0:1])
                act(small[:, 1:2], small[:, 0:1], AF.Sqrt, ["small0"], ["small1"], bias=EPS, scale=1.0 / D)
                recip(small[:, 2:3], small[:, 1:2], ["small1"], ["small2"])
                ts("dve", hb[:], xt[:, a, :], small[:, 2:3], None, ALU.mult, None, [("xt", a), "small2"], ["hb"])
                tp, tpk = tpR.next()
                for kc in range(8):
                    tr(tp[:, kc, :], hb[:, kc * 128:(kc + 1) * 128], ["hb"], [tpk])
                g0 = l * 16 + which * 8
                tt("dve", hT[:, :, a * 128:(a + 1) * 128], tp[:, :, :],
                   gT[:, g0:g0 + 8].unsqueeze(2).to_broadcast([128, 8, 128]), ALU.mult, [tpk, "gT"], [("hT", a)])

        HT_ALL = [("hT", a) for a in range(NSUB)]

        def proj_tok(W, wk, ncol, a):
            ps, pk = mmR.next()
            for kc in range(8):
                mm(ps[:, 0:ncol], hT[:, kc, a * 128:(a + 1) * 128], W[:, kc, :], kc == 0, kc == 7, [("hT", a), wk], [pk])
            return ps, pk

        def main_loop():
          for s in range(n_seq):
            for t in range(n_tiles):
                tok0 = s * seq_len + t * T
                blk0 = t * NSUB
                first_tile = (t == 0)
                for a in range(NSUB):
                    dma("sp", xt[:, a, :], x_d[tok0 + a * 128: tok0 + (a + 1) * 128, :], (), [("xt", a)], "xt")
                for l in range(n_layers):
                    if first_tile:
                        mset("pool", fncar[l][:, 0, :], 0.0, [("fncar", l)])
                        mset("pool", mCf[l][:], 0.0, [("mCf", l)])
                        mset("pool", mCb[l][0][:], 0.0, [("mCb", l, 0)])
                        mset("pool", cz[l][:, :, 0:2], 0.0, [("cz", l)])
                    if stop_at <= 0:
                        raise _Stop()
                    resid_norm(l, 0)

                    if stop_at <= 1:
                        raise _Stop()
                    W, wk = WS.next("w_in")
                    for a in range(NSUB):
                        ps, pk = proj_tok(W, wk, 512, a)
                        stg, sk = stR.next()
                        cp("act", stg[:], ps[:, 0:512], [pk], [sk])
                        if stop_at <= 1.1:
                            raise _Stop()
                        head_norm(stg[:], 8, [(0, 4, rp(l, RP_FQG, 64)), (4, 8, rp(l, RP_FKG, 64))], qkn, [sk], ["qkn"])
                        if stop_at <= 1.2:
                            raise _Stop()
                        tp, tpk = tpR.next()
                        for k in range(4):
                            tr(tp[:, k, :], qkn[:, k * 128:(k + 1) * 128], ["qkn"], [tpk])
                        cp("act", fqT[:, :, a * 128:(a + 1) * 128], tp[:, 0:2, :], [tpk], ["fqT"])
                        cp("act", fkT[l][:, :, (blk0 + a) * 128:(blk0 + a + 1) * 128], tp[:, 2:4, :], [tpk], [("fkT", l)])
                    if stop_at <= 1.3:
                        raise _Stop()
                    W, wk = WS.next("w_in")
                    for a in range(NSUB):
                        ps, pk = proj_tok(W, wk, 260, a)
                        cp("dve", fV[l][:, blk0 + a, :, 0:64], ps[:, 0:256].rearrange("p (h d) -> p h d", d=64), [pk], [("fV", l)])
                        if stop_at <= 1.4:
                            raise _Stop()
                        tt("dve", fxf[:], ps[:, 256:260], rp(l, RP_FFB, 4), ALU.add, [pk, "rowp"], ["fxf"])
                        act(fxf[:], fxf[:], AF.Exp, ["fxf"], ["fxf"], scale=-1.0)
                        act(fsp[:, a, :], fxf[:], AF.Ln, ["fxf"], [("fsp", a)], bias=1.0)
                        if stop_at <= 1.5:
                            raise _Stop()
                        p2, p2k = aoR.next()
                        mm(p2[:, 0:4], tri, fsp[:, a, :], True, True, ["cst", ("fsp", a)], [p2k])
                        mm(p2[:, 4:8], ones, fsp[:, a, :], True, True, ["cst", ("fsp", a)], [p2k])
                        tt("dve", fnegc[l][:, blk0 + a, :], p2[:, 0:4], fncar[l][:, a, :], ALU.add, [p2k, ("fncar", l)], [("fnegc", l)])
                        tt("dve", fncar[l][:, a + 1, :], p2[:, 4:8], fncar[l][:, a, :], ALU.add, [p2k, ("fncar", l)], [("fncar", l)])

                    if stop_at <= 2:
                        raise _Stop()
                    nblk = blk0 + NSUB
                    for h in range(4):
                        hp, hbase = h // 2, (h % 2) * 64
                        cb = fcbc[h % 2]
                        cbk = "fcbc%d" % (h % 2)
                        pc, pck = mmR.next()
                        for a in range(NSUB):
                            ts("dve", fD[:], ones, fsp[:, a, h:h + 1], None, ALU.mult, None, ["cst", ("fsp", a)], ["fD"])
                            mm(pc[:, a * 128:(a + 1) * 128], fD[:], tri, True, True, ["fD", "cst"], [pck])
                        for a in range(NSUB):
                            act(cb[:, a * 128:(a + 1) * 128], pc[:, a * 128:(a + 1) * 128], AF.Identity, [pck, ("fncar", l)], [cbk],
                                bias=fncar[l][:, a, h:h + 1])
                        acc, acck = aoR.next()
                        accv = acc[:, 0:NSUB * 65].rearrange("p (a c) -> p a c", c=65)
                        for j in range(nblk):
                            a0 = max(0, j - blk0)
                            c0 = a0 * 128
                            sc, sck = scR.next()
                            mm(sc[:, c0:T], fkT[l][hbase:hbase + 64, hp, j * 128:(j + 1) * 128], fqT[hbase:hbase + 64, hp, c0:T],
                               True, True, [("fkT", l), "fqT"], [sck])
                            bi = j % 2
                            tmp, tmpk, pT, pTk = atmp[bi], "atmp%d" % bi, apT[bi], "apT%d" % bi
                            stt("dve", tmp[:, c0:T], sc[:, c0:T], 0.125, cb[:, c0:T], ALU.mult, ALU.subtract, [sck, cbk], [tmpk])
                            if j >= blk0:
                                tt("pool", tmp[:, c0:c0 + 128], tmp[:, c0:c0 + 128], cmaskT, ALU.add, [tmpk, "cst"], [tmpk])
                            act(pT[:, c0:T], tmp[:, c0:T], AF.Exp, [tmpk, ("fnegc", l)], [pTk], bias=fnegc[l][:, j, h:h + 1])
                            for a in range(a0, NSUB):
                                mm(accv[:, a, :], pT[:, a * 128:(a + 1) * 128], fV[l][:, j, h, :], (j == 0 and a == 0), (j == nblk - 1 and a == NSUB - 1),
                                   [pTk, ("fV", l)], [acck])
                        recip(frec[:, 0:NSUB], accv[:, :, 64], [acck], ["frec"])
                        tt("dve", yfox[:, :, h * 64:(h + 1) * 64], accv[:, :, 0:64],
                           frec[:, 0:NSUB].unsqueeze(2).to_broadcast([128, NSUB, 64]), ALU.mult, [acck, "frec"], ["yfox"])
                    cp("pool", fncar[l][:, 0, :], fncar[l][:, NSUB, :], [("fncar", l)], [("fncar", l)])
                    for a in range(NSUB):
                        tp, tpk = tpR.next()
                        for k in range(2):
                            tr(tp[:, k, :], yfox[:, a, k * 128:(k + 1) * 128], ["yfox"], [tpk])
                        cp("act", yT[:, 0:2, a * 128:(a + 1) * 128], tp[:, 0:2, :], [tpk], [("yT", 0)])

                    if stop_at <= 3:
                        raise _Stop()
                    W, wk = WS.next("w_in")
                    for a in range(NSUB):
                        ps, pk = proj_tok(W, wk, 256, a)
                        cp("act", skst[:], ps[:, 0:128], [pk], ["skst"])
                        cp("act", sV[l][:, a + 1, :, 0:64], ps[:, 128:256].rearrange("p (h d) -> p h d", d=64), [pk], [("sV", l)])
                        head_norm(skst[:], 2, [(0, 2, rp(l, RP_SKG, 64))], skn, ["skst"], ["skn"])
                        tp, tpk = tpR.next()
                        for g in range(2):
                            tr(tp[0:64, g, :], skn[:, g * 64:(g + 1) * 64], ["skn"], [tpk])
                        cp("dve", skT[l][:, a + 1, :, :], tp[0:64, 0:2, :], [tpk], [("skT", l)])
                    W, wk = WS.next("w_in")
                    for a in range(NSUB):
                        ps, pk = proj_tok(W, wk, 512, a)
                        stg, sk = stR.next()
                        cp("act", stg[:], ps[:, 0:512], [pk], [sk])
                        head_norm(stg[:], 8, [(0, 8, rp(l, RP_SQG, 64))], qkn, [sk], ["qkn"])
                        qb_, qbk = sqT[a % 2], "sqT%d" % (a % 2)
                        for half in range(2):
                            tp, tpk = tpR.next()
                            for k in range(4):
                                hh = half * 4 + k
                                tr(tp[0:64, k, :], qkn[:, hh * 64:(hh + 1) * 64], ["qkn"], [tpk])
                            cp("act" if half else "dve", qb_[:, half * 4:half * 4 + 4, :], tp[0:64, 0:4, :], [tpk], [qbk])
                        has_prev = not (first_tile and a == 0)
                        yb, ybk = yswa[a % 2], "yswa%d" % (a % 2)
                        for g in range(2):
                            pTs = []
                            for which in ([0, 1] if has_prev else [1]):
                                blk = a + which
                                sc, sck = scR.next()
                                mm(sc[:], skT[l][:, blk, g, :], qb_[:, 4 * g:4 * g + 4, :].rearrange("p h t -> p (h t)"),
                                   True, True, [("skT", l), qbk], [sck])
                                tmp, tmpk, pT, pTk = atmp[which], "atmp%d" % which, apT[which], "apT%d" % which
                                stt("dve", tmp[:], sc[:], 0.125, bm[:, which, 4 * g:4 * g + 4, :].rearrange("p h t -> p (h t)"),
                                    ALU.mult, ALU.add, [sck, "bm"], [tmpk])
                                act(pT[:], tmp[:], AF.Exp, [tmpk], [pTk])
                                pTs.append((pT, pTk, blk))
                            ao, aok = aoR.next()
                            aov = ao[:, 0:260].rearrange("p (h c) -> p h c", c=65)
                            for hh in range(4):
                                for i, (pT, pTk, blk) in enumerate(pTs):
                                    mm(aov[:, hh, :], pT[:, hh * 128:(hh + 1) * 128], sV[l][:, blk, g, :], i == 0, i == len(pTs) - 1,
                                       [pTk, ("sV", l)], [aok])
                            tt("dve", sden[:], aov[:, :, 64], expsink[:, l, 4 * g:4 * g + 4], ALU.add, [aok, "expsink"], ["sden"])
                            recip(sden[:], sden[:], ["sden"], ["sden"])
                            tt("dve", yb[:, g * 256:(g + 1) * 256].rearrange("p (h d) -> p h d", d=64), aov[:, :, 0:64],
                               sden[:].unsqueeze(2).to_broadcast([128, 4, 64]), ALU.mult, [aok, "sden"], [ybk])
                        tp, tpk = tpR.next()
                        for k in range(4):
                            tr(tp[:, k, :], yb[:, k * 128:(k + 1) * 128], [ybk], [tpk])
                        cp("act", yT[:, 6:10, a * 128:(a + 1) * 128], tp[:, 0:4, :], [tpk], [("yT", 3)])
                    cp("pool", skT[l][:, 0, :, :], skT[l][:, NSUB, :, :], [("skT", l)], [("skT", l)])
                    cp("pool", sV[l][:, 0, :, :], sV[l][:, NSUB, :, :], [("sV", l)], [("sV", l)])

                    if stop_at <= 4:
                        raise _Stop()
                    W, wk = WS.next("w_in")
                    for a in range(NSUB):
                        ps, pk = proj_tok(W, wk, 264, a)
                        cp("dve", mV[:, a, :, 0:64], ps[:, 0:256].rearrange("p (h d) -> p h d", d=64), [pk], ["mV"])
                        tt("dve", mif[:, a, 0:4], ps[:, 256:260], rp(l, RP_MIB, 4), ALU.add, [pk, "rowp"], [("mif", a)])
                        tt("dve", mif[:, a, 4:8], ps[:, 260:264], rp(l, RP_MFB, 4), ALU.add, [pk, "rowp"], [("mif", a)])
                        act(mt1[:, 0:4], mif[:, a, 4:8], AF.Exp, [("mif", a)], ["mt1"], scale=-1.0)
                        act(msp[:, a, :], mt1[:, 0:4], AF.Ln, ["mt1"], [("msp", a)], bias=1.0)
                        p2, p2k = aoR.next()
                        mm(p2[:, 0:4], triblk, msp[:, a, :], True, True, ["cst", ("msp", a)], [p2k])
                        for c in range(2):
                            mm(p2[0:64, 8 + 4 * c:12 + 4 * c], chunkind[:, c, :], msp[:, a, :], True, True, ["chunkind", ("msp", a)], [p2k])
                        cp("dve", mp2[:], p2[:, 0:16], [p2k], ["mp2"])
                        tt("dve", mt1[:, 4:8], mp2[:, 0:4], mif[:, a, 0:4], ALU.add, ["mp2", ("mif", a)], ["mt1"])
                        act(mea[:, a, :], mt1[:, 4:8], AF.Exp, ["mt1"], [("mea", a)])
                        act(meb[:, a, :], mp2[:, 0:4], AF.Exp, ["mp2"], [("meb", a)], scale=-1.0)
                        act(mdec[:, a, :, :], mp2[0:64, 8:16].rearrange("p (c h) -> p c h", h=4), AF.Exp, ["mp2"], [("mdec", a)], scale=-1.0)
                    W, wk = WS.next("w_in")
                    for a in range(NSUB):
                        ps, pk = proj_tok(W, wk, 256, a)
                        act(mso[:, a, :], ps[:, 0:256], AF.Sigmoid, [pk], [("mso", a)])
                    W, wk = WS.next("w_in")
                    for a in range(NSUB):
                        ps, pk = proj_tok(W, wk, 512, a)
                        ts("dve", mqb[:], ps[:, 0:256], 0.125, None, ALU.mult, None, [pk], ["mqb"])
                        tt("dve", mkt[:].rearrange("p (h d) -> p h d", d=64), ps[:, 256:512].rearrange("p (h d) -> p h d", d=64),
                           mea[:, a, :].unsqueeze(2).to_broadcast([128, 4, 64]), ALU.mult, [pk, ("mea", a)], ["mkt"])
                        tq, tqk = tpR.next()
                        for h in range(4):
                            tr(tq[0:64, h, :], mqb[:, h * 64:(h + 1) * 64], ["mqb"], [tqk])
                        tk, tkk = tpR.next()
                        for h in range(4):
                            tr(tk[0:64, h, :], mkt[:, h * 64:(h + 1) * 64], ["mkt"], [tkk])
                        cp("dve", mqT[:], tq[0:64, 0:4, :], [tqk], ["mqT"])
                        cp("dve", mq0T[:, :, 0:64], tq[0:64, 0:4, 0:64], [tqk], ["mq0T"])
                        cp("dve", mq1T[:, :, 64:128], tq[0:64, 0:4, 64:128], [tqk], ["mq1T"])
                        cp("act", mkT[:], tk[0:64, 0:4, :], [tkk], ["mkT"])
                        sc, sck = scR.next()
                        scv = sc[:].rearrange("p (h t) -> p h t", t=128)
                        for h in range(4):
                            mm(scv[:, h, :], mkT[:, h, :], mqT[:, h, :], True, True, ["mkT", "mqT"], [sck])
                        tt("dve", mPT[:], scv, m01b[:].unsqueeze(1).to_broadcast([128, 4, 128]), ALU.mult, [sck, "m01b"], ["mPT"])
                        pd, pdk = aoR.next()
                        pdv = pd[0:64, 0:260].rearrange("p (h c) -> p h c", c=65)
                        for h in range(4):
                            mm(pdv[:, h, :], mkt[0:64, h * 64:(h + 1) * 64], mV[0:64, a, h, :], True, True, ["mkt", "mV"], [pdk])
                        tt("dve", mCf[l][:], mCf[l][:], pdv, ALU.add, [("mCf", l), pdk], [("mCf", l)])
                        tt("dve", mCf[l][:], mCf[l][:], mdec[:, a, 0, :].unsqueeze(2).to_broadcast([64, 4, 65]), ALU.mult,
                           [("mCf", l), ("mdec", a)], [("mCf", l)])
                        cp("pool", mCb[l][1][:], mCf[l][:], [("mCf", l)], [("mCb", l, 1)])
                        ao, aok = aoR.next()
                        aov = ao[:, 0:260].rearrange("p (h c) -> p h c", c=65)
                        for h in range(4):
                            mm(aov[:, h, :], mPT[:, h, :], mV[:, a, h, :], True, False, ["mPT", "mV"], [aok])
                            mm(aov[:, h, :], mq0T[:, h, :], mCb[l][0][:, h, :], False, False, ["mq0T", ("mCb", l, 0)], [aok])
                            mm(aov[:, h, :], mq1T[:, h, :], mCb[l][1][:, h, :], False, True, ["mq1T", ("mCb", l, 1)], [aok])
                        pd, pdk = aoR.next()
                        pdv = pd[0:64, 0:260].rearrange("p (h c) -> p h c", c=65)
                        for h in range(4):
                            mm(pdv[:, h, :], mkt[64:128, h * 64:(h + 1) * 64], mV[64:128, a, h, :], True, True, ["mkt", "mV"], [pdk])
                        tt("dve", mCf[l][:], mCf[l][:], pdv, ALU.add, [("mCf", l), pdk], [("mCf", l)])
                        tt("dve", mCf[l][:], mCf[l][:], mdec[:, a, 1, :].unsqueeze(2).to_broadcast([64, 4, 65]), ALU.mult,
                           [("mCf", l), ("mdec", a)], [("mCf", l)])
                        cp("pool", mCb[l][0][:], mCf[l][:], [("mCf", l)], [("mCb", l, 0)])
                        tt("dve", mt1[:, 0:4], aov[:, :, 64], meb[:, a, :], ALU.mult, [aok, ("meb", a)], ["mt1"])
                        stt("dve", mt1[:, 0:4], mt1[:, 0:4], -1.0, mt1[:, 0:4], ALU.mult, ALU.max, ["mt1"], ["mt1"])
                        P.emit("dve", lambda e: e.tensor_scalar_max(out=mt1[:, 0:4], in0=mt1[:, 0:4], scalar1=1.0), ["mt1"], ["mt1"])
                        recip(mt1[:, 0:4], mt1[:, 0:4], ["mt1"], ["mt1"])
                        tt("dve", mt1[:, 0:4], mt1[:, 0:4], meb[:, a, :], ALU.mult, ["mt1", ("meb", a)], ["mt1"])
                        tt("dve", mhm[:].rearrange("p (h d) -> p h d", d=64), aov[:, :, 0:64],
                           mt1[:, 0:4].unsqueeze(2).to_broadcast([128, 4, 64]), ALU.mult, [aok, "mt1"], ["mhm"])
                        tt("pool", sqtmp[:, 0:256], mhm[:], mhm[:], ALU.mult, ["mhm"], ["sqtmp"])
                        P.emit("dve", lambda e: e.tensor_reduce(out=nrm_ss[:, 0:4], in_=sqtmp[:, 0:256].rearrange("p (h d) -> p h d", d=64),
                                                                 axis=AX.X, op=ALU.add), ["sqtmp"], ["nrm_ss"])
                        act(nrm_ss[:, 0:4], nrm_ss[:, 0:4], AF.Sqrt, ["nrm_ss"], ["nrm_ss"], bias=EPS, scale=1.0 / 64)
                        recip(nrm_ss[:, 0:4], nrm_ss[:, 0:4], ["nrm_ss"], ["nrm_ss"])
                        tt("dve", mhm[:].rearrange("p (h d) -> p h d", d=64), mhm[:].rearrange("p (h d) -> p h d", d=64),
                           nrm_ss[:, 0:4].unsqueeze(2).to_broadcast([128, 4, 64]), ALU.mult, ["mhm", "nrm_ss"], ["mhm"])
                        tt("pool", mhm[:], mhm[:], rp(l, RP_MHG, 256), ALU.mult, ["mhm", "rowp"], ["mhm"])
                        tt("dve", ymls[:], mhm[:], mso[:, a, :], ALU.mult, ["mhm", ("mso", a)], ["ymls"])
                        tp, tpk = tpR.next()
                        for k in range(2):
                            tr(tp[:, k, :], ymls[:, k * 128:(k + 1) * 128], ["ymls"], [tpk])
                        cp("act", yT[:, 4:6, a * 128:(a + 1) * 128], tp[:, 0:2, :], [tpk], [("yT", 2)])

                    if stop_at <= 5:
                        raise _Stop()
                    W, wk = WS.next("w_in")
                    for cb_ in range(4):
                        ps, pk = mmR.next()
                        for kc in range(8):
                            mm(ps[:, 0:T], W[:, kc, cb_ * 128:(cb_ + 1) * 128], hT[:, kc, :], kc == 0, kc == 7, HT_ALL + [wk], [pk])
                        dst = cu if cb_ < 2 else cbt
                        cp("act", dst[:, cb_ % 2, :], ps[:, 0:T], [pk], ["cu" if cb_ < 2 else "cbt"])
                    W, wk = WS.next("w_in")
                    for cc in range(2):
                        ps, pk = mmR.next()
                        for kc in range(8):
                            mm(ps[:, 0:T], W[:, kc, cc * 128:(cc + 1) * 128], hT[:, kc, :], kc == 0, kc == 7, HT_ALL + [wk], [pk])
                        z = cz[l]
                        tt("dve", z[:, cc, 2:T + 2], ps[:, 0:T], cu[:, cc, :], ALU.mult, [pk, "cu"], [("cz", l)])
                        w0 = convw[:, l * 6 + cc * 3 + 0: l * 6 + cc * 3 + 1]
                        w1 = convw[:, l * 6 + cc * 3 + 1: l * 6 + cc * 3 + 2]
                        w2 = convw[:, l * 6 + cc * 3 + 2: l * 6 + cc * 3 + 3]
                        ts("pool", cy[:], z[:, cc, 2:T + 2], w2, None, ALU.mult, None, [("cz", l), "convw"], ["cy"])
                        stt("dve", cy[:], z[:, cc, 1:T + 1], w1, cy[:], ALU.mult, ALU.add, [("cz", l), "convw", "cy"], ["cy"])
                        stt("dve", cy[:], z[:, cc, 0:T], w0, cy[:], ALU.mult, ALU.add, [("cz", l), "convw", "cy"], ["cy"])
                        tt("dve", yT[:, 2 + cc, :], cy[:], cbt[:, cc, :], ALU.mult, ["cy", "cbt"], [("yT", 1)])
                        cp("pool", z[:, cc, 0:2], z[:, cc, T:T + 2], [("cz", l)], [("cz", l)])

                    if stop_at <= 6:
                        raise _Stop()
                    ych0 = [0, 2, 4, 6]
                    for b, nm in enumerate(["w_fox_out", "w_conv_out", "w_mlstm_out", "w_swa_out"]):
                        Wb, wbk = WS.next(nm, hold=2)
                        nkc = 4 if b == 3 else 2
                        for half in range(2):
                            Wg, wgk = WS.next("w_in")
                            for a in range(NSUB):
                                pg, pgk = proj_tok(Wg, wgk, 512, a)
                                s_, s_k = sg[a % 2], "sg%d" % (a % 2)
                                act(s_[:], pg[:, 0:512], AF.Sigmoid, [pgk], [s_k])
                                po, pok = mmR.next()
                                for kc in range(nkc):
                                    mm(po[:, 0:512], yT[:, ych0[b] + kc, a * 128:(a + 1) * 128], Wb[:, kc, half * 512:(half + 1) * 512],
                                       kc == 0, kc == nkc - 1, [("yT", b), wbk], [pok])
                                mdst = merged[:, a, half * 512:(half + 1) * 512]
                                if b == 0:
                                    tt("dve", mdst, po[:, 0:512], s_[:], ALU.mult, [pok, s_k], [("merged", a)])
                                else:
                                    tt("dve", mtmp[:], po[:, 0:512], s_[:], ALU.mult, [pok, s_k], ["mtmp"])
                                    tt("pool", mdst, mdst, mtmp[:], ALU.add, [("merged", a), "mtmp"], [("merged", a)])
                    for a in range(NSUB):
                        cp("act", hb[:], merged[:, a, :], [("merged", a)], ["hb"])
                        tp, tpk = tpR.next()
                        for kc in range(8):
                            tr(tp[:, kc, :], hb[:, kc * 128:(kc + 1) * 128], ["hb"], [tpk])
                        cp("act" if a % 2 else "dve", hT[:, :, a * 128:(a + 1) * 128], tp[:, :, :], [tpk], [("hT", a)])
                    for half in range(2):
                        Wm, wmk = WS.next("w_merge_out")
                        for a in range(NSUB):
                            ps, pk = proj_tok(Wm, wmk, 512, a)
                            xs = xt[:, a, half * 512:(half + 1) * 512]
                            tt("dve", xs, ps[:, 0:512], xs, ALU.add, [pk, ("xt", a)], [("xt", a)])

                    if stop_at <= 7:
                        raise _Stop()
                    resid_norm(l, 1)
                    for c in range(6):
                        ncol = min(512, DFF - c * 512)
                        Wg, wgk = WS.next("w_gate", hold=1)
                        Wu, wuk = WS.next("w_up")
                        for cb_ in range(ncol // 128):
                            ffc = c * 4 + cb_
                            pg, pgk = mmR.next()
                            for kc in range(8):
                                mm(pg[:, 0:T], Wg[:, kc, cb_ * 128:(cb_ + 1) * 128], hT[:, kc, :], kc == 0, kc == 7, HT_ALL + [wgk], [pgk])
                            pu, puk = mmR.next()
                            for kc in range(8):
                                mm(pu[:, 0:T], Wu[:, kc, cb_ * 128:(cb_ + 1) * 128], hT[:, kc, :], kc == 0, kc == 7, HT_ALL + [wuk], [puk])
                            s_, s_k = sg[ffc % 2], "sg%d" % (ffc % 2)
                            act(s_[:, 0:T], pg[:, 0:T], AF.Silu, [pgk], [s_k])
                            tt("dve", actT[:, ffc, :], pu[:, 0:T], s_[:, 0:T], ALU.mult, [puk, s_k], [("actT", ffc)])
                    ACT_ALL = [("actT", f) for f in range(22)]
                    for half in range(2):
                        accs = []
                        for a in range(NSUB):
                            accs.append(mmR.next() if a < 2 else scR.next())
                        for c in range(3):
                            Wd, wdk = WS.next("w_down")
                            k0 = c * 8
                            nk = min(8, 22 - k0)
                            for a in range(NSUB):
                                ps, pk = accs[a]
                                for k in range(nk):
                                    mm(ps[:, 0:512], actT[:, k0 + k, a * 128:(a + 1) * 128], Wd[:, k, :], (k0 + k) == 0, (k0 + k) == 21,
                                       ACT_ALL + [wdk], [pk])
                        for a in range(NSUB):
                            ps, pk = accs[a]
                            xs = xt[:, a, half * 512:(half + 1) * 512]
                            tt("dve", xs, ps[:, 0:512], xs, ALU.add, [pk, ("xt", a)], [("xt", a)])
                for a in range(NSUB):
                    dma("sp", out_d[tok0 + a * 128: tok0 + (a + 1) * 128, :], xt[:, a, :], [("xt", a)], ["out"], "out")
        try:
            main_loop()
        except _Stop:
            for a in range(NSUB):
                dma("sp", out_d[a * 128:(a + 1) * 128, :], xt[:, a, :], [("xt", a)], ["out"], "out")
        P.emit("sp", lambda e: e.nop(), ["out"], ())
        P.finalize(nc)
    return nc


def _t5_bucket(n):
    max_exact = 16
    nf = np.maximum(n, 1).astype(np.float32)
    large = max_exact + (np.log(nf / max_exact) / math.log(128 / max_exact) * (32 - max_exact)).astype(np.int32)
    large = np.minimum(large, 31)
    return np.where(n < max_exact, n, large)


def host_consts(inp):
    f32 = np.float32
    s = np.arange(128)[:, None]
    t = np.arange(128)[None, :]
    cst = np.zeros((128, 6, 128), f32)
    cst[:, 0] = np.eye(128, dtype=f32)
    cst[:, 1] = (s <= t)
    cst[:, 2] = (s <= t) & (s // 64 == t // 64)
    cst[:, 3] = np.where(s <= t, 0.0, NEG)
    cst[:, 4] = 1.0
    cst[:, 5] = cst[:, 2]
    j = np.arange(128)[:, None]
    i = np.arange(128)[None, :]
    swam = np.zeros((128, 2, 128), f32)
    d_prev = i + 128 - j
    d_cur = i - j
    swam[:, 0] = np.where((d_prev >= 0) & (d_prev < 128), 0.0, NEG)
    swam[:, 1] = np.where((d_cur >= 0) & (d_cur < 128), 0.0, NEG)
    rel = np.asarray(inp["rel_bias"], f32)
    swab = np.zeros((128, 2, 8, 128), f32)
    swab[:, 0] = rel[_t5_bucket(np.maximum(d_prev, 0))].transpose(0, 2, 1)
    swab[:, 1] = rel[_t5_bucket(np.maximum(d_cur, 0))].transpose(0, 2, 1)
    gT = np.zeros((128, 32), f32)
    for l in range(DEPTH):
        gT[:, l * 16:l * 16 + 8] = np.asarray(inp["attn_norm"][l], f32).reshape(8, 128).T
        gT[:, l * 16 + 8:l * 16 + 16] = np.asarray(inp["ffn_norm"][l], f32).reshape(8, 128).T
    rowp = np.zeros((1, DEPTH * ROWP), f32)
    for l in range(DEPTH):
        o = l * ROWP
        rowp[0, o + RP_FQG:o + RP_FQG + 64] = inp["fox_q_gain"][l]
        rowp[0, o + RP_FKG:o + RP_FKG + 64] = inp["fox_k_gain"][l]
        rowp[0, o + RP_SQG:o + RP_SQG + 64] = inp["swa_q_gain"][l]
        rowp[0, o + RP_SKG:o + RP_SKG + 64] = inp["swa_k_gain"][l]
        rowp[0, o + RP_MHG:o + RP_MHG + 256] = inp["mlstm_h_gain"][l]
        rowp[0, o + RP_FFB:o + RP_FFB + 4] = inp["fox_f_bias"][l]
        rowp[0, o + RP_MIB:o + RP_MIB + 4] = inp["mlstm_i_bias"][l]
        rowp[0, o + RP_MFB:o + RP_MFB + 4] = inp["mlstm_f_bias"][l]
        rowp[0, o + RP_SNK:o + RP_SNK + 8] = inp["swa_sinks"][l]
    convw = np.zeros((128, 12), f32)
    cw = np.asarray(inp["conv_w"], f32)
    for l in range(DEPTH):
        for cc in range(2):
            for k in range(3):
                convw[:, l * 6 + cc * 3 + k] = cw[l, k, cc * 128:(cc + 1) * 128]
    return {"cst": cst, "swab": swab, "swam": swam, "gT": gT, "rowp": rowp, "convw": convw}


_NC_CACHE = {}


def kernel(**inputs):
    inp = {k: np.asarray(v) for k, v in inputs.items()}
    x = np.ascontiguousarray(inp["x"], dtype=np.float32)
    n_cores = 8
    key = "full"
    if key not in _NC_CACHE:
        _NC_CACHE[key] = build()
    nc = _NC_CACHE[key]
    shared = host_consts(inp)
    for name, r, c in WEIGHTS:
        shared[name] = np.ascontiguousarray(inp[name], dtype=np.float32)
    in_maps = []
    for c in range(n_cores):
        m = dict(shared)
        m["x"] = x[2 * c:2 * c + 2].reshape(2 * SEQ, D)
        in_maps.append(m)
    res = run_bass_kernel_spmd(nc, in_maps, core_ids=list(range(n_cores)))
    out = np.stack([r["out"].reshape(2, SEQ, D) for r in res.results], axis=0).reshape(16, SEQ, D)
    return out.astype(np.float32)
```

```python
import contextlib
import math
import numpy as np
import concourse.bass as bass
import concourse.mybir as mybir
from concourse.bass_utils import run_bass_kernel_spmd

F32 = mybir.dt.float32
BF16 = mybir.dt.bfloat16
ALU = mybir.AluOpType
AF = mybir.ActivationFunctionType
AX = mybir.AxisListType

D = 1024
SEQ = 2048
DEPTH = 2
DFF = 2816
INC = 7436
EPS = 1e-6
NEG = -30000.0
C_FOX, C_CONV, C_ML, C_SWA, C_GATE = 0, 772, 1540, 2572, 3340

COMPUTE = ("pe", "act", "dve", "pool")
ISSUERS = ("pe", "act", "dve", "pool", "sp")


class Op:
    __slots__ = ("eng", "fn", "waits", "marked", "idx", "vc", "dma_sem", "dma_val", "count", "phase")

    def __init__(self, eng, fn):
        self.eng = eng
        self.fn = fn
        self.waits = []
        self.marked = False
        self.idx = -1
        self.vc = None
        self.dma_sem = None
        self.dma_val = 0
        self.count = 0


class Prog:
    def __init__(self):
        self.ops = {e: [] for e in ISSUERS}
        self.last_w = {}
        self.readers = {}
        self.known = {e: {} for e in ISSUERS}
        self.known_dma = {e: {} for e in ISSUERS}
        self.dma_count = {}
        self.phase = "init"
        self.annotate = False

    def _dep(self, op, prod):
        if prod is None or prod is op:
            return
        A = op.eng
        if prod.dma_sem is not None:
            val = self.dma_count[prod.dma_sem]
            if self.known_dma[A].get(prod.dma_sem, 0) >= val:
                return
            self.known_dma[A][prod.dma_sem] = val
            op.waits.append((prod, val))
        else:
            B = prod.eng
            if B == "pe" and A == "pe":
                return
            if self.known[A].get(B, -1) >= prod.idx:
                return
            op.waits.append((prod, None))
            prod.marked = True
        for b, n in prod.vc[0].items():
            if self.known[A].get(b, -1) < n:
                self.known[A][b] = n
        for s, v in prod.vc[1].items():
            if self.known_dma[A].get(s, 0) < v:
                self.known_dma[A][s] = v

    def last(self, eng):
        return self.ops[eng][-1] if self.ops[eng] else None

    def emit(self, eng, fn, reads=(), writes=(), dma_sem=None, after=()):
        op = Op(eng, fn)
        op.phase = self.phase
        op.idx = len(self.ops[eng])
        for r in reads:
            self._dep(op, self.last_w.get(r))
        for w in writes:
            self._dep(op, self.last_w.get(w))
            for rd in self.readers.get(w, ()):
                self._dep(op, rd)
        for a in after:
            self._dep(op, a)
        if dma_sem is not None:
            op.dma_sem = dma_sem
            self.dma_count[dma_sem] = self.dma_count.get(dma_sem, 0) + 16
            op.dma_val = self.dma_count[dma_sem]
        kc = dict(self.known[eng])
        if dma_sem is None:
            kc[eng] = op.idx
            op.vc = (kc, dict(self.known_dma[eng]))
        else:
            kd = dict(self.known_dma[eng])
            kd[dma_sem] = op.dma_val
            op.vc = (kc, kd)
        for r in reads:
            self.readers.setdefault(r, []).append(op)
        for w in writes:
            self.last_w[w] = op
            self.readers[w] = []
        self.ops[eng].append(op)
        return op

    def finalize(self, nc):
        for e in ISSUERS:
            c = 0
            for op in self.ops[e]:
                if op.dma_sem is None and op.marked:
                    c += 1
                op.count = c
        with contextlib.ExitStack() as st:
            esem = {e: st.enter_context(nc.semaphore("s_" + e)) for e in COMPUTE}
            dsem = {k: st.enter_context(nc.semaphore("d_" + str(k))) for k in self.dma_count}
            block = st.enter_context(nc.Block())

            def run(e, engine):
                for op in self.ops[e]:
                    for p, val in op.waits:
                        if p.dma_sem is not None:
                            engine.wait_ge(dsem[p.dma_sem], val)
                        else:
                            engine.wait_ge(esem[p.eng], p.count)
                    ins = op.fn(engine)
                    if self.annotate:
                        ins.annotate(op.phase)
                    if op.dma_sem is not None:
                        ins.then_inc(dsem[op.dma_sem], 16)
                    elif op.marked:
                        ins.then_inc(esem[e], 1)

            @block.tensor
            def _(eng):
                run("pe", eng)

            @block.scalar
            def _(eng):
                run("act", eng)

            @block.vector
            def _(eng):
                run("dve", eng)

            @block.gpsimd
            def _(eng):
                run("pool", eng)

            @block.sync
            def _(eng):
                run("sp", eng)


WEIGHTS = [
    ("w_in", D, INC), ("w_fox_out", 256, D), ("w_conv_out", 256, D), ("w_mlstm_out", 256, D),
    ("w_swa_out", 512, D), ("w_merge_out", D, D), ("w_gate", D, DFF), ("w_up", D, DFF),
    ("w_down", DFF, D),
]
ROWP = 532
RP_FQG, RP_FKG, RP_SQG, RP_SKG, RP_MHG, RP_FFB, RP_MIB, RP_MFB, RP_SNK = 0, 64, 128, 192, 256, 512, 516, 520, 524


class _Stop(Exception):
    pass


def build(n_seq=2, n_tiles=8, n_layers=2, NSUB=2, seq_len=SEQ, stop_at=99, do_cast=True, annotate=False):
    T = NSUB * 128
    nc = bass.Bass("TRN2", target_bir_lowering=False)
    NTOK = n_seq * seq_len
    x_d = nc.dram_tensor("x", [NTOK, D], F32, kind="ExternalInput").ap()
    out_d = nc.dram_tensor("out", [NTOK, D], F32, kind="ExternalOutput").ap()
    wf = {}
    wb = {}
    for name, r, c in WEIGHTS:
        wf[name] = nc.dram_tensor(name, [DEPTH, r, c], F32, kind="ExternalInput").ap()
        wb[name] = nc.dram_tensor(name + "_b", [DEPTH, r, c], BF16, kind="Internal").ap()
    cst_d = nc.dram_tensor("cst", [128, 6, 128], F32, kind="ExternalInput").ap()
    swab_d = nc.dram_tensor("swab", [128, 2, 8, 128], F32, kind="ExternalInput").ap()
    swam_d = nc.dram_tensor("swam", [128, 2, 128], F32, kind="ExternalInput").ap()
    gT_d = nc.dram_tensor("gT", [128, 32], F32, kind="ExternalInput").ap()
    rowp_d = nc.dram_tensor("rowp", [1, DEPTH * ROWP], F32, kind="ExternalInput").ap()
    convw_d = nc.dram_tensor("convw", [128, 12], F32, kind="ExternalInput").ap()

    P = Prog()
    P.annotate = annotate
    st = contextlib.ExitStack()
    with st:
        def sb(name, shape, dt):
            return st.enter_context(nc.sbuf_tensor("s_" + name, shape, dt))

        def psum(name, shape, dt):
            return st.enter_context(nc.psum_tensor("p_" + name, shape, dt))

        def tt(eng, out, in0, in1, op, R, W):
            return P.emit(eng, lambda e: e.tensor_tensor(out=out, in0=in0, in1=in1, op=op), R, W)

        def ts(eng, out, in0, s1, s2, op0, op1, R, W):
            if s2 is None:
                return P.emit(eng, lambda e: e.tensor_scalar(out=out, in0=in0, scalar1=s1, scalar2=None, op0=op0), R, W)
            return P.emit(eng, lambda e: e.tensor_scalar(out=out, in0=in0, scalar1=s1, scalar2=s2, op0=op0, op1=op1), R, W)

        def stt(eng, out, in0, scalar, in1, op0, op1, R, W):
            return P.emit(eng, lambda e: e.scalar_tensor_tensor(out=out, in0=in0, scalar=scalar, in1=in1, op0=op0, op1=op1), R, W)

        def cp(eng, out, in_, R, W):
            if eng == "act":
                return P.emit("act", lambda e: e.copy(out=out, in_=in_), R, W)
            return P.emit(eng, lambda e: e.tensor_copy(out=out, in_=in_), R, W)

        def act(out, in_, func, R, W, bias=None, scale=1.0, accum=None):
            kw = {}
            if bias is not None:
                kw["bias"] = bias
            if accum is not None:
                kw["accum_out"] = accum
            return P.emit("act", lambda e: e.activation(out=out, in_=in_, func=func, scale=scale, **kw), R, W)

        def mm(out, lhsT, rhs, start, stop, R, W):
            return P.emit("pe", lambda e: e.matmul(out, lhsT=lhsT, rhs=rhs, start=start, stop=stop), R, W)

        def tr(out, in_, R, W):
            return P.emit("pe", lambda e: e.transpose(out=out, in_=in_, identity=identb[:]), list(R) + ["identb"], W)

        def mset(eng, ap, val, W):
            return P.emit(eng, lambda e: e.memset(ap, val), (), W)

        def recip(out, in_, R, W):
            return P.emit("dve", lambda e: e.reciprocal(out=out, in_=in_), R, W)

        def dma(eng, out, in_, R, W, sem):
            return P.emit(eng, lambda e: e.dma_start(out=out, in_=in_), R, W, dma_sem=sem)

        class Rot:
            def __init__(self, items):
                self.items = items
                self.i = 0

            def next(self):
                it = self.items[self.i % len(self.items)]
                self.i += 1
                return it

        mmR = Rot([(psum("mm%d" % i, [128, 512], F32), "mm%d" % i) for i in range(2)])
        tpR = Rot([(psum("tp%d" % i, [128, 8, 128], BF16), "tp%d" % i) for i in range(2)])
        scR = Rot([(psum("sc%d" % i, [128, 512], F32), "sc%d" % i) for i in range(2)])
        aoR = Rot([(psum("ao%d" % i, [128, 512], F32), "ao%d" % i) for i in range(2)])
        denseR = Rot(mmR.items + scR.items + aoR.items)
        projR = Rot(mmR.items + scR.items)

        cst = sb("cst", [128, 6, 128], F32)
        identb = sb("identb", [128, 128], BF16)
        m01b = sb("m01b", [128, 128], BF16)
        chunkind = sb("chunkind", [128, 2, 64], F32)
        bm = sb("bm", [128, 2, 8, 128], F32)
        swam = sb("swam", [128, 2, 128], F32)
        gT = sb("gT", [128, 32], F32)
        rowp = sb("rowp", [128, DEPTH * ROWP], F32)
        convw = sb("convw", [128, 12], F32)
        expsink = sb("expsink", [128, DEPTH, 8], F32)
        tri, triblk, cmaskT, ones = cst[:, 1, :], cst[:, 2, :], cst[:, 3, :], cst[:, 4, :]

        dma("sp", cst[:], cst_d, (), ["cst"], "c0")
        dma("sp", bm[:], swab_d, (), ["bm"], "c1")
        dma("sp", swam[:], swam_d, (), ["swam"], "c2")
        dma("sp", gT[:], gT_d, (), ["gT"], "c3")
        dma("sp", rowp[:], rowp_d.partition_broadcast(128), (), ["rowp"], "c4")
        dma("sp", convw[:], convw_d, (), ["convw"], "c5")
        cp("dve", identb[:], cst[:, 0, :], ["cst"], ["identb"])
        cp("dve", m01b[:], cst[:, 5, :], ["cst"], ["m01b"])
        mset("pool", chunkind[:], 0.0, ["chunkind"])
        mset("pool", chunkind[0:64, 0, :], 1.0, ["chunkind"])
        mset("pool", chunkind[64:128, 1, :], 1.0, ["chunkind"])
        for blk in range(2):
            tt("dve", bm[:, blk, :, :], bm[:, blk, :, :], swam[:, blk, :].unsqueeze(1).to_broadcast([128, 8, 128]),
               ALU.add, ["bm", "swam"], ["bm"])
        for l in range(DEPTH):
            act(expsink[:, l, :], rowp[:, l * ROWP + RP_SNK: l * ROWP + RP_SNK + 8], AF.Exp, ["rowp"], ["expsink"])

        NSLOT = 4
        CAST_AHEAD = 8
        wslots = [sb("wslot%d" % i, [128, 8, 512], BF16) for i in range(NSLOT)]

        def layer_chunks(l):
            ch = []

            def add(name, r0, nrows, c0, ncol):
                ch.append((name, l, r0, nrows, c0, ncol, nrows // 128))

            def win(c0, ncol):
                add("w_in", 0, D, c0, ncol)

            win(C_FOX, 512)
            win(C_FOX + 512, 260)
            win(C_SWA + 512, 256)
            win(C_SWA, 512)
            win(C_ML + 512, 264)
            win(C_ML + 776, 256)
            win(C_ML, 512)
            win(C_CONV, 512)
            win(C_CONV + 512, 256)
            for b_, (nm, rows) in enumerate([("w_fox_out", 256), ("w_conv_out", 256), ("w_mlstm_out", 256), ("w_swa_out", 512)]):
                add(nm, 0, rows, 0, 1024)
                win(C_GATE + b_ * 1024, 512)
                win(C_GATE + b_ * 1024 + 512, 512)
            for h in range(2):
                add("w_merge_out", 0, D, h * 512, 512)
            for c in range(6):
                ncol = min(512, DFF - c * 512)
                add("w_gate", 0, D, c * 512, ncol)
                add("w_up", 0, D, c * 512, ncol)
            for h in range(2):
                for c in range(3):
                    k0 = c * 8
                    nk = min(8, 22 - k0)
                    add("w_down", k0 * 128, nk * 128, h * 512, 512)
            return ch

        first_pass = []
        for l in range(n_layers):
            first_pass.extend(layer_chunks(l))
        NFP = len(first_pass)
        n_pass = n_seq * n_tiles

        class WStream:
            def __init__(self):
                self.issued = 0
                self.taken = 0
                self.holds = {}
                self.cast_next = 0
                self.total = NFP * n_pass

            def _cast_upto(self, k_end):
                while self.cast_next < min(k_end, NFP):
                    k = self.cast_next
                    name, l, r0, nr, c0, nc_, nk = first_pass[k]
                    if do_cast:
                        dma("pool", wb[name][l, r0:r0 + nr, c0:c0 + nc_], wf[name][l, r0:r0 + nr, c0:c0 + nc_], (), [("wbc", k)], "cast%d" % k)
                    self.cast_next += 1

            def _view(self, n):
                name, l, r0, nr, c0, ncol, nk = first_pass[n % NFP]
                slot = n % NSLOT
                return wslots[slot][:].rearrange("p k c -> p (k c)")[:, 0:nk * ncol].rearrange("p (k c) -> p k c", c=ncol)

            def _issue(self, n):
                k = n % NFP
                name, l, r0, nr, c0, ncol, nk = first_pass[k]
                self._cast_upto(k + 1 + (CAST_AHEAD if n < NFP else 0))
                src = wb[name][l, r0:r0 + nr, c0:c0 + ncol].rearrange("(k p) c -> p k c", p=128)
                dma("sp", self._view(n), src, [("wbc", k)] if do_cast else [], [("ws", n % NSLOT)], "ws%d" % (n % NSLOT))

            def next(self, expect, hold=0):
                m = self.taken
                while self.issued < self.total and self.issued < m + NSLOT:
                    k = self.issued
                    prev = k - NSLOT
                    if prev >= 0 and not (prev + 1 + self.holds[prev] <= m):
                        break
                    self._issue(k)
                    self.issued += 1
                n = self.taken
                assert self.issued > n
                self.holds[n] = hold
                self.taken += 1
                assert first_pass[n % NFP][0] == expect, (first_pass[n % NFP][0], expect)
                return self._view(n), ("ws", n % NSLOT)

        WS = WStream()

        xt = sb("xt", [128, NSUB, D], F32)
        hT = sb("hT", [128, 8, T], BF16)
        hb = sb("hb", [128, D], BF16)
        junk = sb("junk", [128, D], BF16)
        small = sb("small", [128, 64], F32)
        stages = [sb("stage%d" % i, [128, 512], F32) for i in range(3)]
        stR = Rot([(stages[i], "stage%d" % i) for i in range(3)])
        sqtmp = sb("sqtmp", [128, 512], F32)
        nrm_ss = sb("nrm_ss", [128, 8], F32)
        qkn = sb("qkn", [128, 512], BF16)
        fqT = sb("fqT", [128, 2, T], BF16)
        fkT = [sb("fkT%d" % l, [128, 2, seq_len], BF16) for l in range(n_layers)]
        fV = [sb("fV%d" % l, [128, seq_len // 128, 4, 65], BF16) for l in range(n_layers)]
        fnegc = [sb("fnegc%d" % l, [128, seq_len // 128, 4], F32) for l in range(n_layers)]
        fncar = [sb("fncar%d" % l, [128, NSUB + 1, 4], F32) for l in range(n_layers)]
        fsp = sb("fsp", [128, NSUB, 4], F32)
        fxf = sb("fxf", [128, 4], F32)
        fD = sb("fD", [128, 128], F32)
        fcbc = [sb("fcbc%d" % i, [128, T], F32) for i in range(2)]
        atmp = [sb("atmp%d" % i, [128, 512], F32) for i in range(2)]
        apT = [sb("apT%d" % i, [128, 512], BF16) for i in range(2)]
        frec = sb("frec", [128, 8], F32)
        yfox = sb("yfox", [128, NSUB, 256], BF16)
        sqT = [sb("sqT%d" % i, [64, 8, 128], BF16) for i in range(NSUB)]
        swtmp = [sb("swtmp%d" % i, [128, 512], F32) for i in range(2)]
        swpT = [sb("swpT%d" % i, [128, 512], BF16) for i in range(2)]
        skT = [sb("skT%d" % l, [64, NSUB + 1, 2, 128], BF16) for l in range(n_layers)]
        sV = [sb("sV%d" % l, [128, NSUB + 1, 2, 65], BF16) for l in range(n_layers)]
        skst = sb("skst", [128, 128], F32)
        skn = sb("skn", [128, 128], BF16)
        yswa = [sb("yswa%d" % i, [128, 512], BF16) for i in range(2)]
        sden = sb("sden", [128, 4], F32)
        mV = sb("mV", [128, NSUB, 4, 65], BF16)
        mif = sb("mif", [128, NSUB, 8], F32)
        msp = sb("msp", [128, NSUB, 4], F32)
        mea = sb("mea", [128, NSUB, 4], F32)
        meb = sb("meb", [128, NSUB, 4], F32)
        mdec = sb("mdec", [64, NSUB, 2, 4], F32)
        mso = sb("mso", [128, NSUB, 256], F32)
        mkt = [sb("mkt%d" % i, [128, 256], BF16) for i in range(NSUB)]
        mqb = sb("mqb", [128, 256], BF16)
        mqT = [sb("mqT%d" % i, [64, 4, 128], BF16) for i in range(NSUB)]
        mq0T = [sb("mq0T%d" % i, [64, 4, 128], BF16) for i in range(NSUB)]
        mq1T = [sb("mq1T%d" % i, [64, 4, 128], BF16) for i in range(NSUB)]
        mkT = [sb("mkT%d" % i, [64, 4, 128], BF16) for i in range(NSUB)]
        mPT = sb("mPT", [128, 4, 128], BF16)
        mCf = [sb("mCf%d" % l, [64, 4, 65], F32) for l in range(n_layers)]
        mCb = [[sb("mCb%d_%d" % (l, i), [64, 4, 65], BF16) for i in range(2)] for l in range(n_layers)]
        mt1 = sb("mt1", [128, 8], F32)
        mp2 = sb("mp2", [128, 16], F32)
        mhm = sb("mhm", [128, 256], F32)
        ymls = sb("ymls", [128, 256], BF16)
        cu = sb("cu", [128, 2, T], F32)
        cbt = sb("cbt", [128, 2, T], F32)
        cz = [sb("cz%d" % l, [128, 2, T + 2], F32) for l in range(n_layers)]
        cy = sb("cy", [128, T], F32)
        yT = sb("yT", [128, 10, T], BF16)
        sg = [sb("sg%d" % i, [128, 512], F32) for i in range(2)]
        mtmp = sb("mtmp", [128, 512], F32)
        merged = sb("merged", [128, NSUB, D], F32)
        actT = sb("actT", [128, 22, T], BF16)

        def rp(l, off, n):
            return rowp[:, l * ROWP + off: l * ROWP + off + n]

        for a in range(NSUB):
            mset("pool", mq0T[a][:], 0.0, [("mq0T", a)])
            mset("pool", mq1T[a][:], 0.0, [("mq1T", a)])
        mset("pool", mV[:], 1.0, ["mV"])
        for l in range(n_layers):
            mset("pool", fV[l][:], 1.0, [("fV", l)])
            mset("pool", sV[l][:], 1.0, [("sV", l)])

        def head_norm(src, nh, gain_views, out_bf, srcR, outW, out_scale=1.0):
            w = nh * 64
            tt("pool", sqtmp[:, 0:w], src, src, ALU.mult, srcR, ["sqtmp"])
            P.emit("dve", lambda e: e.tensor_reduce(out=nrm_ss[:, 0:nh], in_=sqtmp[:, 0:w].rearrange("p (h d) -> p h d", d=64),
                                                     axis=AX.X, op=ALU.add), ["sqtmp"], ["nrm_ss"])
            act(nrm_ss[:, 0:nh], nrm_ss[:, 0:nh], AF.Sqrt, ["nrm_ss"], ["nrm_ss"], bias=EPS, scale=1.0 / 64)
            recip(nrm_ss[:, 0:nh], nrm_ss[:, 0:nh], ["nrm_ss"], ["nrm_ss"])
            tt("dve", sqtmp[:, 0:w].rearrange("p (h d) -> p h d", d=64), src.rearrange("p (h d) -> p h d", d=64),
               nrm_ss[:, 0:nh].unsqueeze(2).to_broadcast([128, nh, 64]), ALU.mult, list(srcR) + ["nrm_ss"], ["sqtmp"])
            for h0, h1, g in gain_views:
                n = h1 - h0
                tt("pool", out_bf[:, h0 * 64:h1 * 64].rearrange("p (h d) -> p h d", d=64),
                   sqtmp[:, h0 * 64:h1 * 64].rearrange("p (h d) -> p h d", d=64),
                   g.unsqueeze(1).to_broadcast([128, n, 64]), ALU.mult, ["sqtmp", "rowp"], outW)

        def resid_norm(l, which):
            for a in range(NSUB):
                mset("pool", small[:, 0:1], 0.0, ["small0"])
                act(junk[:], xt[:, a, :], AF.Square, [("xt", a), "small0"], ["junk", "small0"], accum=small[:, 0:1])
                act(small[:, 1:2], small[:, 0:1], AF.Sqrt, ["small0"], ["small1"], bias=EPS, scale=1.0 / D)
                recip(small[:, 2:3], small[:, 1:2], ["small1"], ["small2"])
                ts("dve", hb[:], xt[:, a, :], small[:, 2:3], None, ALU.mult, None, [("xt", a), "small2"], ["hb"])
                tp, tpk = tpR.next()
                for kc in range(8):
                    tr(tp[:, kc, :], hb[:, kc * 128:(kc + 1) * 128], ["hb"], [tpk])
                g0 = l * 16 + which * 8
                tt("dve", hT[:, :, a * 128:(a + 1) * 128], tp[:, :, :],
                   gT[:, g0:g0 + 8].unsqueeze(2).to_broadcast([128, 8, 128]), ALU.mult, [tpk, "gT"], [("hT", a)])

        HT_ALL = [("hT", a) for a in range(NSUB)]

        def proj_tok(W, wk, ncol, a, rot=None):
            ps, pk = (rot or projR).next()
            for kc in range(8):
                mm(ps[:, 0:ncol], hT[:, kc, a * 128:(a + 1) * 128], W[:, kc, :], kc == 0, kc == 7, [("hT", a), wk], [pk])
            return ps, pk

        def main_loop():
          for s in range(n_seq):
            for t in range(n_tiles):
                tok0 = s * seq_len + t * T
                blk0 = t * NSUB
                first_tile = (t == 0)
                for a in range(NSUB):
                    dma("sp", xt[:, a, :], x_d[tok0 + a * 128: tok0 + (a + 1) * 128, :], (), [("xt", a)], "xt")
                for l in range(n_layers):
                    if first_tile:
                        mset("pool", fncar[l][:, 0, :], 0.0, [("fncar", l)])
                        mset("pool", mCf[l][:], 0.0, [("mCf", l)])
                        mset("pool", mCb[l][0][:], 0.0, [("mCb", l, 0)])
                        mset("pool", cz[l][:, :, 0:2], 0.0, [("cz", l)])
                    if stop_at <= 0:
                        raise _Stop()
                    P.phase = "norm1"
                    resid_norm(l, 0)

                    if stop_at <= 1:
                        raise _Stop()
                    P.phase = "fox_proj"
                    W, wk = WS.next("w_in")
                    for a in range(NSUB):
                        ps, pk = proj_tok(W, wk, 512, a)
                        stg, sk = stR.next()
                        cp("act", stg[:], ps[:, 0:512], [pk], [sk])
                        if stop_at <= 1.1:
                            raise _Stop()
                        head_norm(stg[:], 8, [(0, 4, rp(l, RP_FQG, 64)), (4, 8, rp(l, RP_FKG, 64))], qkn, [sk], ["qkn"])
                        if stop_at <= 1.2:
                            raise _Stop()
                        tp, tpk = tpR.next()
                        for k in range(4):
                            tr(tp[:, k, :], qkn[:, k * 128:(k + 1) * 128], ["qkn"], [tpk])
                        cp("act", fqT[:, :, a * 128:(a + 1) * 128], tp[:, 0:2, :], [tpk], ["fqT"])
                        cp("act", fkT[l][:, :, (blk0 + a) * 128:(blk0 + a + 1) * 128], tp[:, 2:4, :], [tpk], [("fkT", l)])
                    if stop_at <= 1.3:
                        raise _Stop()
                    W, wk = WS.next("w_in")
                    for a in range(NSUB):
                        ps, pk = proj_tok(W, wk, 260, a)
                        cp("dve", fV[l][:, blk0 + a, :, 0:64], ps[:, 0:256].rearrange("p (h d) -> p h d", d=64), [pk], [("fV", l)])
                        if stop_at <= 1.4:
                            raise _Stop()
                        tt("dve", fxf[:], ps[:, 256:260], rp(l, RP_FFB, 4), ALU.add, [pk, "rowp"], ["fxf"])
                        act(fxf[:], fxf[:], AF.Exp, ["fxf"], ["fxf"], scale=-1.0)
                        act(fsp[:, a, :], fxf[:], AF.Ln, ["fxf"], [("fsp", a)], bias=1.0)
                        if stop_at <= 1.5:
                            raise _Stop()
                        p2, p2k = aoR.next()
                        mm(p2[:, 0:4], tri, fsp[:, a, :], True, True, ["cst", ("fsp", a)], [p2k])
                        mm(p2[:, 4:8], ones, fsp[:, a, :], True, True, ["cst", ("fsp", a)], [p2k])
                        tt("dve", fnegc[l][:, blk0 + a, :], p2[:, 0:4], fncar[l][:, a, :], ALU.add, [p2k, ("fncar", l)], [("fnegc", l)])
                        tt("dve", fncar[l][:, a + 1, :], p2[:, 4:8], fncar[l][:, a, :], ALU.add, [p2k, ("fncar", l)], [("fncar", l)])

                    if stop_at <= 2:
                        raise _Stop()
                    P.phase = "swa_proj"
                    W, wk = WS.next("w_in")
                    for a in range(NSUB):
                        ps, pk = proj_tok(W, wk, 256, a)
                        cp("act", skst[:], ps[:, 0:128], [pk], ["skst"])
                        cp("act", sV[l][:, a + 1, :, 0:64], ps[:, 128:256].rearrange("p (h d) -> p h d", d=64), [pk], [("sV", l)])
                        head_norm(skst[:], 2, [(0, 2, rp(l, RP_SKG, 64))], skn, ["skst"], ["skn"])
                        tp, tpk = tpR.next()
                        for g in range(2):
                            tr(tp[0:64, g, :], skn[:, g * 64:(g + 1) * 64], ["skn"], [tpk])
                        cp("dve", skT[l][:, a + 1, :, :], tp[0:64, 0:2, :], [tpk], [("skT", l)])
                    W, wk = WS.next("w_in")
                    for a in range(NSUB):
                        ps, pk = proj_tok(W, wk, 512, a)
                        stg, sk = stR.next()
                        cp("act", stg[:], ps[:, 0:512], [pk], [sk])
                        head_norm(stg[:], 8, [(0, 8, rp(l, RP_SQG, 64))], qkn, [sk], ["qkn"])
                        qb_, qbk = sqT[a], "sqT%d" % a
                        for half in range(2):
                            tp, tpk = tpR.next()
                            for k in range(4):
                                hh = half * 4 + k
                                tr(tp[0:64, k, :], qkn[:, hh * 64:(hh + 1) * 64], ["qkn"], [tpk])
                            cp("act" if half else "dve", qb_[:, half * 4:half * 4 + 4, :], tp[0:64, 0:4, :], [tpk], [qbk])

                    P.phase = "mlstm_proj"
                    W, wk = WS.next("w_in")
                    for a in range(NSUB):
                        ps, pk = proj_tok(W, wk, 264, a)
                        cp("dve", mV[:, a, :, 0:64], ps[:, 0:256].rearrange("p (h d) -> p h d", d=64), [pk], ["mV"])
                        tt("dve", mif[:, a, 0:4], ps[:, 256:260], rp(l, RP_MIB, 4), ALU.add, [pk, "rowp"], [("mif", a)])
                        tt("dve", mif[:, a, 4:8], ps[:, 260:264], rp(l, RP_MFB, 4), ALU.add, [pk, "rowp"], [("mif", a)])
                        act(mt1[:, 0:4], mif[:, a, 4:8], AF.Exp, [("mif", a)], ["mt1"], scale=-1.0)
                        act(msp[:, a, :], mt1[:, 0:4], AF.Ln, ["mt1"], [("msp", a)], bias=1.0)
                        p2, p2k = aoR.next()
                        mm(p2[:, 0:4], triblk, msp[:, a, :], True, True, ["cst", ("msp", a)], [p2k])
                        for c in range(2):
                            mm(p2[0:64, 8 + 4 * c:12 + 4 * c], chunkind[:, c, :], msp[:, a, :], True, True, ["chunkind", ("msp", a)], [p2k])
                        cp("dve", mp2[:, 0:4], p2[:, 0:4], [p2k], ["mp2"])
                        cp("dve", mp2[0:64, 8:16], p2[0:64, 8:16], [p2k], ["mp2"])
                        tt("dve", mt1[:, 4:8], mp2[:, 0:4], mif[:, a, 0:4], ALU.add, ["mp2", ("mif", a)], ["mt1"])
                        act(mea[:, a, :], mt1[:, 4:8], AF.Exp, ["mt1"], [("mea", a)])
                        act(meb[:, a, :], mp2[:, 0:4], AF.Exp, ["mp2"], [("meb", a)], scale=-1.0)
                        act(mdec[:, a, :, :], mp2[0:64, 8:16].rearrange("p (c h) -> p c h", h=4), AF.Exp, ["mp2"], [("mdec", a)], scale=-1.0)
                    W, wk = WS.next("w_in")
                    for a in range(NSUB):
                        ps, pk = proj_tok(W, wk, 256, a)
                        act(mso[:, a, :], ps[:, 0:256], AF.Sigmoid, [pk], [("mso", a)])
                    W, wk = WS.next("w_in")
                    for a in range(NSUB):
                        ps, pk = proj_tok(W, wk, 512, a)
                        ts("dve", mqb[:], ps[:, 0:256], 0.125, None, ALU.mult, None, [pk], ["mqb"])
                        tt("dve", mkt[a][:].rearrange("p (h d) -> p h d", d=64), ps[:, 256:512].rearrange("p (h d) -> p h d", d=64),
                           mea[:, a, :].unsqueeze(2).to_broadcast([128, 4, 64]), ALU.mult, [pk, ("mea", a)], [("mkt", a)])
                        tq, tqk = tpR.next()
                        for h in range(4):
                            tr(tq[0:64, h, :], mqb[:, h * 64:(h + 1) * 64], ["mqb"], [tqk])
                        tk, tkk = tpR.next()
                        for h in range(4):
                            tr(tk[0:64, h, :], mkt[a][:, h * 64:(h + 1) * 64], [("mkt", a)], [tkk])
                        cp("dve", mqT[a][:], tq[0:64, 0:4, :], [tqk], [("mqT", a)])
                        cp("dve", mq0T[a][:, :, 0:64], tq[0:64, 0:4, 0:64], [tqk], [("mq0T", a)])
                        cp("dve", mq1T[a][:, :, 64:128], tq[0:64, 0:4, 64:128], [tqk], [("mq1T", a)])
                        cp("act", mkT[a][:], tk[0:64, 0:4, :], [tkk], [("mkT", a)])

                    if stop_at <= 5:
                        raise _Stop()
                    P.phase = "conv"
                    W, wk = WS.next("w_in")
                    for cb_ in range(4):
                        ps, pk = projR.next()
                        for kc in range(8):
                            mm(ps[:, 0:T], W[:, kc, cb_ * 128:(cb_ + 1) * 128], hT[:, kc, :], kc == 0, kc == 7, HT_ALL + [wk], [pk])
                        dst = cu if cb_ < 2 else cbt
                        cp("act", dst[:, cb_ % 2, :], ps[:, 0:T], [pk], ["cu" if cb_ < 2 else "cbt"])
                    W, wk = WS.next("w_in")
                    for cc in range(2):
                        ps, pk = projR.next()
                        for kc in range(8):
                            mm(ps[:, 0:T], W[:, kc, cc * 128:(cc + 1) * 128], hT[:, kc, :], kc == 0, kc == 7, HT_ALL + [wk], [pk])
                        z = cz[l]
                        tt("dve", z[:, cc, 2:T + 2], ps[:, 0:T], cu[:, cc, :], ALU.mult, [pk, "cu"], [("cz", l)])
                        w0 = convw[:, l * 6 + cc * 3 + 0: l * 6 + cc * 3 + 1]
                        w1 = convw[:, l * 6 + cc * 3 + 1: l * 6 + cc * 3 + 2]
                        w2 = convw[:, l * 6 + cc * 3 + 2: l * 6 + cc * 3 + 3]
                        ts("pool", cy[:], z[:, cc, 2:T + 2], w2, None, ALU.mult, None, [("cz", l), "convw"], ["cy"])
                        stt("dve", cy[:], z[:, cc, 1:T + 1], w1, cy[:], ALU.mult, ALU.add, [("cz", l), "convw", "cy"], ["cy"])
                        stt("dve", cy[:], z[:, cc, 0:T], w0, cy[:], ALU.mult, ALU.add, [("cz", l), "convw", "cy"], ["cy"])
                        tt("dve", yT[:, 2 + cc, :], cy[:], cbt[:, cc, :], ALU.mult, ["cy", "cbt"], [("yT", 1)])
                        cp("pool", z[:, cc, 0:2], z[:, cc, T:T + 2], [("cz", l)], [("cz", l)])


                    nblk = blk0 + NSUB

                    def gen_fox():
                        for h in range(4):
                            P.phase = "fox_attn"
                            hp, hbase = h // 2, (h % 2) * 64
                            cb = fcbc[h % 2]
                            cbk = "fcbc%d" % (h % 2)
                            pc, pck = scR.next()
                            for a in range(NSUB):
                                ts("dve", fD[:], ones, fsp[:, a, h:h + 1], None, ALU.mult, None, ["cst", ("fsp", a)], ["fD"])
                                mm(pc[:, a * 128:(a + 1) * 128], fD[:], tri, True, True, ["fD", "cst"], [pck])
                            for a in range(NSUB):
                                act(cb[:, a * 128:(a + 1) * 128], pc[:, a * 128:(a + 1) * 128], AF.Identity, [pck, ("fncar", l)], [cbk],
                                    bias=fncar[l][:, a, h:h + 1])
                            acc, acck = mmR.next()
                            accv = acc[:, 0:NSUB * 65].rearrange("p (a c) -> p a c", c=65)
                            yield
                            for j in range(nblk):
                                P.phase = "fox_attn"
                                a0 = max(0, j - blk0)
                                c0 = a0 * 128
                                sc, sck = scR.next()
                                mm(sc[:, c0:T], fkT[l][hbase:hbase + 64, hp, j * 128:(j + 1) * 128], fqT[hbase:hbase + 64, hp, c0:T],
                                   True, True, [("fkT", l), "fqT"], [sck])
                                bi = j % 2
                                tmp, tmpk, pT, pTk = atmp[bi], "atmp%d" % bi, apT[bi], "apT%d" % bi
                                stt("dve", tmp[:, c0:T], sc[:, c0:T], 0.125, cb[:, c0:T], ALU.mult, ALU.subtract, [sck, cbk], [tmpk])
                                if j >= blk0:
                                    tt("pool", tmp[:, c0:c0 + 128], tmp[:, c0:c0 + 128], cmaskT, ALU.add, [tmpk, "cst"], [tmpk])
                                act(pT[:, c0:T], tmp[:, c0:T], AF.Exp, [tmpk, ("fnegc", l)], [pTk], bias=fnegc[l][:, j, h:h + 1])
                                for a in range(a0, NSUB):
                                    mm(accv[:, a, :], pT[:, a * 128:(a + 1) * 128], fV[l][:, j, h, :], (j == 0 and a == 0),
                                       (j == nblk - 1 and a == NSUB - 1), [pTk, ("fV", l)], [acck])
                                yield
                            P.phase = "fox_attn"
                            recip(frec[:, 0:NSUB], accv[:, :, 64], [acck], ["frec"])
                            tt("dve", yfox[:, :, h * 64:(h + 1) * 64], accv[:, :, 0:64],
                               frec[:, 0:NSUB].unsqueeze(2).to_broadcast([128, NSUB, 64]), ALU.mult, [acck, "frec"], ["yfox"])
                            yield
                        P.phase = "fox_attn"
                        cp("pool", fncar[l][:, 0, :], fncar[l][:, NSUB, :], [("fncar", l)], [("fncar", l)])
                        for a in range(NSUB):
                            tp, tpk = tpR.next()
                            for k in range(2):
                                tr(tp[:, k, :], yfox[:, a, k * 128:(k + 1) * 128], ["yfox"], [tpk])
                            cp("act", yT[:, 0:2, a * 128:(a + 1) * 128], tp[:, 0:2, :], [tpk], [("yT", 0)])

                    def gen_swa():
                        for a in range(NSUB):
                            qb_, qbk = sqT[a], "sqT%d" % a
                            has_prev = not (first_tile and a == 0)
                            yb, ybk = yswa[a % 2], "yswa%d" % (a % 2)
                            for g in range(2):
                                P.phase = "swa"
                                pTs = []
                                for which in ([0, 1] if has_prev else [1]):
                                    blk = a + which
                                    sc, sck = scR.next()
                                    mm(sc[:], skT[l][:, blk, g, :], qb_[:, 4 * g:4 * g + 4, :].rearrange("p h t -> p (h t)"),
                                       True, True, [("skT", l), qbk], [sck])
                                    tmp, tmpk, pT, pTk = swtmp[which], "swtmp%d" % which, swpT[which], "swpT%d" % which
                                    stt("dve", tmp[:], sc[:], 0.125, bm[:, which, 4 * g:4 * g + 4, :].rearrange("p h t -> p (h t)"),
                                        ALU.mult, ALU.add, [sck, "bm"], [tmpk])
                                    act(pT[:], tmp[:], AF.Exp, [tmpk], [pTk])
                                    pTs.append((pT, pTk, blk))
                                ao, aok = aoR.next()
                                aov = ao[:, 0:260].rearrange("p (h c) -> p h c", c=65)
                                for hh in range(4):
                                    for i, (pT, pTk, blk) in enumerate(pTs):
                                        mm(aov[:, hh, :], pT[:, hh * 128:(hh + 1) * 128], sV[l][:, blk, g, :], i == 0, i == len(pTs) - 1,
                                           [pTk, ("sV", l)], [aok])
                                tt("dve", sden[:], aov[:, :, 64], expsink[:, l, 4 * g:4 * g + 4], ALU.add, [aok, "expsink"], ["sden"])
                                recip(sden[:], sden[:], ["sden"], ["sden"])
                                tt("dve", yb[:, g * 256:(g + 1) * 256].rearrange("p (h d) -> p h d", d=64), aov[:, :, 0:64],
                                   sden[:].unsqueeze(2).to_broadcast([128, 4, 64]), ALU.mult, [aok, "sden"], [ybk])
                                yield
                            P.phase = "swa"
                            tp, tpk = tpR.next()
                            for k in range(4):
                                tr(tp[:, k, :], yb[:, k * 128:(k + 1) * 128], [ybk], [tpk])
                            cp("act", yT[:, 6:10, a * 128:(a + 1) * 128], tp[:, 0:4, :], [tpk], [("yT", 3)])
                            yield
                        P.phase = "swa"
                        cp("pool", skT[l][:, 0, :, :], skT[l][:, NSUB, :, :], [("skT", l)], [("skT", l)])
                        cp("pool", sV[l][:, 0, :, :], sV[l][:, NSUB, :, :], [("sV", l)], [("sV", l)])

                    def gen_mlstm():
                        for a in range(NSUB):
                            P.phase = "mlstm"
                            sc, sck = scR.next()
                            scv = sc[:].rearrange("p (h t) -> p h t", t=128)
                            for h in range(4):
                                mm(scv[:, h, :], mkT[a][:, h, :], mqT[a][:, h, :], True, True, [("mkT", a), ("mqT", a)], [sck])
                            tt("dve", mPT[:], scv, m01b[:].unsqueeze(1).to_broadcast([128, 4, 128]), ALU.mult, [sck, "m01b"], ["mPT"])
                            yield
                            P.phase = "mlstm"
                            pd, pdk = aoR.next()
                            pdv = pd[0:64, 0:260].rearrange("p (h c) -> p h c", c=65)
                            for h in range(4):
                                mm(pdv[:, h, :], mkt[a][0:64, h * 64:(h + 1) * 64], mV[0:64, a, h, :], True, True, [("mkt", a), "mV"], [pdk])
                            tt("dve", mCf[l][:], mCf[l][:], pdv, ALU.add, [("mCf", l), pdk], [("mCf", l)])
                            tt("dve", mCf[l][:], mCf[l][:], mdec[:, a, 0, :].unsqueeze(2).to_broadcast([64, 4, 65]), ALU.mult,
                               [("mCf", l), ("mdec", a)], [("mCf", l)])
                            cp("pool", mCb[l][1][:], mCf[l][:], [("mCf", l)], [("mCb", l, 1)])
                            yield
                            P.phase = "mlstm"
                            ao, aok = aoR.next()
                            aov = ao[:, 0:260].rearrange("p (h c) -> p h c", c=65)
                            for h in range(4):
                                mm(aov[:, h, :], mPT[:, h, :], mV[:, a, h, :], True, False, ["mPT", "mV"], [aok])
                                mm(aov[:, h, :], mq0T[a][:, h, :], mCb[l][0][:, h, :], False, False, [("mq0T", a), ("mCb", l, 0)], [aok])
                                mm(aov[:, h, :], mq1T[a][:, h, :], mCb[l][1][:, h, :], False, True, [("mq1T", a), ("mCb", l, 1)], [aok])
                            tt("dve", mt1[:, 0:4], aov[:, :, 64], meb[:, a, :], ALU.mult, [aok, ("meb", a)], ["mt1"])
                            stt("dve", mt1[:, 0:4], mt1[:, 0:4], -1.0, mt1[:, 0:4], ALU.mult, ALU.max, ["mt1"], ["mt1"])
                            P.emit("dve", lambda e: e.tensor_scalar_max(out=mt1[:, 0:4], in0=mt1[:, 0:4], scalar1=1.0), ["mt1"], ["mt1"])
                            recip(mt1[:, 0:4], mt1[:, 0:4], ["mt1"], ["mt1"])
                            tt("dve", mt1[:, 0:4], mt1[:, 0:4], meb[:, a, :], ALU.mult, ["mt1", ("meb", a)], ["mt1"])
                            tt("dve", mhm[:].rearrange("p (h d) -> p h d", d=64), aov[:, :, 0:64],
                               mt1[:, 0:4].unsqueeze(2).to_broadcast([128, 4, 64]), ALU.mult, [aok, "mt1"], ["mhm"])
                            yield
                            P.phase = "mlstm"
                            pd, pdk = aoR.next()
                            pdv = pd[0:64, 0:260].rearrange("p (h c) -> p h c", c=65)
                            for h in range(4):
                                mm(pdv[:, h, :], mkt[a][64:128, h * 64:(h + 1) * 64], mV[64:128, a, h, :], True, True, [("mkt", a), "mV"], [pdk])
                            tt("dve", mCf[l][:], mCf[l][:], pdv, ALU.add, [("mCf", l), pdk], [("mCf", l)])
                            tt("dve", mCf[l][:], mCf[l][:], mdec[:, a, 1, :].unsqueeze(2).to_broadcast([64, 4, 65]), ALU.mult,
                               [("mCf", l), ("mdec", a)], [("mCf", l)])
                            cp("pool", mCb[l][0][:], mCf[l][:], [("mCf", l)], [("mCb", l, 0)])
                            yield
                            P.phase = "mlstm"
                            tt("pool", sqtmp[:, 0:256], mhm[:], mhm[:], ALU.mult, ["mhm"], ["sqtmp"])
                            P.emit("dve", lambda e: e.tensor_reduce(out=nrm_ss[:, 0:4], in_=sqtmp[:, 0:256].rearrange("p (h d) -> p h d", d=64),
                                                                     axis=AX.X, op=ALU.add), ["sqtmp"], ["nrm_ss"])
                            act(nrm_ss[:, 0:4], nrm_ss[:, 0:4], AF.Sqrt, ["nrm_ss"], ["nrm_ss"], bias=EPS, scale=1.0 / 64)
                            recip(nrm_ss[:, 0:4], nrm_ss[:, 0:4], ["nrm_ss"], ["nrm_ss"])
                            tt("dve", mhm[:].rearrange("p (h d) -> p h d", d=64), mhm[:].rearrange("p (h d) -> p h d", d=64),
                               nrm_ss[:, 0:4].unsqueeze(2).to_broadcast([128, 4, 64]), ALU.mult, ["mhm", "nrm_ss"], ["mhm"])
                            tt("pool", mhm[:], mhm[:], rp(l, RP_MHG, 256), ALU.mult, ["mhm", "rowp"], ["mhm"])
                            tt("dve", ymls[:], mhm[:], mso[:, a, :], ALU.mult, ["mhm", ("mso", a)], ["ymls"])
                            tp, tpk = tpR.next()
                            for k in range(2):
                                tr(tp[:, k, :], ymls[:, k * 128:(k + 1) * 128], ["ymls"], [tpk])
                            cp("act", yT[:, 4:6, a * 128:(a + 1) * 128], tp[:, 0:2, :], [tpk], [("yT", 2)])
                            yield

                    gens = [[gen_fox(), max(1, (4 * (nblk + 2)) // (5 * NSUB))], [gen_swa(), 1], [gen_mlstm(), 1]]
                    while gens:
                        for ge in list(gens):
                            for _ in range(ge[1]):
                                try:
                                    next(ge[0])
                                except StopIteration:
                                    gens.remove(ge)
                                    break

                    if stop_at <= 6:
                        raise _Stop()
                    P.phase = "merge"
                    ych0 = [0, 2, 4, 6]
                    for b, nm in enumerate(["w_fox_out", "w_conv_out", "w_mlstm_out", "w_swa_out"]):
                        Wb, wbk = WS.next(nm, hold=2)
                        nkc = 4 if b == 3 else 2
                        for half in range(2):
                            Wg, wgk = WS.next("w_in")
                            for a in range(NSUB):
                                pg, pgk = proj_tok(Wg, wgk, 512, a, denseR)
                                s_, s_k = sg[a % 2], "sg%d" % (a % 2)
                                act(s_[:], pg[:, 0:512], AF.Sigmoid, [pgk], [s_k])
                                po, pok = denseR.next()
                                for kc in range(nkc):
                                    mm(po[:, 0:512], yT[:, ych0[b] + kc, a * 128:(a + 1) * 128], Wb[:, kc, half * 512:(half + 1) * 512],
                                       kc == 0, kc == nkc - 1, [("yT", b), wbk], [pok])
                                mdst = merged[:, a, half * 512:(half + 1) * 512]
                                if b == 0:
                                    tt("dve", mdst, po[:, 0:512], s_[:], ALU.mult, [pok, s_k], [("merged", a)])
                                else:
                                    tt("dve", mtmp[:], po[:, 0:512], s_[:], ALU.mult, [pok, s_k], ["mtmp"])
                                    tt("pool", mdst, mdst, mtmp[:], ALU.add, [("merged", a), "mtmp"], [("merged", a)])
                    for a in range(NSUB):
                        cp("act", hb[:], merged[:, a, :], [("merged", a)], ["hb"])
                        tp, tpk = tpR.next()
                        for kc in range(8):
                            tr(tp[:, kc, :], hb[:, kc * 128:(kc + 1) * 128], ["hb"], [tpk])
                        cp("act" if a % 2 else "dve", hT[:, :, a * 128:(a + 1) * 128], tp[:, :, :], [tpk], [("hT", a)])
                    P.phase = "merge_out"
                    for half in range(2):
                        Wm, wmk = WS.next("w_merge_out")
                        for a in range(NSUB):
                            ps, pk = proj_tok(Wm, wmk, 512, a, denseR)
                            xs = xt[:, a, half * 512:(half + 1) * 512]
                            tt("dve", xs, ps[:, 0:512], xs, ALU.add, [pk, ("xt", a)], [("xt", a)])

                    if stop_at <= 7:
                        raise _Stop()
                    P.phase = "ffn"
                    resid_norm(l, 1)
                    for c in range(6):
                        ncol = min(512, DFF - c * 512)
                        Wg, wgk = WS.next("w_gate", hold=1)
                        Wu, wuk = WS.next("w_up")
                        for cb_ in range(ncol // 128):
                            ffc = c * 4 + cb_
                            pg, pgk = denseR.next()
                            for kc in range(8):
                                mm(pg[:, 0:T], Wg[:, kc, cb_ * 128:(cb_ + 1) * 128], hT[:, kc, :], kc == 0, kc == 7, HT_ALL + [wgk], [pgk])
                            pu, puk = denseR.next()
                            for kc in range(8):
                                mm(pu[:, 0:T], Wu[:, kc, cb_ * 128:(cb_ + 1) * 128], hT[:, kc, :], kc == 0, kc == 7, HT_ALL + [wuk], [puk])
                            s_, s_k = sg[ffc % 2], "sg%d" % (ffc % 2)
                            act(s_[:, 0:T], pg[:, 0:T], AF.Silu, [pgk], [s_k])
                            tt("dve", actT[:, ffc, :], pu[:, 0:T], s_[:, 0:T], ALU.mult, [puk, s_k], [("actT", ffc)])
                    P.phase = "ffn_down"
                    ACT_ALL = [("actT", f) for f in range(22)]
                    for half in range(2):
                        accs = []
                        for a in range(NSUB):
                            accs.append(denseR.next())
                        for c in range(3):
                            Wd, wdk = WS.next("w_down")
                            k0 = c * 8
                            nk = min(8, 22 - k0)
                            for a in range(NSUB):
                                ps, pk = accs[a]
                                for k in range(nk):
                                    mm(ps[:, 0:512], actT[:, k0 + k, a * 128:(a + 1) * 128], Wd[:, k, :], (k0 + k) == 0, (k0 + k) == 21,
                                       ACT_ALL + [wdk], [pk])
                        for a in range(NSUB):
                            ps, pk = accs[a]
                            xs = xt[:, a, half * 512:(half + 1) * 512]
                            tt("dve", xs, ps[:, 0:512], xs, ALU.add, [pk, ("xt", a)], [("xt", a)])
                for a in range(NSUB):
                    dma("sp", out_d[tok0 + a * 128: tok0 + (a + 1) * 128, :], xt[:, a, :], [("xt", a)], ["out"], "out")
        try:
            main_loop()
        except _Stop:
            for a in range(NSUB):
                dma("sp", out_d[a * 128:(a + 1) * 128, :], xt[:, a, :], [("xt", a)], ["out"], "out")
        P.emit("sp", lambda e: e.nop(), ["out"], ())
        P.finalize(nc)
    return nc


def _t5_bucket(n):
    max_exact = 16
    nf = np.maximum(n, 1).astype(np.float32)
    large = max_exact + (np.log(nf / max_exact) / math.log(128 / max_exact) * (32 - max_exact)).astype(np.int32)
    large = np.minimum(large, 31)
    return np.where(n < max_exact, n, large)


def host_consts(inp):
    f32 = np.float32
    s = np.arange(128)[:, None]
    t = np.arange(128)[None, :]
    cst = np.zeros((128, 6, 128), f32)
    cst[:, 0] = np.eye(128, dtype=f32)
    cst[:, 1] = (s <= t)
    cst[:, 2] = (s <= t) & (s // 64 == t // 64)
    cst[:, 3] = np.where(s <= t, 0.0, NEG)
    cst[:, 4] = 1.0
    cst[:, 5] = cst[:, 2]
    j = np.arange(128)[:, None]
    i = np.arange(128)[None, :]
    swam = np.zeros((128, 2, 128), f32)
    d_prev = i + 128 - j
    d_cur = i - j
    swam[:, 0] = np.where((d_prev >= 0) & (d_prev < 128), 0.0, NEG)
    swam[:, 1] = np.where((d_cur >= 0) & (d_cur < 128), 0.0, NEG)
    rel = np.asarray(inp["rel_bias"], f32)
    swab = np.zeros((128, 2, 8, 128), f32)
    swab[:, 0] = rel[_t5_bucket(np.maximum(d_prev, 0))].transpose(0, 2, 1)
    swab[:, 1] = rel[_t5_bucket(np.maximum(d_cur, 0))].transpose(0, 2, 1)
    gT = np.zeros((128, 32), f32)
    for l in range(DEPTH):
        gT[:, l * 16:l * 16 + 8] = np.asarray(inp["attn_norm"][l], f32).reshape(8, 128).T
        gT[:, l * 16 + 8:l * 16 + 16] = np.asarray(inp["ffn_norm"][l], f32).reshape(8, 128).T
    rowp = np.zeros((1, DEPTH * ROWP), f32)
    for l in range(DEPTH):
        o = l * ROWP
        rowp[0, o + RP_FQG:o + RP_FQG + 64] = inp["fox_q_gain"][l]
        rowp[0, o + RP_FKG:o + RP_FKG + 64] = inp["fox_k_gain"][l]
        rowp[0, o + RP_SQG:o + RP_SQG + 64] = inp["swa_q_gain"][l]
        rowp[0, o + RP_SKG:o + RP_SKG + 64] = inp["swa_k_gain"][l]
        rowp[0, o + RP_MHG:o + RP_MHG + 256] = inp["mlstm_h_gain"][l]
        rowp[0, o + RP_FFB:o + RP_FFB + 4] = inp["fox_f_bias"][l]
        rowp[0, o + RP_MIB:o + RP_MIB + 4] = inp["mlstm_i_bias"][l]
        rowp[0, o + RP_MFB:o + RP_MFB + 4] = inp["mlstm_f_bias"][l]
        rowp[0, o + RP_SNK:o + RP_SNK + 8] = inp["swa_sinks"][l]
    convw = np.zeros((128, 12), f32)
    cw = np.asarray(inp["conv_w"], f32)
    for l in range(DEPTH):
        for cc in range(2):
            for k in range(3):
                convw[:, l * 6 + cc * 3 + k] = cw[l, k, cc * 128:(cc + 1) * 128]
    return {"cst": cst, "swab": swab, "swam": swam, "gT": gT, "rowp": rowp, "convw": convw}


_NC_CACHE = {}


def kernel(**inputs):
    inp = {k: np.asarray(v) for k, v in inputs.items()}
    x = np.ascontiguousarray(inp["x"], dtype=np.float32)
    n_cores = 8
    key = "full"
    if key not in _NC_CACHE:
        _NC_CACHE[key] = build()
    nc = _NC_CACHE[key]
    shared = host_consts(inp)
    for name, r, c in WEIGHTS:
        shared[name] = np.ascontiguousarray(inp[name], dtype=np.float32)
    in_maps = []
    for c in range(n_cores):
        m = dict(shared)
        m["x"] = x[2 * c:2 * c + 2].reshape(2 * SEQ, D)
        in_maps.append(m)
    res = run_bass_kernel_spmd(nc, in_maps, core_ids=list(range(n_cores)))
    out = np.stack([r["out"].reshape(2, SEQ, D) for r in res.results], axis=0).reshape(16, SEQ, D)
    return out.astype(np.float32)
```

```python
import contextlib
import math
import numpy as np
import concourse.bass as bass
import concourse.mybir as mybir
from concourse.bass_utils import run_bass_kernel_spmd

F32 = mybir.dt.float32
BF16 = mybir.dt.bfloat16
ALU = mybir.AluOpType
AF = mybir.ActivationFunctionType
AX = mybir.AxisListType

D = 1024
SEQ = 2048
DEPTH = 2
DFF = 2816
INC = 7436
EPS = 1e-6
NEG = -30000.0
C_FOX, C_CONV, C_ML, C_SWA, C_GATE = 0, 772, 1540, 2572, 3340

COMPUTE = ("pe", "act", "dve", "pool")
ISSUERS = ("pe", "act", "dve", "pool", "sp")


class Op:
    __slots__ = ("eng", "fn", "waits", "marked", "idx", "vc", "dma_sem", "dma_val", "count", "phase")

    def __init__(self, eng, fn):
        self.eng = eng
        self.fn = fn
        self.waits = []
        self.marked = False
        self.idx = -1
        self.vc = None
        self.dma_sem = None
        self.dma_val = 0
        self.count = 0


class Prog:
    def __init__(self):
        self.ops = {e: [] for e in ISSUERS}
        self.last_w = {}
        self.readers = {}
        self.known = {e: {} for e in ISSUERS}
        self.known_dma = {e: {} for e in ISSUERS}
        self.dma_count = {}
        self.phase = "init"
        self.annotate = False

    def _dep(self, op, prod):
        if prod is None or prod is op:
            return
        A = op.eng
        if prod.dma_sem is not None:
            val = self.dma_count[prod.dma_sem]
            if self.known_dma[A].get(prod.dma_sem, 0) >= val:
                return
            self.known_dma[A][prod.dma_sem] = val
            op.waits.append((prod, val))
        else:
            B = prod.eng
            if B == "pe" and A == "pe":
                return
            if self.known[A].get(B, -1) >= prod.idx:
                return
            op.waits.append((prod, None))
            prod.marked = True
        for b, n in prod.vc[0].items():
            if self.known[A].get(b, -1) < n:
                self.known[A][b] = n
        for s, v in prod.vc[1].items():
            if self.known_dma[A].get(s, 0) < v:
                self.known_dma[A][s] = v

    def last(self, eng):
        return self.ops[eng][-1] if self.ops[eng] else None

    def emit(self, eng, fn, reads=(), writes=(), dma_sem=None, after=()):
        op = Op(eng, fn)
        op.phase = self.phase
        op.idx = len(self.ops[eng])
        for r in reads:
            self._dep(op, self.last_w.get(r))
        for w in writes:
            self._dep(op, self.last_w.get(w))
            for rd in self.readers.get(w, ()):
                self._dep(op, rd)
        for a in after:
            self._dep(op, a)
        if dma_sem is not None:
            op.dma_sem = dma_sem
            self.dma_count[dma_sem] = self.dma_count.get(dma_sem, 0) + 16
            op.dma_val = self.dma_count[dma_sem]
        kc = dict(self.known[eng])
        if dma_sem is None:
            kc[eng] = op.idx
            op.vc = (kc, dict(self.known_dma[eng]))
        else:
            kd = dict(self.known_dma[eng])
            kd[dma_sem] = op.dma_val
            op.vc = (kc, kd)
        for r in reads:
            self.readers.setdefault(r, []).append(op)
        for w in writes:
            self.last_w[w] = op
            self.readers[w] = []
        self.ops[eng].append(op)
        return op

    def finalize(self, nc):
        for e in ISSUERS:
            c = 0
            for op in self.ops[e]:
                if op.dma_sem is None and op.marked:
                    c += 1
                op.count = c
        with contextlib.ExitStack() as st:
            esem = {e: st.enter_context(nc.semaphore("s_" + e)) for e in COMPUTE}
            dsem = {k: st.enter_context(nc.semaphore("d_" + str(k))) for k in self.dma_count}
            block = st.enter_context(nc.Block())

            def run(e, engine):
                for op in self.ops[e]:
                    for p, val in op.waits:
                        if p.dma_sem is not None:
                            engine.wait_ge(dsem[p.dma_sem], val)
                        else:
                            engine.wait_ge(esem[p.eng], p.count)
                    ins = op.fn(engine)
                    if self.annotate:
                        ins.annotate(op.phase)
                    if op.dma_sem is not None:
                        ins.then_inc(dsem[op.dma_sem], 16)
                    elif op.marked:
                        ins.then_inc(esem[e], 1)

            @block.tensor
            def _(eng):
                run("pe", eng)

            @block.scalar
            def _(eng):
                run("act", eng)

            @block.vector
            def _(eng):
                run("dve", eng)

            @block.gpsimd
            def _(eng):
                run("pool", eng)

            @block.sync
            def _(eng):
                run("sp", eng)


WEIGHTS = [
    ("w_in", D, INC), ("w_fox_out", 256, D), ("w_conv_out", 256, D), ("w_mlstm_out", 256, D),
    ("w_swa_out", 512, D), ("w_merge_out", D, D), ("w_gate", D, DFF), ("w_up", D, DFF),
    ("w_down", DFF, D),
]
ROWP = 532
RP_FQG, RP_FKG, RP_SQG, RP_SKG, RP_MHG, RP_FFB, RP_MIB, RP_MFB, RP_SNK = 0, 64, 128, 192, 256, 512, 516, 520, 524


class _Stop(Exception):
    pass


def build(n_seq=2, n_tiles=8, n_layers=2, NSUB=2, seq_len=SEQ, stop_at=99, do_cast=True, annotate=False):
    T = NSUB * 128
    nc = bass.Bass("TRN2", target_bir_lowering=False)
    NTOK = n_seq * seq_len
    x_d = nc.dram_tensor("x", [NTOK, D], F32, kind="ExternalInput").ap()
    out_d = nc.dram_tensor("out", [NTOK, D], F32, kind="ExternalOutput").ap()
    wf = {}
    wb = {}
    for name, r, c in WEIGHTS:
        wf[name] = nc.dram_tensor(name, [DEPTH, r, c], F32, kind="ExternalInput").ap()
    cst_d = nc.dram_tensor("cst", [128, 6, 128], F32, kind="ExternalInput").ap()
    swab_d = nc.dram_tensor("swab", [128, 2, 8, 128], F32, kind="ExternalInput").ap()
    swam_d = nc.dram_tensor("swam", [128, 2, 128], F32, kind="ExternalInput").ap()
    gT_d = nc.dram_tensor("gT", [128, 32], F32, kind="ExternalInput").ap()
    rowp_d = nc.dram_tensor("rowp", [1, DEPTH * ROWP], F32, kind="ExternalInput").ap()
    convw_d = nc.dram_tensor("convw", [128, 12], F32, kind="ExternalInput").ap()

    P = Prog()
    P.annotate = annotate
    st = contextlib.ExitStack()
    with st:
        def sb(name, shape, dt):
            return st.enter_context(nc.sbuf_tensor("s_" + name, shape, dt))

        def psum(name, shape, dt):
            return st.enter_context(nc.psum_tensor("p_" + name, shape, dt))

        def tt(eng, out, in0, in1, op, R, W):
            return P.emit(eng, lambda e: e.tensor_tensor(out=out, in0=in0, in1=in1, op=op), R, W)

        def ts(eng, out, in0, s1, s2, op0, op1, R, W):
            if s2 is None:
                return P.emit(eng, lambda e: e.tensor_scalar(out=out, in0=in0, scalar1=s1, scalar2=None, op0=op0), R, W)
            return P.emit(eng, lambda e: e.tensor_scalar(out=out, in0=in0, scalar1=s1, scalar2=s2, op0=op0, op1=op1), R, W)

        def stt(eng, out, in0, scalar, in1, op0, op1, R, W):
            return P.emit(eng, lambda e: e.scalar_tensor_tensor(out=out, in0=in0, scalar=scalar, in1=in1, op0=op0, op1=op1), R, W)

        def cp(eng, out, in_, R, W):
            if eng == "act":
                return P.emit("act", lambda e: e.copy(out=out, in_=in_), R, W)
            return P.emit(eng, lambda e: e.tensor_copy(out=out, in_=in_), R, W)

        def act(out, in_, func, R, W, bias=None, scale=1.0, accum=None):
            kw = {}
            if bias is not None:
                kw["bias"] = bias
            if accum is not None:
                kw["accum_out"] = accum
            return P.emit("act", lambda e: e.activation(out=out, in_=in_, func=func, scale=scale, **kw), R, W)

        def mm(out, lhsT, rhs, start, stop, R, W):
            return P.emit("pe", lambda e: e.matmul(out, lhsT=lhsT, rhs=rhs, start=start, stop=stop), R, W)

        def tr(out, in_, R, W):
            return P.emit("pe", lambda e: e.transpose(out=out, in_=in_, identity=identb[:]), list(R) + ["identb"], W)

        def mset(eng, ap, val, W):
            return P.emit(eng, lambda e: e.memset(ap, val), (), W)

        def recip(out, in_, R, W):
            return P.emit("dve", lambda e: e.reciprocal(out=out, in_=in_), R, W)

        def dma(eng, out, in_, R, W, sem):
            return P.emit(eng, lambda e: e.dma_start(out=out, in_=in_), R, W, dma_sem=sem)

        class Rot:
            def __init__(self, items):
                self.items = items
                self.i = 0

            def next(self):
                it = self.items[self.i % len(self.items)]
                self.i += 1
                return it

        mmR = Rot([(psum("mm%d" % i, [128, 512], F32), "mm%d" % i) for i in range(2)])
        tpR = Rot([(psum("tp%d" % i, [128, 8, 128], BF16), "tp%d" % i) for i in range(2)])
        scR = Rot([(psum("sc%d" % i, [128, 512], F32), "sc%d" % i) for i in range(2)])
        aoR = Rot([(psum("ao%d" % i, [128, 512], F32), "ao%d" % i) for i in range(2)])
        denseR = Rot(mmR.items + scR.items + aoR.items)
        foxR = Rot([mmR.items[1], scR.items[0]])
        attR = Rot([scR.items[1]])
        projR = Rot(mmR.items + scR.items)

        cst = sb("cst", [128, 6, 128], F32)
        identb = sb("identb", [128, 128], BF16)
        m01b = sb("m01b", [128, 128], BF16)
        chunkind = sb("chunkind", [128, 2, 64], F32)
        bm = sb("bm", [128, 2, 8, 128], F32)
        swam = sb("swam", [128, 2, 128], F32)
        gT = sb("gT", [128, 32], F32)
        rowp = sb("rowp", [128, DEPTH * ROWP], F32)
        convw = sb("convw", [128, 12], F32)
        expsink = sb("expsink", [128, DEPTH, 8], F32)
        tri, triblk, cmaskT, ones = cst[:, 1, :], cst[:, 2, :], cst[:, 3, :], cst[:, 4, :]

        dma("sp", cst[:], cst_d, (), ["cst"], "c0")
        dma("sp", bm[:], swab_d, (), ["bm"], "c1")
        dma("sp", swam[:], swam_d, (), ["swam"], "c2")
        dma("sp", gT[:], gT_d, (), ["gT"], "c3")
        dma("sp", rowp[:], rowp_d.partition_broadcast(128), (), ["rowp"], "c4")
        dma("sp", convw[:], convw_d, (), ["convw"], "c5")
        cp("dve", identb[:], cst[:, 0, :], ["cst"], ["identb"])
        cp("dve", m01b[:], cst[:, 5, :], ["cst"], ["m01b"])
        mset("pool", chunkind[:], 0.0, ["chunkind"])
        mset("pool", chunkind[0:64, 0, :], 1.0, ["chunkind"])
        mset("pool", chunkind[64:128, 1, :], 1.0, ["chunkind"])
        for blk in range(2):
            tt("dve", bm[:, blk, :, :], bm[:, blk, :, :], swam[:, blk, :].unsqueeze(1).to_broadcast([128, 8, 128]),
               ALU.add, ["bm", "swam"], ["bm"])
        for l in range(DEPTH):
            act(expsink[:, l, :], rowp[:, l * ROWP + RP_SNK: l * ROWP + RP_SNK + 8], AF.Exp, ["rowp"], ["expsink"])

        NSLOT = 4
        CAST_AHEAD = 8
        wslots = [sb("wslot%d" % i, [128, 8, 512], BF16) for i in range(NSLOT)]

        def layer_chunks(l):
            ch = []

            def add(name, r0, nrows, c0, ncol):
                ch.append((name, l, r0, nrows, c0, ncol, nrows // 128))

            def win(c0, ncol):
                add("w_in", 0, D, c0, ncol)

            win(C_FOX, 512)
            win(C_FOX + 512, 260)
            win(C_SWA + 512, 256)
            win(C_SWA, 512)
            win(C_ML + 512, 264)
            win(C_ML + 776, 256)
            win(C_ML, 512)
            win(C_CONV, 512)
            win(C_CONV + 512, 256)
            for b_, (nm, rows) in enumerate([("w_fox_out", 256), ("w_conv_out", 256), ("w_mlstm_out", 256), ("w_swa_out", 512)]):
                add(nm, 0, rows, 0, 1024)
                win(C_GATE + b_ * 1024, 512)
                win(C_GATE + b_ * 1024 + 512, 512)
            for h in range(2):
                add("w_merge_out", 0, D, h * 512, 512)
            for c in range(6):
                ncol = min(512, DFF - c * 512)
                add("w_gate", 0, D, c * 512, ncol)
                add("w_up", 0, D, c * 512, ncol)
            for h in range(2):
                for c in range(3):
                    k0 = c * 8
                    nk = min(8, 22 - k0)
                    add("w_down", k0 * 128, nk * 128, h * 512, 512)
            return ch

        first_pass = []
        for l in range(n_layers):
            first_pass.extend(layer_chunks(l))
        NFP = len(first_pass)
        scr = [nc.dram_tensor("scr%d" % k, [128, ch[6] * ch[5]], BF16, kind="Internal").ap() for k, ch in enumerate(first_pass)]
        n_pass = n_seq * n_tiles

        class WStream:
            def __init__(self):
                self.issued = 0
                self.taken = 0
                self.holds = {}
                self.cast_next = 0
                self.total = NFP * n_pass

            def _cast_upto(self, k_end):
                while self.cast_next < min(k_end, NFP):
                    k = self.cast_next
                    name, l, r0, nr, c0, nc_, nk = first_pass[k]
                    if do_cast:
                        dma("pool", scr[k].rearrange("p (k c) -> k p c", c=nc_),
                            wf[name][l, r0:r0 + nr, c0:c0 + nc_].rearrange("(k p) c -> k p c", p=128), (), [("wbc", k)], "cast%d" % k)
                    self.cast_next += 1

            def _view(self, n):
                name, l, r0, nr, c0, ncol, nk = first_pass[n % NFP]
                slot = n % NSLOT
                return wslots[slot][:].rearrange("p k c -> p (k c)")[:, 0:nk * ncol].rearrange("p (k c) -> p k c", c=ncol)

            def _issue(self, n):
                k = n % NFP
                name, l, r0, nr, c0, ncol, nk = first_pass[k]
                self._cast_upto(k + 1 + (CAST_AHEAD if n < NFP else 0))
                flat = wslots[n % NSLOT][:].rearrange("p k c -> p (k c)")[:, 0:nk * ncol]
                dma("sp", flat, scr[k], [("wbc", k)] if do_cast else [], [("ws", n % NSLOT)], "ws%d" % (n % NSLOT))

            def next(self, expect, hold=0):
                m = self.taken
                while self.issued < self.total and self.issued < m + NSLOT:
                    k = self.issued
                    prev = k - NSLOT
                    if prev >= 0 and not (prev + 1 + self.holds[prev] <= m):
                        break
                    self._issue(k)
                    self.issued += 1
                n = self.taken
                assert self.issued > n
                self.holds[n] = hold
                self.taken += 1
                assert first_pass[n % NFP][0] == expect, (first_pass[n % NFP][0], expect)
                return self._view(n), ("ws", n % NSLOT)

        WS = WStream()

        xt = sb("xt", [128, NSUB, D], F32)
        hT = sb("hT", [128, 8, T], BF16)
        hb = sb("hb", [128, D], BF16)
        junk = sb("junk", [128, D], BF16)
        small = sb("small", [128, 64], F32)
        stages = [sb("stage%d" % i, [128, 512], F32) for i in range(3)]
        stR = Rot([(stages[i], "stage%d" % i) for i in range(3)])
        sqtmp = sb("sqtmp", [128, 512], F32)
        nrm_ss = sb("nrm_ss", [128, 8], F32)
        qkn_l = [sb("qkn%d" % i, [128, 512], BF16) for i in range(NSUB)]
        fqT = sb("fqT", [128, 2, T], BF16)
        fkT = [sb("fkT%d" % l, [128, 2, seq_len], BF16) for l in range(n_layers)]
        fV = [sb("fV%d" % l, [128, seq_len // 128, 4, 65], BF16) for l in range(n_layers)]
        fnegc = [sb("fnegc%d" % l, [128, seq_len // 128, 4], F32) for l in range(n_layers)]
        fncar = [sb("fncar%d" % l, [128, NSUB + 1, 4], F32) for l in range(n_layers)]
        fsp = sb("fsp", [128, NSUB, 4], F32)
        fxf = sb("fxf", [128, 4], F32)
        fD = sb("fD", [128, 128], F32)
        fcbc = [sb("fcbc%d" % i, [128, T], F32) for i in range(2)]
        fcbm = [sb("fcbm%d" % i, [128, T], F32) for i in range(2)]
        atmp = [sb("atmp%d" % i, [128, 512], F32) for i in range(2)]
        apT = [sb("apT%d" % i, [128, 512], BF16) for i in range(2)]
        frec = sb("frec", [128, 8], F32)
        yfox = sb("yfox", [128, NSUB, 256], BF16)
        sqT = [sb("sqT%d" % i, [64, 8, 128], BF16) for i in range(NSUB)]
        swtmp = [sb("swtmp%d" % i, [128, 512], F32) for i in range(2)]
        swpT = [sb("swpT%d" % i, [128, 512], BF16) for i in range(2)]
        skT = [sb("skT%d" % l, [64, NSUB + 1, 2, 128], BF16) for l in range(n_layers)]
        sV = [sb("sV%d" % l, [128, NSUB + 1, 2, 65], BF16) for l in range(n_layers)]
        skst = sb("skst", [128, 128], F32)
        skn = sb("skn", [128, 128], BF16)
        yswa = [sb("yswa%d" % i, [128, 512], BF16) for i in range(2)]
        sden = sb("sden", [128, 4], F32)
        mV = sb("mV", [128, NSUB, 4, 65], BF16)
        mif = sb("mif", [128, NSUB, 8], F32)
        msp = sb("msp", [128, NSUB, 4], F32)
        mea = sb("mea", [128, NSUB, 4], F32)
        meb = sb("meb", [128, NSUB, 4], F32)
        mdec = sb("mdec", [64, NSUB, 2, 4], F32)
        mso = sb("mso", [128, NSUB, 256], F32)
        mkt = [sb("mkt%d" % i, [128, 256], BF16) for i in range(NSUB)]
        mqb_l = [sb("mqb%d" % i, [128, 256], BF16) for i in range(NSUB)]
        mqT = [sb("mqT%d" % i, [64, 4, 128], BF16) for i in range(NSUB)]
        mq0T = [sb("mq0T%d" % i, [64, 4, 128], BF16) for i in range(NSUB)]
        mq1T = [sb("mq1T%d" % i, [64, 4, 128], BF16) for i in range(NSUB)]
        mkT = [sb("mkT%d" % i, [64, 4, 128], BF16) for i in range(NSUB)]
        mPT = sb("mPT", [128, 4, 128], BF16)
        mCf = [sb("mCf%d" % l, [64, 4, 65], F32) for l in range(n_layers)]
        mCb = [[sb("mCb%d_%d" % (l, i), [64, 4, 65], BF16) for i in range(2)] for l in range(n_layers)]
        mt1 = sb("mt1", [128, 8], F32)
        mp2 = sb("mp2", [128, 16], F32)
        mhm = sb("mhm", [128, 256], F32)
        ymls = sb("ymls", [128, 256], BF16)
        cu = sb("cu", [128, 2, T], F32)
        cbt = sb("cbt", [128, 2, T], F32)
        cz = [sb("cz%d" % l, [128, 2, T + 2], F32) for l in range(n_layers)]
        cy = sb("cy", [128, T], F32)
        yT = sb("yT", [128, 10, T], BF16)
        sg = [sb("sg%d" % i, [128, 512], F32) for i in range(2)]
        mtmp = sb("mtmp", [128, 512], F32)
        merged = sb("merged", [128, NSUB, D], F32)
        actT = sb("actT", [128, 22, T], BF16)

        def rp(l, off, n):
            return rowp[:, l * ROWP + off: l * ROWP + off + n]

        for a in range(NSUB):
            mset("pool", mq0T[a][:], 0.0, [("mq0T", a)])
            mset("pool", mq1T[a][:], 0.0, [("mq1T", a)])
        mset("pool", mV[:], 1.0, ["mV"])
        for l in range(n_layers):
            mset("pool", fV[l][:], 1.0, [("fV", l)])
            mset("pool", sV[l][:], 1.0, [("sV", l)])

        def head_norm(src, nh, gain_views, out_bf, srcR, outW, out_scale=1.0):
            w = nh * 64
            tt("pool", sqtmp[:, 0:w], src, src, ALU.mult, srcR, ["sqtmp"])
            P.emit("dve", lambda e: e.tensor_reduce(out=nrm_ss[:, 0:nh], in_=sqtmp[:, 0:w].rearrange("p (h d) -> p h d", d=64),
                                                     axis=AX.X, op=ALU.add), ["sqtmp"], ["nrm_ss"])
            act(nrm_ss[:, 0:nh], nrm_ss[:, 0:nh], AF.Sqrt, ["nrm_ss"], ["nrm_ss"], bias=EPS, scale=1.0 / 64)
            recip(nrm_ss[:, 0:nh], nrm_ss[:, 0:nh], ["nrm_ss"], ["nrm_ss"])
            tt("dve", sqtmp[:, 0:w].rearrange("p (h d) -> p h d", d=64), src.rearrange("p (h d) -> p h d", d=64),
               nrm_ss[:, 0:nh].unsqueeze(2).to_broadcast([128, nh, 64]), ALU.mult, list(srcR) + ["nrm_ss"], ["sqtmp"])
            for h0, h1, g in gain_views:
                n = h1 - h0
                tt("pool", out_bf[:, h0 * 64:h1 * 64].rearrange("p (h d) -> p h d", d=64),
                   sqtmp[:, h0 * 64:h1 * 64].rearrange("p (h d) -> p h d", d=64),
                   g.unsqueeze(1).to_broadcast([128, n, 64]), ALU.mult, ["sqtmp", "rowp"], outW)

        def resid_norm(l, which):
            for a in range(NSUB):
                mset("pool", small[:, 0:1], 0.0, ["small0"])
                act(junk[:], xt[:, a, :], AF.Square, [("xt", a), "small0"], ["junk", "small0"], accum=small[:, 0:1])
                act(small[:, 1:2], small[:, 0:1], AF.Sqrt, ["small0"], ["small1"], bias=EPS, scale=1.0 / D)
                recip(small[:, 2:3], small[:, 1:2], ["small1"], ["small2"])
                ts("dve", hb[:], xt[:, a, :], small[:, 2:3], None, ALU.mult, None, [("xt", a), "small2"], ["hb"])
                tp, tpk = tpR.next()
                for kc in range(8):
                    tr(tp[:, kc, :], hb[:, kc * 128:(kc + 1) * 128], ["hb"], [tpk])
                g0 = l * 16 + which * 8
                tt("dve", hT[:, :, a * 128:(a + 1) * 128], tp[:, :, :],
                   gT[:, g0:g0 + 8].unsqueeze(2).to_broadcast([128, 8, 128]), ALU.mult, [tpk, "gT"], [("hT", a)])

        HT_ALL = [("hT", a) for a in range(NSUB)]

        def proj_tok(W, wk, ncol, a, rot=None):
            ps, pk = (rot or projR).next()
            for kc in range(8):
                mm(ps[:, 0:ncol], hT[:, kc, a * 128:(a + 1) * 128], W[:, kc, :], kc == 0, kc == 7, [("hT", a), wk], [pk])
            return ps, pk

        def main_loop():
          for s in range(n_seq):
            for t in range(n_tiles):
                tok0 = s * seq_len + t * T
                blk0 = t * NSUB
                first_tile = (t == 0)
                for a in range(NSUB):
                    dma("sp", xt[:, a, :], x_d[tok0 + a * 128: tok0 + (a + 1) * 128, :], (), [("xt", a)], "xt")
                for l in range(n_layers):
                    if first_tile:
                        mset("pool", fncar[l][:, 0, :], 0.0, [("fncar", l)])
                        mset("pool", mCf[l][:], 0.0, [("mCf", l)])
                        mset("pool", mCb[l][0][:], 0.0, [("mCb", l, 0)])
                        mset("pool", cz[l][:, :, 0:2], 0.0, [("cz", l)])
                    if stop_at <= 0:
                        raise _Stop()
                    deferred = []

                    def flush():
                        while deferred:
                            deferred.pop(0)()
                    P.phase = "norm1"
                    resid_norm(l, 0)

                    if stop_at <= 1:
                        raise _Stop()
                    P.phase = "fox_proj"
                    W, wk = WS.next("w_in")
                    for a in range(NSUB):
                        ps, pk = proj_tok(W, wk, 512, a)
                        stg, sk = stR.next()
                        cp("act", stg[:], ps[:, 0:512], [pk], [sk])
                        if stop_at <= 1.1:
                            raise _Stop()
                        qkn, qknk = qkn_l[a], "qkn%d" % a
                        head_norm(stg[:], 8, [(0, 4, rp(l, RP_FQG, 64)), (4, 8, rp(l, RP_FKG, 64))], qkn, [sk], [qknk])

                        def fox_tr(a=a, qkn=qkn, qknk=qknk):
                            tp, tpk = tpR.next()
                            for k in range(4):
                                tr(tp[:, k, :], qkn[:, k * 128:(k + 1) * 128], [qknk], [tpk])
                            cp("act", fqT[:, :, a * 128:(a + 1) * 128], tp[:, 0:2, :], [tpk], ["fqT"])
                            cp("act", fkT[l][:, :, (blk0 + a) * 128:(blk0 + a + 1) * 128], tp[:, 2:4, :], [tpk], [("fkT", l)])
                        deferred.append(fox_tr)
                    if stop_at <= 1.3:
                        raise _Stop()
                    W, wk = WS.next("w_in")
                    for a in range(NSUB):
                        ps, pk = proj_tok(W, wk, 260, a)
                        cp("dve", fV[l][:, blk0 + a, :, 0:64], ps[:, 0:256].rearrange("p (h d) -> p h d", d=64), [pk], [("fV", l)])
                        if stop_at <= 1.4:
                            raise _Stop()
                        tt("dve", fxf[:], ps[:, 256:260], rp(l, RP_FFB, 4), ALU.add, [pk, "rowp"], ["fxf"])
                        act(fxf[:], fxf[:], AF.Exp, ["fxf"], ["fxf"], scale=-1.0)
                        act(fsp[:, a, :], fxf[:], AF.Ln, ["fxf"], [("fsp", a)], bias=1.0)
                        if stop_at <= 1.5:
                            raise _Stop()
                        p2, p2k = aoR.next()
                        mm(p2[:, 0:4], tri, fsp[:, a, :], True, True, ["cst", ("fsp", a)], [p2k])
                        mm(p2[:, 4:8], ones, fsp[:, a, :], True, True, ["cst", ("fsp", a)], [p2k])
                        tt("dve", fnegc[l][:, blk0 + a, :], p2[:, 0:4], fncar[l][:, a, :], ALU.add, [p2k, ("fncar", l)], [("fnegc", l)])
                        tt("dve", fncar[l][:, a + 1, :], p2[:, 4:8], fncar[l][:, a, :], ALU.add, [p2k, ("fncar", l)], [("fncar", l)])

                    flush()
                    if stop_at <= 2:
                        raise _Stop()
                    P.phase = "swa_proj"
                    W, wk = WS.next("w_in")
                    for a in range(NSUB):
                        ps, pk = proj_tok(W, wk, 256, a)
                        cp("act", skst[:], ps[:, 0:128], [pk], ["skst"])
                        cp("act", sV[l][:, a + 1, :, 0:64], ps[:, 128:256].rearrange("p (h d) -> p h d", d=64), [pk], [("sV", l)])
                        head_norm(skst[:], 2, [(0, 2, rp(l, RP_SKG, 64))], skn, ["skst"], ["skn"])
                        tp, tpk = tpR.next()
                        for g in range(2):
                            tr(tp[0:64, g, :], skn[:, g * 64:(g + 1) * 64], ["skn"], [tpk])
                        cp("dve", skT[l][:, a + 1, :, :], tp[0:64, 0:2, :], [tpk], [("skT", l)])
                    W, wk = WS.next("w_in")
                    for a in range(NSUB):
                        ps, pk = proj_tok(W, wk, 512, a)
                        stg, sk = stR.next()
                        cp("act", stg[:], ps[:, 0:512], [pk], [sk])
                        qkn, qknk = qkn_l[a], "qkn%d" % a
                        head_norm(stg[:], 8, [(0, 8, rp(l, RP_SQG, 64))], qkn, [sk], [qknk])

                        def swa_tr(a=a, qkn=qkn, qknk=qknk):
                            qb_, qbk = sqT[a], "sqT%d" % a
                            for half in range(2):
                                tp, tpk = tpR.next()
                                for k in range(4):
                                    hh = half * 4 + k
                                    tr(tp[0:64, k, :], qkn[:, hh * 64:(hh + 1) * 64], [qknk], [tpk])
                                cp("act" if half else "dve", qb_[:, half * 4:half * 4 + 4, :], tp[0:64, 0:4, :], [tpk], [qbk])
                        deferred.append(swa_tr)

                    P.phase = "mlstm_proj"
                    W, wk = WS.next("w_in")
                    for a in range(NSUB):
                        ps, pk = proj_tok(W, wk, 264, a)
                        cp("dve", mV[:, a, :, 0:64], ps[:, 0:256].rearrange("p (h d) -> p h d", d=64), [pk], ["mV"])
                        tt("dve", mif[:, a, 0:4], ps[:, 256:260], rp(l, RP_MIB, 4), ALU.add, [pk, "rowp"], [("mif", a)])
                        tt("dve", mif[:, a, 4:8], ps[:, 260:264], rp(l, RP_MFB, 4), ALU.add, [pk, "rowp"], [("mif", a)])
                        act(mt1[:, 0:4], mif[:, a, 4:8], AF.Exp, [("mif", a)], ["mt1"], scale=-1.0)
                        act(msp[:, a, :], mt1[:, 0:4], AF.Ln, ["mt1"], [("msp", a)], bias=1.0)
                        p2, p2k = aoR.next()
                        mm(p2[:, 0:4], triblk, msp[:, a, :], True, True, ["cst", ("msp", a)], [p2k])
                        for c in range(2):
                            mm(p2[0:64, 8 + 4 * c:12 + 4 * c], chunkind[:, c, :], msp[:, a, :], True, True, ["chunkind", ("msp", a)], [p2k])
                        cp("dve", mp2[:, 0:4], p2[:, 0:4], [p2k], ["mp2"])
                        cp("dve", mp2[0:64, 8:16], p2[0:64, 8:16], [p2k], ["mp2"])
                        tt("dve", mt1[:, 4:8], mp2[:, 0:4], mif[:, a, 0:4], ALU.add, ["mp2", ("mif", a)], ["mt1"])
                        act(mea[:, a, :], mt1[:, 4:8], AF.Exp, ["mt1"], [("mea", a)])
                        act(meb[:, a, :], mp2[:, 0:4], AF.Exp, ["mp2"], [("meb", a)], scale=-1.0)
                        act(mdec[:, a, :, :], mp2[0:64, 8:16].rearrange("p (c h) -> p c h", h=4), AF.Exp, ["mp2"], [("mdec", a)], scale=-1.0)
                    W, wk = WS.next("w_in")
                    for a in range(NSUB):
                        ps, pk = proj_tok(W, wk, 256, a)
                        act(mso[:, a, :], ps[:, 0:256], AF.Sigmoid, [pk], [("mso", a)])
                    flush()
                    W, wk = WS.next("w_in")
                    for a in range(NSUB):
                        ps, pk = proj_tok(W, wk, 512, a)
                        ts("dve", mqb_l[a][:], ps[:, 0:256], 0.125, None, ALU.mult, None, [pk], [("mqb", a)])
                        tt("dve", mkt[a][:].rearrange("p (h d) -> p h d", d=64), ps[:, 256:512].rearrange("p (h d) -> p h d", d=64),
                           mea[:, a, :].unsqueeze(2).to_broadcast([128, 4, 64]), ALU.mult, [pk, ("mea", a)], [("mkt", a)])

                        def ml_tr(a=a):
                            tq, tqk = tpR.next()
                            for h in range(4):
                                tr(tq[0:64, h, :], mqb_l[a][:, h * 64:(h + 1) * 64], [("mqb", a)], [tqk])
                            tk, tkk = tpR.next()
                            for h in range(4):
                                tr(tk[0:64, h, :], mkt[a][:, h * 64:(h + 1) * 64], [("mkt", a)], [tkk])
                            cp("dve", mqT[a][:], tq[0:64, 0:4, :], [tqk], [("mqT", a)])
                            cp("dve", mq0T[a][:, :, 0:64], tq[0:64, 0:4, 0:64], [tqk], [("mq0T", a)])
                            cp("dve", mq1T[a][:, :, 64:128], tq[0:64, 0:4, 64:128], [tqk], [("mq1T", a)])
                            cp("act", mkT[a][:], tk[0:64, 0:4, :], [tkk], [("mkT", a)])
                        deferred.append(ml_tr)

                    if stop_at <= 5:
                        raise _Stop()
                    P.phase = "conv"
                    W, wk = WS.next("w_in")
                    for cb_ in range(4):
                        ps, pk = projR.next()
                        for kc in range(8):
                            mm(ps[:, 0:T], W[:, kc, cb_ * 128:(cb_ + 1) * 128], hT[:, kc, :], kc == 0, kc == 7, HT_ALL + [wk], [pk])
                        dst = cu if cb_ < 2 else cbt
                        cp("act", dst[:, cb_ % 2, :], ps[:, 0:T], [pk], ["cu" if cb_ < 2 else "cbt"])
                    W, wk = WS.next("w_in")
                    for cc in range(2):
                        ps, pk = projR.next()
                        for kc in range(8):
                            mm(ps[:, 0:T], W[:, kc, cc * 128:(cc + 1) * 128], hT[:, kc, :], kc == 0, kc == 7, HT_ALL + [wk], [pk])
                        z = cz[l]
                        tt("dve", z[:, cc, 2:T + 2], ps[:, 0:T], cu[:, cc, :], ALU.mult, [pk, "cu"], [("cz", l)])
                        w0 = convw[:, l * 6 + cc * 3 + 0: l * 6 + cc * 3 + 1]
                        w1 = convw[:, l * 6 + cc * 3 + 1: l * 6 + cc * 3 + 2]
                        w2 = convw[:, l * 6 + cc * 3 + 2: l * 6 + cc * 3 + 3]
                        ts("pool", cy[:], z[:, cc, 2:T + 2], w2, None, ALU.mult, None, [("cz", l), "convw"], ["cy"])
                        stt("dve", cy[:], z[:, cc, 1:T + 1], w1, cy[:], ALU.mult, ALU.add, [("cz", l), "convw", "cy"], ["cy"])
                        stt("dve", cy[:], z[:, cc, 0:T], w0, cy[:], ALU.mult, ALU.add, [("cz", l), "convw", "cy"], ["cy"])
                        tt("dve", yT[:, 2 + cc, :], cy[:], cbt[:, cc, :], ALU.mult, ["cy", "cbt"], [("yT", 1)])
                        cp("pool", z[:, cc, 0:2], z[:, cc, T:T + 2], [("cz", l)], [("cz", l)])


                    flush()
                    nblk = blk0 + NSUB

                    def gen_fox():
                        for h in range(4):
                            P.phase = "fox_attn"
                            hp, hbase = h // 2, (h % 2) * 64
                            cb, cbk = fcbc[h % 2], "fcbc%d" % (h % 2)
                            cbm, cbmk = fcbm[h % 2], "fcbm%d" % (h % 2)
                            pc, pck = foxR.next()
                            for a in range(NSUB):
                                ts("dve", fD[:], ones, fsp[:, a, h:h + 1], None, ALU.mult, None, ["cst", ("fsp", a)], ["fD"])
                                mm(pc[:, a * 128:(a + 1) * 128], fD[:], tri, True, True, ["fD", "cst"], [pck])
                            for a in range(NSUB):
                                act(cb[:, a * 128:(a + 1) * 128], pc[:, a * 128:(a + 1) * 128], AF.Identity, [pck, ("fncar", l)], [cbk],
                                    bias=fncar[l][:, a, h:h + 1])
                            tt("pool", cbm[:].rearrange("p (a t) -> p a t", t=128), cb[:].rearrange("p (a t) -> p a t", t=128),
                               cmaskT.unsqueeze(1).to_broadcast([128, NSUB, 128]), ALU.subtract, [cbk, "cst"], [cbmk])
                            acc, acck = mmR.items[0]
                            accv = acc[:, 0:NSUB * 65].rearrange("p (a c) -> p a c", c=65)
                            yield

                            def s_qk(j):
                                a0 = max(0, j - blk0)
                                c0 = a0 * 128
                                sc, sck = foxR.next()
                                scs[j] = (sc, sck)
                                mm(sc[:, c0:T], fkT[l][hbase:hbase + 64, hp, j * 128:(j + 1) * 128], fqT[hbase:hbase + 64, hp, c0:T],
                                   True, True, [("fkT", l), "fqT"], [sck])

                            def s_dve(j):
                                a0 = max(0, j - blk0)
                                c0 = a0 * 128
                                sc, sck = scs.pop(j)
                                bi = j % 2
                                tmp, tmpk = atmp[bi], "atmp%d" % bi
                                if j >= blk0:
                                    stt("dve", tmp[:, c0:c0 + 128], sc[:, c0:c0 + 128], 0.125, cbm[:, c0:c0 + 128], ALU.mult, ALU.subtract,
                                        [sck, cbmk], [tmpk])
                                    if c0 + 128 < T:
                                        stt("dve", tmp[:, c0 + 128:T], sc[:, c0 + 128:T], 0.125, cb[:, c0 + 128:T], ALU.mult, ALU.subtract,
                                            [sck, cbk], [tmpk])
                                else:
                                    stt("dve", tmp[:, c0:T], sc[:, c0:T], 0.125, cb[:, c0:T], ALU.mult, ALU.subtract, [sck, cbk], [tmpk])

                            def s_act(j):
                                c0 = max(0, j - blk0) * 128
                                bi = j % 2
                                act(apT[bi][:, c0:T], atmp[bi][:, c0:T], AF.Exp, ["atmp%d" % bi, ("fnegc", l)], ["apT%d" % bi],
                                    bias=fnegc[l][:, j, h:h + 1])

                            def s_pv(j):
                                a0 = max(0, j - blk0)
                                bi = j % 2
                                pT, pTk = apT[bi], "apT%d" % bi
                                for a in range(a0, NSUB):
                                    mm(accv[:, a, :], pT[:, a * 128:(a + 1) * 128], fV[l][:, j, h, :], (j == 0 and a == 0),
                                       (j == nblk - 1 and a == NSUB - 1), [pTk, ("fV", l)], [acck])

                            scs = {}
                            for step in range(nblk + 3):
                                P.phase = "fox_attn"
                                if 0 <= step - 3 < nblk:
                                    s_pv(step - 3)
                                if 0 <= step - 2 < nblk:
                                    s_act(step - 2)
                                if 0 <= step - 1 < nblk:
                                    s_dve(step - 1)
                                if step < nblk:
                                    s_qk(step)
                                yield
                            P.phase = "fox_attn"
                            recip(frec[:, 0:NSUB], accv[:, :, 64], [acck], ["frec"])
                            tt("dve", yfox[:, :, h * 64:(h + 1) * 64], accv[:, :, 0:64],
                               frec[:, 0:NSUB].unsqueeze(2).to_broadcast([128, NSUB, 64]), ALU.mult, [acck, "frec"], ["yfox"])
                            yield
                        P.phase = "fox_attn"
                        cp("pool", fncar[l][:, 0, :], fncar[l][:, NSUB, :], [("fncar", l)], [("fncar", l)])
                        for a in range(NSUB):
                            tp, tpk = tpR.next()
                            for k in range(2):
                                tr(tp[:, k, :], yfox[:, a, k * 128:(k + 1) * 128], ["yfox"], [tpk])
                            cp("act", yT[:, 0:2, a * 128:(a + 1) * 128], tp[:, 0:2, :], [tpk], [("yT", 0)])

                    def gen_swa():
                        for a in range(NSUB):
                            qb_, qbk = sqT[a], "sqT%d" % a
                            has_prev = not (first_tile and a == 0)
                            yb, ybk = yswa[a % 2], "yswa%d" % (a % 2)
                            for g in range(2):
                                P.phase = "swa"
                                pTs = []
                                for which in ([0, 1] if has_prev else [1]):
                                    blk = a + which
                                    sc, sck = attR.next()
                                    mm(sc[:], skT[l][:, blk, g, :], qb_[:, 4 * g:4 * g + 4, :].rearrange("p h t -> p (h t)"),
                                       True, True, [("skT", l), qbk], [sck])
                                    tmp, tmpk, pT, pTk = swtmp[which], "swtmp%d" % which, swpT[which], "swpT%d" % which
                                    stt("dve", tmp[:], sc[:], 0.125, bm[:, which, 4 * g:4 * g + 4, :].rearrange("p h t -> p (h t)"),
                                        ALU.mult, ALU.add, [sck, "bm"], [tmpk])
                                    act(pT[:], tmp[:], AF.Exp, [tmpk], [pTk])
                                    pTs.append((pT, pTk, blk))
                                ao, aok = aoR.next()
                                aov = ao[:, 0:260].rearrange("p (h c) -> p h c", c=65)
                                for hh in range(4):
                                    for i, (pT, pTk, blk) in enumerate(pTs):
                                        mm(aov[:, hh, :], pT[:, hh * 128:(hh + 1) * 128], sV[l][:, blk, g, :], i == 0, i == len(pTs) - 1,
                                           [pTk, ("sV", l)], [aok])
                                tt("dve", sden[:], aov[:, :, 64], expsink[:, l, 4 * g:4 * g + 4], ALU.add, [aok, "expsink"], ["sden"])
                                recip(sden[:], sden[:], ["sden"], ["sden"])
                                tt("dve", yb[:, g * 256:(g + 1) * 256].rearrange("p (h d) -> p h d", d=64), aov[:, :, 0:64],
                                   sden[:].unsqueeze(2).to_broadcast([128, 4, 64]), ALU.mult, [aok, "sden"], [ybk])
                                yield
                            P.phase = "swa"
                            tp, tpk = tpR.next()
                            for k in range(4):
                                tr(tp[:, k, :], yb[:, k * 128:(k + 1) * 128], [ybk], [tpk])
                            cp("act", yT[:, 6:10, a * 128:(a + 1) * 128], tp[:, 0:4, :], [tpk], [("yT", 3)])
                            yield
                        P.phase = "swa"
                        cp("pool", skT[l][:, 0, :, :], skT[l][:, NSUB, :, :], [("skT", l)], [("skT", l)])
                        cp("pool", sV[l][:, 0, :, :], sV[l][:, NSUB, :, :], [("sV", l)], [("sV", l)])

                    def gen_mlstm():
                        for a in range(NSUB):
                            P.phase = "mlstm"
                            sc, sck = attR.next()
                            scv = sc[:].rearrange("p (h t) -> p h t", t=128)
                            for h in range(4):
                                mm(scv[:, h, :], mkT[a][:, h, :], mqT[a][:, h, :], True, True, [("mkT", a), ("mqT", a)], [sck])
                            tt("dve", mPT[:], scv, m01b[:].unsqueeze(1).to_broadcast([128, 4, 128]), ALU.mult, [sck, "m01b"], ["mPT"])
                            yield
                            P.phase = "mlstm"
                            pd, pdk = aoR.next()
                            pdv = pd[0:64, 0:260].rearrange("p (h c) -> p h c", c=65)
                            for h in range(4):
                                mm(pdv[:, h, :], mkt[a][0:64, h * 64:(h + 1) * 64], mV[0:64, a, h, :], True, True, [("mkt", a), "mV"], [pdk])
                            tt("dve", mCf[l][:], mCf[l][:], pdv, ALU.add, [("mCf", l), pdk], [("mCf", l)])
                            tt("dve", mCf[l][:], mCf[l][:], mdec[:, a, 0, :].unsqueeze(2).to_broadcast([64, 4, 65]), ALU.mult,
                               [("mCf", l), ("mdec", a)], [("mCf", l)])
                            cp("pool", mCb[l][1][:], mCf[l][:], [("mCf", l)], [("mCb", l, 1)])
                            yield
                            P.phase = "mlstm"
                            ao, aok = aoR.next()
                            aov = ao[:, 0:260].rearrange("p (h c) -> p h c", c=65)
                            for h in range(4):
                                mm(aov[:, h, :], mPT[:, h, :], mV[:, a, h, :], True, False, ["mPT", "mV"], [aok])
                                mm(aov[:, h, :], mq0T[a][:, h, :], mCb[l][0][:, h, :], False, False, [("mq0T", a), ("mCb", l, 0)], [aok])
                                mm(aov[:, h, :], mq1T[a][:, h, :], mCb[l][1][:, h, :], False, True, [("mq1T", a), ("mCb", l, 1)], [aok])
                            tt("dve", mt1[:, 0:4], aov[:, :, 64], meb[:, a, :], ALU.mult, [aok, ("meb", a)], ["mt1"])
                            stt("dve", mt1[:, 0:4], mt1[:, 0:4], -1.0, mt1[:, 0:4], ALU.mult, ALU.max, ["mt1"], ["mt1"])
                            P.emit("dve", lambda e: e.tensor_scalar_max(out=mt1[:, 0:4], in0=mt1[:, 0:4], scalar1=1.0), ["mt1"], ["mt1"])
                            recip(mt1[:, 0:4], mt1[:, 0:4], ["mt1"], ["mt1"])
                            tt("dve", mt1[:, 0:4], mt1[:, 0:4], meb[:, a, :], ALU.mult, ["mt1", ("meb", a)], ["mt1"])
                            tt("dve", mhm[:].rearrange("p (h d) -> p h d", d=64), aov[:, :, 0:64],
                               mt1[:, 0:4].unsqueeze(2).to_broadcast([128, 4, 64]), ALU.mult, [aok, "mt1"], ["mhm"])
                            yield
                            P.phase = "mlstm"
                            pd, pdk = aoR.next()
                            pdv = pd[0:64, 0:260].rearrange("p (h c) -> p h c", c=65)
                            for h in range(4):
                                mm(pdv[:, h, :], mkt[a][64:128, h * 64:(h + 1) * 64], mV[64:128, a, h, :], True, True, [("mkt", a), "mV"], [pdk])
                            tt("dve", mCf[l][:], mCf[l][:], pdv, ALU.add, [("mCf", l), pdk], [("mCf", l)])
                            tt("dve", mCf[l][:], mCf[l][:], mdec[:, a, 1, :].unsqueeze(2).to_broadcast([64, 4, 65]), ALU.mult,
                               [("mCf", l), ("mdec", a)], [("mCf", l)])
                            cp("pool", mCb[l][0][:], mCf[l][:], [("mCf", l)], [("mCb", l, 0)])
                            yield
                            P.phase = "mlstm"
                            tt("pool", sqtmp[:, 0:256], mhm[:], mhm[:], ALU.mult, ["mhm"], ["sqtmp"])
                            P.emit("dve", lambda e: e.tensor_reduce(out=nrm_ss[:, 0:4], in_=sqtmp[:, 0:256].rearrange("p (h d) -> p h d", d=64),
                                                                     axis=AX.X, op=ALU.add), ["sqtmp"], ["nrm_ss"])
                            act(nrm_ss[:, 0:4], nrm_ss[:, 0:4], AF.Sqrt, ["nrm_ss"], ["nrm_ss"], bias=EPS, scale=1.0 / 64)
                            recip(nrm_ss[:, 0:4], nrm_ss[:, 0:4], ["nrm_ss"], ["nrm_ss"])
                            tt("dve", mhm[:].rearrange("p (h d) -> p h d", d=64), mhm[:].rearrange("p (h d) -> p h d", d=64),
                               nrm_ss[:, 0:4].unsqueeze(2).to_broadcast([128, 4, 64]), ALU.mult, ["mhm", "nrm_ss"], ["mhm"])
                            tt("pool", mhm[:], mhm[:], rp(l, RP_MHG, 256), ALU.mult, ["mhm", "rowp"], ["mhm"])
                            tt("dve", ymls[:], mhm[:], mso[:, a, :], ALU.mult, ["mhm", ("mso", a)], ["ymls"])
                            tp, tpk = tpR.next()
                            for k in range(2):
                                tr(tp[:, k, :], ymls[:, k * 128:(k + 1) * 128], ["ymls"], [tpk])
                            cp("act", yT[:, 4:6, a * 128:(a + 1) * 128], tp[:, 0:2, :], [tpk], [("yT", 2)])
                            yield

                    gens = [[gen_fox(), max(1, (4 * (nblk + 2)) // (5 * NSUB))], [gen_swa(), 1], [gen_mlstm(), 1]]
                    while gens:
                        for ge in list(gens):
                            for _ in range(ge[1]):
                                try:
                                    next(ge[0])
                                except StopIteration:
                                    gens.remove(ge)
                                    break

                    if stop_at <= 6:
                        raise _Stop()
                    P.phase = "merge"
                    ych0 = [0, 2, 4, 6]
                    for b, nm in enumerate(["w_fox_out", "w_conv_out", "w_mlstm_out", "w_swa_out"]):
                        Wb, wbk = WS.next(nm, hold=2)
                        nkc = 4 if b == 3 else 2
                        for half in range(2):
                            Wg, wgk = WS.next("w_in")
                            for a in range(NSUB):
                                pg, pgk = proj_tok(Wg, wgk, 512, a, denseR)
                                s_, s_k = sg[a % 2], "sg%d" % (a % 2)
                                act(s_[:], pg[:, 0:512], AF.Sigmoid, [pgk], [s_k])
                                po, pok = denseR.next()
                                for kc in range(nkc):
                                    mm(po[:, 0:512], yT[:, ych0[b] + kc, a * 128:(a + 1) * 128], Wb[:, kc, half * 512:(half + 1) * 512],
                                       kc == 0, kc == nkc - 1, [("yT", b), wbk], [pok])
                                mdst = merged[:, a, half * 512:(half + 1) * 512]
                                if b == 0:
                                    tt("dve", mdst, po[:, 0:512], s_[:], ALU.mult, [pok, s_k], [("merged", a)])
                                else:
                                    tt("dve", mtmp[:], po[:, 0:512], s_[:], ALU.mult, [pok, s_k], ["mtmp"])
                                    tt("pool", mdst, mdst, mtmp[:], ALU.add, [("merged", a), "mtmp"], [("merged", a)])
                    for a in range(NSUB):
                        cp("act", hb[:], merged[:, a, :], [("merged", a)], ["hb"])
                        tp, tpk = tpR.next()
                        for kc in range(8):
                            tr(tp[:, kc, :], hb[:, kc * 128:(kc + 1) * 128], ["hb"], [tpk])
                        cp("act" if a % 2 else "dve", hT[:, :, a * 128:(a + 1) * 128], tp[:, :, :], [tpk], [("hT", a)])
                    P.phase = "merge_out"
                    for half in range(2):
                        Wm, wmk = WS.next("w_merge_out")
                        for a in range(NSUB):
                            ps, pk = proj_tok(Wm, wmk, 512, a, denseR)
                            xs = xt[:, a, half * 512:(half + 1) * 512]
                            tt("dve", xs, ps[:, 0:512], xs, ALU.add, [pk, ("xt", a)], [("xt", a)])

                    if stop_at <= 7:
                        raise _Stop()
                    P.phase = "ffn"
                    resid_norm(l, 1)
                    for c in range(6):
                        ncol = min(512, DFF - c * 512)
                        Wg, wgk = WS.next("w_gate", hold=1)
                        Wu, wuk = WS.next("w_up")
                        for cb_ in range(ncol // 128):
                            ffc = c * 4 + cb_
                            pg, pgk = denseR.next()
                            for kc in range(8):
                                mm(pg[:, 0:T], Wg[:, kc, cb_ * 128:(cb_ + 1) * 128], hT[:, kc, :], kc == 0, kc == 7, HT_ALL + [wgk], [pgk])
                            pu, puk = denseR.next()
                            for kc in range(8):
                                mm(pu[:, 0:T], Wu[:, kc, cb_ * 128:(cb_ + 1) * 128], hT[:, kc, :], kc == 0, kc == 7, HT_ALL + [wuk], [puk])
                            s_, s_k = sg[ffc % 2], "sg%d" % (ffc % 2)
                            act(s_[:, 0:T], pg[:, 0:T], AF.Silu, [pgk], [s_k])
                            tt("dve", actT[:, ffc, :], pu[:, 0:T], s_[:, 0:T], ALU.mult, [puk, s_k], [("actT", ffc)])
                    P.phase = "ffn_down"
                    ACT_ALL = [("actT", f) for f in range(22)]
                    for half in range(2):
                        accs = []
                        for a in range(NSUB):
                            accs.append(denseR.next())
                        for c in range(3):
                            Wd, wdk = WS.next("w_down")
                            k0 = c * 8
                            nk = min(8, 22 - k0)
                            for a in range(NSUB):
                                ps, pk = accs[a]
                                for k in range(nk):
                                    mm(ps[:, 0:512], actT[:, k0 + k, a * 128:(a + 1) * 128], Wd[:, k, :], (k0 + k) == 0, (k0 + k) == 21,
                                       ACT_ALL + [wdk], [pk])
                        for a in range(NSUB):
                            ps, pk = accs[a]
                            xs = xt[:, a, half * 512:(half + 1) * 512]
                            tt("dve", xs, ps[:, 0:512], xs, ALU.add, [pk, ("xt", a)], [("xt", a)])
                for a in range(NSUB):
                    dma("sp", out_d[tok0 + a * 128: tok0 + (a + 1) * 128, :], xt[:, a, :], [("xt", a)], ["out"], "out")
        try:
            main_loop()
        except _Stop:
            for a in range(NSUB):
                dma("sp", out_d[a * 128:(a + 1) * 128, :], xt[:, a, :], [("xt", a)], ["out"], "out")
        P.emit("sp", lambda e: e.nop(), ["out"], ())
        P.finalize(nc)
    return nc


def _t5_bucket(n):
    max_exact = 16
    nf = np.maximum(n, 1).astype(np.float32)
    large = max_exact + (np.log(nf / max_exact) / math.log(128 / max_exact) * (32 - max_exact)).astype(np.int32)
    large = np.minimum(large, 31)
    return np.where(n < max_exact, n, large)


def host_consts(inp):
    f32 = np.float32
    s = np.arange(128)[:, None]
    t = np.arange(128)[None, :]
    cst = np.zeros((128, 6, 128), f32)
    cst[:, 0] = np.eye(128, dtype=f32)
    cst[:, 1] = (s <= t)
    cst[:, 2] = (s <= t) & (s // 64 == t // 64)
    cst[:, 3] = np.where(s <= t, 0.0, NEG)
    cst[:, 4] = 1.0
    cst[:, 5] = cst[:, 2]
    j = np.arange(128)[:, None]
    i = np.arange(128)[None, :]
    swam = np.zeros((128, 2, 128), f32)
    d_prev = i + 128 - j
    d_cur = i - j
    swam[:, 0] = np.where((d_prev >= 0) & (d_prev < 128), 0.0, NEG)
    swam[:, 1] = np.where((d_cur >= 0) & (d_cur < 128), 0.0, NEG)
    rel = np.asarray(inp["rel_bias"], f32)
    swab = np.zeros((128, 2, 8, 128), f32)
    swab[:, 0] = rel[_t5_bucket(np.maximum(d_prev, 0))].transpose(0, 2, 1)
    swab[:, 1] = rel[_t5_bucket(np.maximum(d_cur, 0))].transpose(0, 2, 1)
    gT = np.zeros((128, 32), f32)
    for l in range(DEPTH):
        gT[:, l * 16:l * 16 + 8] = np.asarray(inp["attn_norm"][l], f32).reshape(8, 128).T
        gT[:, l * 16 + 8:l * 16 + 16] = np.asarray(inp["ffn_norm"][l], f32).reshape(8, 128).T
    rowp = np.zeros((1, DEPTH * ROWP), f32)
    for l in range(DEPTH):
        o = l * ROWP
        rowp[0, o + RP_FQG:o + RP_FQG + 64] = inp["fox_q_gain"][l]
        rowp[0, o + RP_FKG:o + RP_FKG + 64] = inp["fox_k_gain"][l]
        rowp[0, o + RP_SQG:o + RP_SQG + 64] = inp["swa_q_gain"][l]
        rowp[0, o + RP_SKG:o + RP_SKG + 64] = inp["swa_k_gain"][l]
        rowp[0, o + RP_MHG:o + RP_MHG + 256] = inp["mlstm_h_gain"][l]
        rowp[0, o + RP_FFB:o + RP_FFB + 4] = inp["fox_f_bias"][l]
        rowp[0, o + RP_MIB:o + RP_MIB + 4] = inp["mlstm_i_bias"][l]
        rowp[0, o + RP_MFB:o + RP_MFB + 4] = inp["mlstm_f_bias"][l]
        rowp[0, o + RP_SNK:o + RP_SNK + 8] = inp["swa_sinks"][l]
    convw = np.zeros((128, 12), f32)
    cw = np.asarray(inp["conv_w"], f32)
    for l in range(DEPTH):
        for cc in range(2):
            for k in range(3):
                convw[:, l * 6 + cc * 3 + k] = cw[l, k, cc * 128:(cc + 1) * 128]
    return {"cst": cst, "swab": swab, "swam": swam, "gT": gT, "rowp": rowp, "convw": convw}


_NC_CACHE = {}


def kernel(**inputs):
    inp = {k: np.asarray(v) for k, v in inputs.items()}
    x = np.ascontiguousarray(inp["x"], dtype=np.float32)
    n_cores = 8
    key = "full"
    if key not in _NC_CACHE:
        _NC_CACHE[key] = build()
    nc = _NC_CACHE[key]
    shared = host_consts(inp)
    for name, r, c in WEIGHTS:
        shared[name] = np.ascontiguousarray(inp[name], dtype=np.float32)
    in_maps = []
    for c in range(n_cores):
        m = dict(shared)
        m["x"] = x[2 * c:2 * c + 2].reshape(2 * SEQ, D)
        in_maps.append(m)
    res = run_bass_kernel_spmd(nc, in_maps, core_ids=list(range(n_cores)))
    out = np.stack([r["out"].reshape(2, SEQ, D) for r in res.results], axis=0).reshape(16, SEQ, D)
    return out.astype(np.float32)
```

```python
import contextlib
import math
import numpy as np
import concourse.bass as bass
import concourse.mybir as mybir
from concourse.bass_utils import run_bass_kernel_spmd

F32 = mybir.dt.float32
BF16 = mybir.dt.bfloat16
ALU = mybir.AluOpType
AF = mybir.ActivationFunctionType
AX = mybir.AxisListType

D = 1024
SEQ = 2048
DEPTH = 2
DFF = 2816
INC = 7436
EPS = 1e-6
NEG = -30000.0
C_FOX, C_CONV, C_ML, C_SWA, C_GATE = 0, 772, 1540, 2572, 3340

COMPUTE = ("pe", "act", "dve", "pool")
ISSUERS = ("pe", "act", "dve", "pool", "sp")


class Op:
    __slots__ = ("eng", "fn", "waits", "marked", "idx", "vc", "dma_sem", "dma_val", "count", "phase")

    def __init__(self, eng, fn):
        self.eng = eng
        self.fn = fn
        self.waits = []
        self.marked = False
        self.idx = -1
        self.vc = None
        self.dma_sem = None
        self.dma_val = 0
        self.count = 0


class Prog:
    def __init__(self):
        self.ops = {e: [] for e in ISSUERS}
        self.last_w = {}
        self.readers = {}
        self.known = {e: {} for e in ISSUERS}
        self.known_dma = {e: {} for e in ISSUERS}
        self.dma_count = {}
        self.phase = "init"
        self.annotate = False

    def _dep(self, op, prod):
        if prod is None or prod is op:
            return
        A = op.eng
        if prod.dma_sem is not None:
            val = self.dma_count[prod.dma_sem]
            if self.known_dma[A].get(prod.dma_sem, 0) >= val:
                return
            self.known_dma[A][prod.dma_sem] = val
            op.waits.append((prod, val))
        else:
            B = prod.eng
            if B == "pe" and A == "pe":
                return
            if self.known[A].get(B, -1) >= prod.idx:
                return
            op.waits.append((prod, None))
            prod.marked = True
        for b, n in prod.vc[0].items():
            if self.known[A].get(b, -1) < n:
                self.known[A][b] = n
        for s, v in prod.vc[1].items():
            if self.known_dma[A].get(s, 0) < v:
                self.known_dma[A][s] = v

    def last(self, eng):
        return self.ops[eng][-1] if self.ops[eng] else None

    def emit(self, eng, fn, reads=(), writes=(), dma_sem=None, after=()):
        op = Op(eng, fn)
        op.phase = self.phase
        op.idx = len(self.ops[eng])
        for r in reads:
            self._dep(op, self.last_w.get(r))
        for w in writes:
            self._dep(op, self.last_w.get(w))
            for rd in self.readers.get(w, ()):
                self._dep(op, rd)
        for a in after:
            self._dep(op, a)
        if dma_sem is not None:
            op.dma_sem = dma_sem
            self.dma_count[dma_sem] = self.dma_count.get(dma_sem, 0) + 16
            op.dma_val = self.dma_count[dma_sem]
        kc = dict(self.known[eng])
        if dma_sem is None:
            kc[eng] = op.idx
            op.vc = (kc, dict(self.known_dma[eng]))
        else:
            kd = dict(self.known_dma[eng])
            kd[dma_sem] = op.dma_val
            op.vc = (kc, kd)
        for r in reads:
            self.readers.setdefault(r, []).append(op)
        for w in writes:
            self.last_w[w] = op
            self.readers[w] = []
        self.ops[eng].append(op)
        return op

    def finalize(self, nc):
        for e in ISSUERS:
            c = 0
            for op in self.ops[e]:
                if op.dma_sem is None and op.marked:
                    c += 1
                op.count = c
        with contextlib.ExitStack() as st:
            esem = {e: st.enter_context(nc.semaphore("s_" + e)) for e in COMPUTE}
            dsem = {k: st.enter_context(nc.semaphore("d_" + str(k))) for k in self.dma_count}
            block = st.enter_context(nc.Block())

            def run(e, engine):
                for op in self.ops[e]:
                    for p, val in op.waits:
                        if p.dma_sem is not None:
                            engine.wait_ge(dsem[p.dma_sem], val)
                        else:
                            engine.wait_ge(esem[p.eng], p.count)
                    ins = op.fn(engine)
                    if self.annotate:
                        ins.annotate(op.phase)
                    if op.dma_sem is not None:
                        ins.then_inc(dsem[op.dma_sem], 16)
                    elif op.marked:
                        ins.then_inc(esem[e], 1)

            @block.tensor
            def _(eng):
                run("pe", eng)

            @block.scalar
            def _(eng):
                run("act", eng)

            @block.vector
            def _(eng):
                run("dve", eng)

            @block.gpsimd
            def _(eng):
                run("pool", eng)

            @block.sync
            def _(eng):
                run("sp", eng)


WEIGHTS = [
    ("w_in", D, INC), ("w_fox_out", 256, D), ("w_conv_out", 256, D), ("w_mlstm_out", 256, D),
    ("w_swa_out", 512, D), ("w_merge_out", D, D), ("w_gate", D, DFF), ("w_up", D, DFF),
    ("w_down", DFF, D),
]
ROWP = 532
RP_FQG, RP_FKG, RP_SQG, RP_SKG, RP_MHG, RP_FFB, RP_MIB, RP_MFB, RP_SNK = 0, 64, 128, 192, 256, 512, 516, 520, 524


class _Stop(Exception):
    pass


_LAST = {}


def build(n_seq=2, n_tiles=8, n_layers=2, NSUB=2, seq_len=SEQ, stop_at=99, do_cast=True, annotate=False):
    T = NSUB * 128
    nc = bass.Bass("TRN2", target_bir_lowering=False)
    NTOK = n_seq * seq_len
    x_d = nc.dram_tensor("x", [NTOK, D], F32, kind="ExternalInput").ap()
    out_d = nc.dram_tensor("out", [NTOK, D], F32, kind="ExternalOutput").ap()
    wf = {}
    wb = {}
    for name, r, c in WEIGHTS:
        wf[name] = nc.dram_tensor(name, [DEPTH, r, c], F32, kind="ExternalInput").ap()
    cst_d = nc.dram_tensor("cst", [128, 6, 128], F32, kind="ExternalInput").ap()
    swab_d = nc.dram_tensor("swab", [128, 2, 8, 128], F32, kind="ExternalInput").ap()
    swam_d = nc.dram_tensor("swam", [128, 2, 128], F32, kind="ExternalInput").ap()
    gT_d = nc.dram_tensor("gT", [128, 32], F32, kind="ExternalInput").ap()
    rowp_d = nc.dram_tensor("rowp", [1, DEPTH * ROWP], F32, kind="ExternalInput").ap()
    convw_d = nc.dram_tensor("convw", [128, 12], F32, kind="ExternalInput").ap()
    gcol_d = nc.dram_tensor("gcol", [128, 8], F32, kind="ExternalInput").ap()

    P = Prog()
    P.annotate = annotate
    st = contextlib.ExitStack()
    with st:
        def sb(name, shape, dt):
            return st.enter_context(nc.sbuf_tensor("s_" + name, shape, dt))

        def psum(name, shape, dt):
            return st.enter_context(nc.psum_tensor("p_" + name, shape, dt))

        def tt(eng, out, in0, in1, op, R, W):
            return P.emit(eng, lambda e: e.tensor_tensor(out=out, in0=in0, in1=in1, op=op), R, W)

        def ts(eng, out, in0, s1, s2, op0, op1, R, W):
            if s2 is None:
                return P.emit(eng, lambda e: e.tensor_scalar(out=out, in0=in0, scalar1=s1, scalar2=None, op0=op0), R, W)
            return P.emit(eng, lambda e: e.tensor_scalar(out=out, in0=in0, scalar1=s1, scalar2=s2, op0=op0, op1=op1), R, W)

        def stt(eng, out, in0, scalar, in1, op0, op1, R, W):
            return P.emit(eng, lambda e: e.scalar_tensor_tensor(out=out, in0=in0, scalar=scalar, in1=in1, op0=op0, op1=op1), R, W)

        def cp(eng, out, in_, R, W):
            if eng == "act":
                return P.emit("act", lambda e: e.copy(out=out, in_=in_), R, W)
            return P.emit(eng, lambda e: e.tensor_copy(out=out, in_=in_), R, W)

        def act(out, in_, func, R, W, bias=None, scale=1.0, accum=None):
            kw = {}
            if bias is not None:
                kw["bias"] = bias
            if accum is not None:
                kw["accum_out"] = accum
            return P.emit("act", lambda e: e.activation(out=out, in_=in_, func=func, scale=scale, **kw), R, W)

        def mm(out, lhsT, rhs, start, stop, R, W):
            return P.emit("pe", lambda e: e.matmul(out, lhsT=lhsT, rhs=rhs, start=start, stop=stop), R, W)

        def tr(out, in_, R, W):
            return P.emit("pe", lambda e: e.transpose(out=out, in_=in_, identity=identb[:]), list(R) + ["identb"], W)

        def mset(eng, ap, val, W):
            return P.emit(eng, lambda e: e.memset(ap, val), (), W)

        def recip(out, in_, R, W):
            return P.emit("dve", lambda e: e.reciprocal(out=out, in_=in_), R, W)

        def dma(eng, out, in_, R, W, sem):
            return P.emit(eng, lambda e: e.dma_start(out=out, in_=in_), R, W, dma_sem=sem)

        class Rot:
            def __init__(self, items):
                self.items = items
                self.i = 0

            def next(self):
                it = self.items[self.i % len(self.items)]
                self.i += 1
                return it

        mmR = Rot([(psum("mm%d" % i, [128, 512], F32), "mm%d" % i) for i in range(2)])
        tpR = Rot([(psum("tp%d" % i, [128, 8, 128], BF16), "tp%d" % i) for i in range(2)])
        scR = Rot([(psum("sc%d" % i, [128, 512], F32), "sc%d" % i) for i in range(2)])
        aoR = Rot([(psum("ao%d" % i, [128, 512], F32), "ao%d" % i) for i in range(2)])
        denseR = Rot(mmR.items + scR.items + aoR.items)
        foxR = Rot([mmR.items[1], scR.items[0]])
        attR = Rot([scR.items[1]])
        projR = Rot(mmR.items + scR.items)

        cst = sb("cst", [128, 6, 128], F32)
        identb = sb("identb", [128, 128], BF16)
        m01b = sb("m01b", [128, 128], BF16)
        chunkind = sb("chunkind", [128, 2, 64], F32)
        bm = sb("bm", [128, 2, 8, 128], F32)
        swam = sb("swam", [128, 2, 128], F32)
        gT = sb("gT", [128, 32], F32)
        rowp = sb("rowp", [128, DEPTH * ROWP], F32)
        convw = sb("convw", [128, 12], F32)
        gcol = sb("gcol", [128, 8], F32)
        expsink = sb("expsink", [128, DEPTH, 8], F32)
        tri, triblk, cmaskT, ones = cst[:, 1, :], cst[:, 2, :], cst[:, 3, :], cst[:, 4, :]

        dma("sp", cst[:], cst_d, (), ["cst"], "c0")
        dma("sp", bm[:], swab_d, (), ["bm"], "c1")
        dma("sp", swam[:], swam_d, (), ["swam"], "c2")
        dma("sp", gT[:], gT_d, (), ["gT"], "c3")
        dma("sp", rowp[:], rowp_d.partition_broadcast(128), (), ["rowp"], "c4")
        dma("sp", convw[:], convw_d, (), ["convw"], "c5")
        dma("sp", gcol[:], gcol_d, (), ["gcol"], "c6")
        cp("dve", identb[:], cst[:, 0, :], ["cst"], ["identb"])
        cp("dve", m01b[:], cst[:, 5, :], ["cst"], ["m01b"])
        mset("pool", chunkind[:], 0.0, ["chunkind"])
        mset("pool", chunkind[0:64, 0, :], 1.0, ["chunkind"])
        mset("pool", chunkind[64:128, 1, :], 1.0, ["chunkind"])
        for blk in range(2):
            tt("dve", bm[:, blk, :, :], bm[:, blk, :, :], swam[:, blk, :].unsqueeze(1).to_broadcast([128, 8, 128]),
               ALU.add, ["bm", "swam"], ["bm"])
        for l in range(DEPTH):
            act(expsink[:, l, :], rowp[:, l * ROWP + RP_SNK: l * ROWP + RP_SNK + 8], AF.Exp, ["rowp"], ["expsink"])

        NSLOT = 5
        CAST_AHEAD = 8
        wslots = [sb("wslot%d" % i, [128, 8, 512], BF16) for i in range(NSLOT)]

        def layer_chunks(l):
            ch = []

            def add(name, r0, nrows, c0, ncol):
                ch.append((name, l, r0, nrows, c0, ncol, nrows // 128))

            def win(c0, ncol):
                add("w_in", 0, D, c0, ncol)

            win(C_FOX, 512)
            win(C_FOX + 512, 260)
            win(C_SWA + 512, 256)
            win(C_SWA, 512)
            win(C_ML + 512, 264)
            win(C_ML + 776, 256)
            win(C_ML, 512)
            win(C_CONV, 512)
            win(C_CONV + 512, 256)
            for b_, (nm, rows) in enumerate([("w_fox_out", 256), ("w_conv_out", 256), ("w_mlstm_out", 256), ("w_swa_out", 512)]):
                add(nm, 0, rows, 0, 1024)
                win(C_GATE + b_ * 1024, 512)
                win(C_GATE + b_ * 1024 + 512, 512)
            for h in range(2):
                add("w_merge_out", 0, D, h * 512, 512)
            for c in range(6):
                ncol = min(512, DFF - c * 512)
                add("w_gate", 0, D, c * 512, ncol)
                add("w_up", 0, D, c * 512, ncol)
            for h in range(2):
                for c in range(3):
                    k0 = c * 8
                    nk = min(8, 22 - k0)
                    add("w_down", k0 * 128, nk * 128, h * 512, 512)
            return ch

        first_pass = []
        for l in range(n_layers):
            first_pass.extend(layer_chunks(l))
        NFP = len(first_pass)
        scr = [nc.dram_tensor("scr%d" % k, [128, ch[6] * ch[5]], BF16, kind="Internal").ap() for k, ch in enumerate(first_pass)]
        n_pass = n_seq * n_tiles

        class WStream:
            def __init__(self):
                self.issued = 0
                self.taken = 0
                self.holds = {}
                self.cast_next = 0
                self.total = NFP * n_pass

            def _cast_upto(self, k_end):
                while self.cast_next < min(k_end, NFP):
                    k = self.cast_next
                    name, l, r0, nr, c0, nc_, nk = first_pass[k]
                    if do_cast:
                        dma("pool", scr[k].rearrange("p (k c) -> k p c", c=nc_),
                            wf[name][l, r0:r0 + nr, c0:c0 + nc_].rearrange("(k p) c -> k p c", p=128), (), [("wbc", k)], "cast%d" % k)
                    self.cast_next += 1

            def _view(self, n):
                name, l, r0, nr, c0, ncol, nk = first_pass[n % NFP]
                slot = n % NSLOT
                return wslots[slot][:].rearrange("p k c -> p (k c)")[:, 0:nk * ncol].rearrange("p (k c) -> p k c", c=ncol)

            def _issue(self, n):
                k = n % NFP
                name, l, r0, nr, c0, ncol, nk = first_pass[k]
                self._cast_upto(k + 1 + (CAST_AHEAD if n < NFP else 0))
                flat = wslots[n % NSLOT][:].rearrange("p k c -> p (k c)")[:, 0:nk * ncol]
                dma("sp", flat, scr[k], [("wbc", k)] if do_cast else [], [("ws", n % NSLOT)], "ws%d" % (n % NSLOT))

            def next(self, expect, hold=0):
                m = self.taken
                while self.issued < self.total and self.issued < m + NSLOT:
                    k = self.issued
                    prev = k - NSLOT
                    if prev >= 0 and not (prev + 1 + self.holds[prev] <= m):
                        break
                    self._issue(k)
                    self.issued += 1
                n = self.taken
                assert self.issued > n
                self.holds[n] = hold
                self.taken += 1
                assert first_pass[n % NFP][0] == expect, (first_pass[n % NFP][0], expect)
                return self._view(n), ("ws", n % NSLOT)

        WS = WStream()

        xt = sb("xt", [128, NSUB, D], F32)
        hT = sb("hT", [128, 8, T], BF16)
        hb = sb("hb", [128, D], BF16)
        small = sb("small", [128, 64], F32)
        stages = [sb("stage%d" % i, [128, 512], F32) for i in range(3)]
        stR = Rot([(stages[i], "stage%d" % i) for i in range(3)])
        sqtmp = sb("sqtmp", [128, 512], F32)
        nrm_ss = sb("nrm_ss", [128, 8], F32)
        qkn_l = [sb("qkn%d" % i, [128, 512], BF16) for i in range(NSUB)]
        fqT = sb("fqT", [128, 2, T], BF16)
        fkT = [sb("fkT%d" % l, [128, 2, seq_len], BF16) for l in range(n_layers)]
        fV = [sb("fV%d" % l, [128, seq_len // 128, 4, 65], BF16) for l in range(n_layers)]
        fnegc = [sb("fnegc%d" % l, [128, seq_len // 128, 4], F32) for l in range(n_layers)]
        fncar = [sb("fncar%d" % l, [128, NSUB + 1, 4], F32) for l in range(n_layers)]
        fsp = sb("fsp", [128, NSUB, 4], F32)
        fxf = sb("fxf", [128, 4], F32)
        fD = sb("fD", [128, 128], F32)
        fcbc = [sb("fcbc%d" % i, [128, T], F32) for i in range(4)]
        fcbm = [sb("fcbm%d" % i, [128, T], F32) for i in range(4)]
        atmp = [sb("atmp%d" % i, [128, 512], F32) for i in range(2)]
        apT = [sb("apT%d" % i, [128, 512], BF16) for i in range(2)]
        frec = sb("frec", [128, 8], F32)
        yfox = sb("yfox", [128, NSUB, 256], BF16)
        sqT = [sb("sqT%d" % i, [64, 8, 128], BF16) for i in range(NSUB)]
        swtmp = [sb("swtmp%d" % i, [128, 512], F32) for i in range(2)]
        swpT = [sb("swpT%d" % i, [128, 512], BF16) for i in range(2)]
        skT = [sb("skT%d" % l, [64, NSUB + 1, 2, 128], BF16) for l in range(n_layers)]
        sV = [sb("sV%d" % l, [128, NSUB + 1, 2, 65], BF16) for l in range(n_layers)]
        skst = sb("skst", [128, 128], F32)
        skn = sb("skn", [128, 128], BF16)
        yswa = [sb("yswa%d" % i, [128, 512], BF16) for i in range(2)]
        sden = sb("sden", [128, 4], F32)
        mV = sb("mV", [128, NSUB, 4, 65], BF16)
        mif = sb("mif", [128, NSUB, 8], F32)
        msp = sb("msp", [128, NSUB, 4], F32)
        mea = sb("mea", [128, NSUB, 4], F32)
        meb = sb("meb", [128, NSUB, 4], F32)
        mdec = sb("mdec", [64, NSUB, 2, 4], F32)
        mso = sb("mso", [128, NSUB, 256], F32)
        mkt = [sb("mkt%d" % i, [128, 256], BF16) for i in range(NSUB)]
        mqb_l = [sb("mqb%d" % i, [128, 256], BF16) for i in range(NSUB)]
        mqT = [sb("mqT%d" % i, [64, 4, 128], BF16) for i in range(NSUB)]
        mq0T = [sb("mq0T%d" % i, [64, 4, 128], BF16) for i in range(NSUB)]
        mq1T = [sb("mq1T%d" % i, [64, 4, 128], BF16) for i in range(NSUB)]
        mkT = [sb("mkT%d" % i, [64, 4, 128], BF16) for i in range(NSUB)]
        mPT = sb("mPT", [128, 4, 128], BF16)
        mCf = [sb("mCf%d" % l, [64, 4, 65], F32) for l in range(n_layers)]
        mCb = [[sb("mCb%d_%d" % (l, i), [64, 4, 65], BF16) for i in range(2)] for l in range(n_layers)]
        mt1 = sb("mt1", [128, 8], F32)
        mp2 = sb("mp2", [128, 16], F32)
        mhm = sb("mhm", [128, 256], F32)
        ymls = sb("ymls", [128, 256], BF16)
        cu = sb("cu", [128, 2, T], F32)
        cbt = sb("cbt", [128, 2, T], F32)
        cz = [sb("cz%d" % l, [128, 2, T + 2], F32) for l in range(n_layers)]
        cy = sb("cy", [128, T], F32)
        yT = sb("yT", [128, 10, T], BF16)
        sg = [sb("sg%d" % i, [128, 512], F32) for i in range(2)]
        mtmp = sb("mtmp", [128, 512], F32)
        merged = sb("merged", [128, NSUB, D], F32)
        actT = sb("actT", [128, 22, T], BF16)

        def rp(l, off, n):
            return rowp[:, l * ROWP + off: l * ROWP + off + n]

        for a in range(NSUB):
            mset("pool", mq0T[a][:], 0.0, [("mq0T", a)])
            mset("pool", mq1T[a][:], 0.0, [("mq1T", a)])
        mset("pool", mV[:], 1.0, ["mV"])
        for l in range(n_layers):
            mset("pool", fV[l][:], 1.0, [("fV", l)])
            mset("pool", sV[l][:], 1.0, [("sV", l)])

        def head_norm(src, nh, gain_views, out_bf, srcR, outW, out_scale=1.0):
            w = nh * 64
            tt("pool", sqtmp[:, 0:w], src, src, ALU.mult, srcR, ["sqtmp"])
            P.emit("dve", lambda e: e.tensor_reduce(out=nrm_ss[:, 0:nh], in_=sqtmp[:, 0:w].rearrange("p (h d) -> p h d", d=64),
                                                     axis=AX.X, op=ALU.add), ["sqtmp"], ["nrm_ss"])
            act(nrm_ss[:, 0:nh], nrm_ss[:, 0:nh], AF.Ln, ["nrm_ss"], ["nrm_ss"], bias=EPS, scale=1.0 / 64)
            act(nrm_ss[:, 0:nh], nrm_ss[:, 0:nh], AF.Exp, ["nrm_ss"], ["nrm_ss"], scale=-0.5)
            if gain_views is None:
                tt("dve", out_bf[:, 0:w].rearrange("p (h d) -> p h d", d=64), src.rearrange("p (h d) -> p h d", d=64),
                   nrm_ss[:, 0:nh].unsqueeze(2).to_broadcast([128, nh, 64]), ALU.mult, list(srcR) + ["nrm_ss"], outW)
                return
            tt("dve", sqtmp[:, 0:w].rearrange("p (h d) -> p h d", d=64), src.rearrange("p (h d) -> p h d", d=64),
               nrm_ss[:, 0:nh].unsqueeze(2).to_broadcast([128, nh, 64]), ALU.mult, list(srcR) + ["nrm_ss"], ["sqtmp"])
            if gain_views is None:
                return
            for h0, h1, g in gain_views:
                n = h1 - h0
                tt("pool", out_bf[:, h0 * 64:h1 * 64].rearrange("p (h d) -> p h d", d=64),
                   sqtmp[:, h0 * 64:h1 * 64].rearrange("p (h d) -> p h d", d=64),
                   g.unsqueeze(1).to_broadcast([128, n, 64]), ALU.mult, ["sqtmp", "rowp"], outW)

        def resid_norm(l, which):
            for a in range(NSUB):
                mset("pool", small[:, 0:1], 0.0, ["small0"])
                act(hb[:], xt[:, a, :], AF.Square, [("xt", a), "small0"], ["hb", "small0"], accum=small[:, 0:1])
                act(small[:, 1:2], small[:, 0:1], AF.Ln, ["small0"], ["small1"], bias=EPS, scale=1.0 / D)
                act(small[:, 2:3], small[:, 1:2], AF.Exp, ["small1"], ["small2"], scale=-0.5)
                ts("dve", hb[:], xt[:, a, :], small[:, 2:3], None, ALU.mult, None, [("xt", a), "small2"], ["hb"])
                tp, tpk = tpR.next()
                for kc in range(8):
                    tr(tp[:, kc, :], hb[:, kc * 128:(kc + 1) * 128], ["hb"], [tpk])
                g0 = l * 16 + which * 8
                tt("dve", hT[:, :, a * 128:(a + 1) * 128], tp[:, :, :],
                   gT[:, g0:g0 + 8].unsqueeze(2).to_broadcast([128, 8, 128]), ALU.mult, [tpk, "gT"], [("hT", a)])

        HT_ALL = [("hT", a) for a in range(NSUB)]

        def proj_tok(W, wk, ncol, a, rot=None):
            ps, pk = (rot or projR).next()
            for kc in range(8):
                mm(ps[:, 0:ncol], hT[:, kc, a * 128:(a + 1) * 128], W[:, kc, :], kc == 0, kc == 7, [("hT", a), wk], [pk])
            return ps, pk

        def main_loop():
          for s in range(n_seq):
            for t in range(n_tiles):
                tok0 = s * seq_len + t * T
                blk0 = t * NSUB
                first_tile = (t == 0)
                for a in range(NSUB):
                    dma("sp", xt[:, a, :], x_d[tok0 + a * 128: tok0 + (a + 1) * 128, :], (), [("xt", a)], "xt")
                for l in range(n_layers):
                    if first_tile:
                        mset("pool", fncar[l][:, 0, :], 0.0, [("fncar", l)])
                        mset("pool", mCf[l][:], 0.0, [("mCf", l)])
                        mset("pool", mCb[l][0][:], 0.0, [("mCb", l, 0)])
                        mset("pool", cz[l][:, :, 0:2], 0.0, [("cz", l)])
                    if stop_at <= 0:
                        raise _Stop()
                    deferred = []

                    def flush():
                        while deferred:
                            deferred.pop(0)()
                    P.phase = "norm1"
                    resid_norm(l, 0)

                    if stop_at <= 1:
                        raise _Stop()
                    P.phase = "fox_proj"
                    W, wk = WS.next("w_in")
                    for a in range(NSUB):
                        ps, pk = proj_tok(W, wk, 512, a)
                        stg, sk = stR.next()
                        cp("act", stg[:], ps[:, 0:512], [pk], [sk])
                        if stop_at <= 1.1:
                            raise _Stop()
                        qkn, qknk = qkn_l[a], "qkn%d" % a
                        head_norm(stg[:], 8, None, qkn, [sk], [qknk])

                        def fox_tr(a=a, qkn=qkn, qknk=qknk):
                            tp, tpk = tpR.next()
                            for k in range(4):
                                tr(tp[:, k, :], qkn[:, k * 128:(k + 1) * 128], [qknk], [tpk])
                            act(fqT[:, :, a * 128:(a + 1) * 128], tp[:, 0:2, :], AF.Copy, [tpk, "gcol"], ["fqT"], scale=gcol[:, l * 4 + 0:l * 4 + 1])
                            act(fkT[l][:, :, (blk0 + a) * 128:(blk0 + a + 1) * 128], tp[:, 2:4, :], AF.Copy, [tpk, "gcol"], [("fkT", l)],
                                scale=gcol[:, l * 4 + 1:l * 4 + 2])
                        deferred.append(fox_tr)
                    if stop_at <= 1.3:
                        raise _Stop()
                    W, wk = WS.next("w_in")
                    for a in range(NSUB):
                        ps, pk = proj_tok(W, wk, 260, a)
                        cp("dve", fV[l][:, blk0 + a, :, 0:64], ps[:, 0:256].rearrange("p (h d) -> p h d", d=64), [pk], [("fV", l)])
                        if stop_at <= 1.4:
                            raise _Stop()
                        tt("dve", fxf[:], ps[:, 256:260], rp(l, RP_FFB, 4), ALU.add, [pk, "rowp"], ["fxf"])
                        act(fxf[:], fxf[:], AF.Exp, ["fxf"], ["fxf"], scale=-1.0)
                        act(fsp[:, a, :], fxf[:], AF.Ln, ["fxf"], [("fsp", a)], bias=1.0)
                        if stop_at <= 1.5:
                            raise _Stop()

                        def fox_cum(a=a):
                            p2, p2k = aoR.next()
                            mm(p2[:, 0:4], tri, fsp[:, a, :], True, True, ["cst", ("fsp", a)], [p2k])
                            mm(p2[:, 4:8], ones, fsp[:, a, :], True, True, ["cst", ("fsp", a)], [p2k])
                            tt("dve", fnegc[l][:, blk0 + a, :], p2[:, 0:4], fncar[l][:, a, :], ALU.add, [p2k, ("fncar", l)], [("fnegc", l)])
                            tt("dve", fncar[l][:, a + 1, :], p2[:, 4:8], fncar[l][:, a, :], ALU.add, [p2k, ("fncar", l)], [("fncar", l)])
                        deferred.append(fox_cum)

                    if stop_at <= 2:
                        flush()
                        raise _Stop()
                    P.phase = "swa_proj"
                    W, wk = WS.next("w_in")
                    for a in range(NSUB):
                        ps, pk = proj_tok(W, wk, 256, a)
                        cp("act", skst[:], ps[:, 0:128], [pk], ["skst"])
                        cp("act", sV[l][:, a + 1, :, 0:64], ps[:, 128:256].rearrange("p (h d) -> p h d", d=64), [pk], [("sV", l)])
                        head_norm(skst[:], 2, None, skn, ["skst"], ["skn"])
                        tp, tpk = tpR.next()
                        for g in range(2):
                            tr(tp[0:64, g, :], skn[:, g * 64:(g + 1) * 64], ["skn"], [tpk])
                        ts("dve", skT[l][:, a + 1, :, :], tp[0:64, 0:2, :], gcol[0:64, l * 4 + 3:l * 4 + 4], None, ALU.mult, None, [tpk, "gcol"], [("skT", l)])
                    W, wk = WS.next("w_in")
                    for a in range(NSUB):
                        ps, pk = proj_tok(W, wk, 512, a)
                        if a == 0:
                            flush()
                        stg, sk = stR.next()
                        cp("act", stg[:], ps[:, 0:512], [pk], [sk])
                        qkn, qknk = qkn_l[a], "qkn%d" % a
                        head_norm(stg[:], 8, None, qkn, [sk], [qknk])

                        def swa_tr(a=a, qkn=qkn, qknk=qknk):
                            qb_, qbk = sqT[a], "sqT%d" % a
                            for half in range(2):
                                tp, tpk = tpR.next()
                                for k in range(4):
                                    hh = half * 4 + k
                                    tr(tp[0:64, k, :], qkn[:, hh * 64:(hh + 1) * 64], [qknk], [tpk])
                                if half:
                                    act(qb_[:, 4:8, :], tp[0:64, 0:4, :], AF.Copy, [tpk, "gcol"], [qbk], scale=gcol[0:64, l * 4 + 2:l * 4 + 3])
                                else:
                                    ts("dve", qb_[:, 0:4, :], tp[0:64, 0:4, :], gcol[0:64, l * 4 + 2:l * 4 + 3], None, ALU.mult, None,
                                       [tpk, "gcol"], [qbk])
                        deferred.append(swa_tr)

                    P.phase = "mlstm_proj"
                    W, wk = WS.next("w_in")
                    for a in range(NSUB):
                        ps, pk = proj_tok(W, wk, 264, a)
                        cp("dve", mV[:, a, :, 0:64], ps[:, 0:256].rearrange("p (h d) -> p h d", d=64), [pk], ["mV"])
                        tt("dve", mif[:, a, 0:4], ps[:, 256:260], rp(l, RP_MIB, 4), ALU.add, [pk, "rowp"], [("mif", a)])
                        tt("dve", mif[:, a, 4:8], ps[:, 260:264], rp(l, RP_MFB, 4), ALU.add, [pk, "rowp"], [("mif", a)])
                        act(mt1[:, 0:4], mif[:, a, 4:8], AF.Exp, [("mif", a)], ["mt1"], scale=-1.0)
                        act(msp[:, a, :], mt1[:, 0:4], AF.Ln, ["mt1"], [("msp", a)], bias=1.0)

                        def ml_cum(a=a):
                            p2, p2k = aoR.next()
                            mm(p2[:, 0:4], triblk, msp[:, a, :], True, True, ["cst", ("msp", a)], [p2k])
                            for c in range(2):
                                mm(p2[0:64, 8 + 4 * c:12 + 4 * c], chunkind[:, c, :], msp[:, a, :], True, True, ["chunkind", ("msp", a)], [p2k])
                            cp("dve", mp2[:, 0:4], p2[:, 0:4], [p2k], ["mp2"])
                            cp("dve", mp2[0:64, 8:16], p2[0:64, 8:16], [p2k], ["mp2"])
                            tt("dve", mt1[:, 4:8], mp2[:, 0:4], mif[:, a, 0:4], ALU.add, ["mp2", ("mif", a)], ["mt1"])
                            act(mea[:, a, :], mt1[:, 4:8], AF.Exp, ["mt1"], [("mea", a)])
                            act(meb[:, a, :], mp2[:, 0:4], AF.Exp, ["mp2"], [("meb", a)], scale=-1.0)
                            act(mdec[:, a, :, :], mp2[0:64, 8:16].rearrange("p (c h) -> p c h", h=4), AF.Exp, ["mp2"], [("mdec", a)], scale=-1.0)
                        deferred.append(ml_cum)
                    W, wk = WS.next("w_in")
                    for a in range(NSUB):
                        ps, pk = proj_tok(W, wk, 256, a)
                        act(mso[:, a, :], ps[:, 0:256], AF.Exp, [pk], [("mso", a)], scale=-1.0)
                        ts("pool", mso[:, a, :], mso[:, a, :], 1.0, None, ALU.add, None, [("mso", a)], [("mso", a)])
                        recip(mso[:, a, :], mso[:, a, :], [("mso", a)], [("mso", a)])
                    flush()
                    W, wk = WS.next("w_in")
                    for a in range(NSUB):
                        ps, pk = proj_tok(W, wk, 512, a)
                        ts("dve", mqb_l[a][:], ps[:, 0:256], 0.125, None, ALU.mult, None, [pk], [("mqb", a)])
                        tt("dve", mkt[a][:].rearrange("p (h d) -> p h d", d=64), ps[:, 256:512].rearrange("p (h d) -> p h d", d=64),
                           mea[:, a, :].unsqueeze(2).to_broadcast([128, 4, 64]), ALU.mult, [pk, ("mea", a)], [("mkt", a)])

                        def ml_tr(a=a):
                            tq, tqk = tpR.next()
                            for h in range(4):
                                tr(tq[0:64, h, :], mqb_l[a][:, h * 64:(h + 1) * 64], [("mqb", a)], [tqk])
                            tk, tkk = tpR.next()
                            for h in range(4):
                                tr(tk[0:64, h, :], mkt[a][:, h * 64:(h + 1) * 64], [("mkt", a)], [tkk])
                            cp("dve", mqT[a][:], tq[0:64, 0:4, :], [tqk], [("mqT", a)])
                            cp("dve", mq0T[a][:, :, 0:64], tq[0:64, 0:4, 0:64], [tqk], [("mq0T", a)])
                            cp("dve", mq1T[a][:, :, 64:128], tq[0:64, 0:4, 64:128], [tqk], [("mq1T", a)])
                            cp("act", mkT[a][:], tk[0:64, 0:4, :], [tkk], [("mkT", a)])
                        deferred.append(ml_tr)

                    if stop_at <= 5:
                        raise _Stop()
                    P.phase = "conv"
                    W, wk = WS.next("w_in")
                    for cb_ in range(4):
                        ps, pk = projR.next()
                        for kc in range(8):
                            mm(ps[:, 0:T], W[:, kc, cb_ * 128:(cb_ + 1) * 128], hT[:, kc, :], kc == 0, kc == 7, HT_ALL + [wk], [pk])
                        dst = cu if cb_ < 2 else cbt
                        cp("act", dst[:, cb_ % 2, :], ps[:, 0:T], [pk], ["cu" if cb_ < 2 else "cbt"])
                    flush()
                    W, wk = WS.next("w_in")
                    for cc in range(2):
                        ps, pk = projR.next()
                        for kc in range(8):
                            mm(ps[:, 0:T], W[:, kc, cc * 128:(cc + 1) * 128], hT[:, kc, :], kc == 0, kc == 7, HT_ALL + [wk], [pk])
                        z = cz[l]
                        tt("dve", z[:, cc, 2:T + 2], ps[:, 0:T], cu[:, cc, :], ALU.mult, [pk, "cu"], [("cz", l)])
                        w0 = convw[:, l * 6 + cc * 3 + 0: l * 6 + cc * 3 + 1]
                        w1 = convw[:, l * 6 + cc * 3 + 1: l * 6 + cc * 3 + 2]
                        w2 = convw[:, l * 6 + cc * 3 + 2: l * 6 + cc * 3 + 3]
                        ts("pool", cy[:], z[:, cc, 2:T + 2], w2, None, ALU.mult, None, [("cz", l), "convw"], ["cy"])
                        stt("dve", cy[:], z[:, cc, 1:T + 1], w1, cy[:], ALU.mult, ALU.add, [("cz", l), "convw", "cy"], ["cy"])
                        stt("dve", cy[:], z[:, cc, 0:T], w0, cy[:], ALU.mult, ALU.add, [("cz", l), "convw", "cy"], ["cy"])
                        tt("dve", yT[:, 2 + cc, :], cy[:], cbt[:, cc, :], ALU.mult, ["cy", "cbt"], [("yT", 1)])
                        cp("pool", z[:, cc, 0:2], z[:, cc, T:T + 2], [("cz", l)], [("cz", l)])


                    flush()
                    nblk = blk0 + NSUB

                    def fox_prologue(h):
                        cb, cbk = fcbc[h], "fcbc%d" % h
                        cbm, cbmk = fcbm[h], "fcbm%d" % h
                        pc, pck = aoR.next()
                        for a in range(NSUB):
                            ts("dve", fD[:], ones, fsp[:, a, h:h + 1], None, ALU.mult, None, ["cst", ("fsp", a)], ["fD"])
                            mm(pc[:, a * 128:(a + 1) * 128], fD[:], tri, True, True, ["fD", "cst"], [pck])
                        for a in range(NSUB):
                            act(cb[:, a * 128:(a + 1) * 128], pc[:, a * 128:(a + 1) * 128], AF.Identity, [pck, ("fncar", l)], [cbk],
                                bias=fncar[l][:, a, h:h + 1])
                        tt("pool", cbm[:].rearrange("p (a t) -> p a t", t=128), cb[:].rearrange("p (a t) -> p a t", t=128),
                           cmaskT.unsqueeze(1).to_broadcast([128, NSUB, 128]), ALU.subtract, [cbk, "cst"], [cbmk])

                    def gen_fox():
                        P.phase = "fox_attn"
                        for h in range(4):
                            fox_prologue(h)
                        yield
                        acc, acck = mmR.items[0]
                        accv = acc[:, 0:NSUB * 65].rearrange("p (a c) -> p a c", c=65)
                        units = [(h, j) for h in range(4) for j in range(nblk)]
                        scs = {}

                        def s_qk(n):
                            h, j = units[n]
                            hp, hbase = h // 2, (h % 2) * 64
                            c0 = max(0, j - blk0) * 128
                            sc, sck = foxR.next()
                            scs[n] = (sc, sck)
                            mm(sc[:, c0:T], fkT[l][hbase:hbase + 64, hp, j * 128:(j + 1) * 128], fqT[hbase:hbase + 64, hp, c0:T],
                               True, True, [("fkT", l), "fqT"], [sck])

                        def s_dve(n):
                            h, j = units[n]
                            cb, cbk, cbm, cbmk = fcbc[h], "fcbc%d" % h, fcbm[h], "fcbm%d" % h
                            c0 = max(0, j - blk0) * 128
                            sc, sck = scs.pop(n)
                            bi = n % 2
                            tmp, tmpk = atmp[bi], "atmp%d" % bi
                            if j >= blk0:
                                stt("dve", tmp[:, c0:c0 + 128], sc[:, c0:c0 + 128], 0.125, cbm[:, c0:c0 + 128], ALU.mult, ALU.subtract,
                                    [sck, cbmk], [tmpk])
                                if c0 + 128 < T:
                                    stt("dve", tmp[:, c0 + 128:T], sc[:, c0 + 128:T], 0.125, cb[:, c0 + 128:T], ALU.mult, ALU.subtract,
                                        [sck, cbk], [tmpk])
                            else:
                                stt("dve", tmp[:, c0:T], sc[:, c0:T], 0.125, cb[:, c0:T], ALU.mult, ALU.subtract, [sck, cbk], [tmpk])

                        def s_act(n):
                            h, j = units[n]
                            c0 = max(0, j - blk0) * 128
                            bi = n % 2
                            act(apT[bi][:, c0:T], atmp[bi][:, c0:T], AF.Exp, ["atmp%d" % bi, ("fnegc", l)], ["apT%d" % bi],
                                bias=fnegc[l][:, j, h:h + 1])

                        def s_pv(n):
                            h, j = units[n]
                            a0 = max(0, j - blk0)
                            bi = n % 2
                            pT, pTk = apT[bi], "apT%d" % bi
                            for a in range(a0, NSUB):
                                mm(accv[:, a, :], pT[:, a * 128:(a + 1) * 128], fV[l][:, j, h, :], (j == 0 and a == 0),
                                   (j == nblk - 1 and a == NSUB - 1), [pTk, ("fV", l)], [acck])
                            if j == nblk - 1:
                                recip(frec[:, 0:NSUB], accv[:, :, 64], [acck], ["frec"])
                                tt("dve", yfox[:, :, h * 64:(h + 1) * 64], accv[:, :, 0:64],
                                   frec[:, 0:NSUB].unsqueeze(2).to_broadcast([128, NSUB, 64]), ALU.mult, [acck, "frec"], ["yfox"])

                        NU = len(units)
                        for step in range(NU + 3):
                            P.phase = "fox_attn"
                            if 0 <= step - 3 < NU:
                                s_pv(step - 3)
                            if 0 <= step - 2 < NU:
                                s_act(step - 2)
                            if 0 <= step - 1 < NU:
                                s_dve(step - 1)
                            if step < NU:
                                s_qk(step)
                            yield
                        P.phase = "fox_attn"
                        cp("pool", fncar[l][:, 0, :], fncar[l][:, NSUB, :], [("fncar", l)], [("fncar", l)])
                        for a in range(NSUB):
                            tp, tpk = tpR.next()
                            for k in range(2):
                                tr(tp[:, k, :], yfox[:, a, k * 128:(k + 1) * 128], ["yfox"], [tpk])
                            cp("act", yT[:, 0:2, a * 128:(a + 1) * 128], tp[:, 0:2, :], [tpk], [("yT", 0)])

                    def gen_swa():
                        for a in range(NSUB):
                            qb_, qbk = sqT[a], "sqT%d" % a
                            has_prev = not (first_tile and a == 0)
                            yb, ybk = yswa[a % 2], "yswa%d" % (a % 2)
                            for g in range(2):
                                P.phase = "swa"
                                pTs = []
                                for which in ([0, 1] if has_prev else [1]):
                                    blk = a + which
                                    sc, sck = attR.next()
                                    mm(sc[:], skT[l][:, blk, g, :], qb_[:, 4 * g:4 * g + 4, :].rearrange("p h t -> p (h t)"),
                                       True, True, [("skT", l), qbk], [sck])
                                    tmp, tmpk, pT, pTk = swtmp[which], "swtmp%d" % which, swpT[which], "swpT%d" % which
                                    stt("dve", tmp[:], sc[:], 0.125, bm[:, which, 4 * g:4 * g + 4, :].rearrange("p h t -> p (h t)"),
                                        ALU.mult, ALU.add, [sck, "bm"], [tmpk])
                                    act(pT[:], tmp[:], AF.Exp, [tmpk], [pTk])
                                    pTs.append((pT, pTk, blk))
                                ao, aok = aoR.next()
                                aov = ao[:, 0:260].rearrange("p (h c) -> p h c", c=65)
                                for hh in range(4):
                                    for i, (pT, pTk, blk) in enumerate(pTs):
                                        mm(aov[:, hh, :], pT[:, hh * 128:(hh + 1) * 128], sV[l][:, blk, g, :], i == 0, i == len(pTs) - 1,
                                           [pTk, ("sV", l)], [aok])
                                tt("dve", sden[:], aov[:, :, 64], expsink[:, l, 4 * g:4 * g + 4], ALU.add, [aok, "expsink"], ["sden"])
                                recip(sden[:], sden[:], ["sden"], ["sden"])
                                tt("dve", yb[:, g * 256:(g + 1) * 256].rearrange("p (h d) -> p h d", d=64), aov[:, :, 0:64],
                                   sden[:].unsqueeze(2).to_broadcast([128, 4, 64]), ALU.mult, [aok, "sden"], [ybk])
                                yield
                            P.phase = "swa"
                            tp, tpk = tpR.next()
                            for k in range(4):
                                tr(tp[:, k, :], yb[:, k * 128:(k + 1) * 128], [ybk], [tpk])
                            cp("act", yT[:, 6:10, a * 128:(a + 1) * 128], tp[:, 0:4, :], [tpk], [("yT", 3)])
                            yield
                        P.phase = "swa"
                        cp("pool", skT[l][:, 0, :, :], skT[l][:, NSUB, :, :], [("skT", l)], [("skT", l)])
                        cp("pool", sV[l][:, 0, :, :], sV[l][:, NSUB, :, :], [("sV", l)], [("sV", l)])

                    def gen_mlstm():
                        for a in range(NSUB):
                            P.phase = "mlstm"
                            sc, sck = attR.next()
                            scv = sc[:].rearrange("p (h t) -> p h t", t=128)
                            for h in range(4):
                                mm(scv[:, h, :], mkT[a][:, h, :], mqT[a][:, h, :], True, True, [("mkT", a), ("mqT", a)], [sck])
                            tt("dve", mPT[:], scv, m01b[:].unsqueeze(1).to_broadcast([128, 4, 128]), ALU.mult, [sck, "m01b"], ["mPT"])
                            yield
                            P.phase = "mlstm"
                            pd, pdk = aoR.next()
                            pdv = pd[0:64, 0:260].rearrange("p (h c) -> p h c", c=65)
                            for h in range(4):
                                mm(pdv[:, h, :], mkt[a][0:64, h * 64:(h + 1) * 64], mV[0:64, a, h, :], True, True, [("mkt", a), "mV"], [pdk])
                            tt("dve", mCf[l][:], mCf[l][:], pdv, ALU.add, [("mCf", l), pdk], [("mCf", l)])
                            tt("dve", mCb[l][1][:], mCf[l][:], mdec[:, a, 0, :].unsqueeze(2).to_broadcast([64, 4, 65]), ALU.mult,
                               [("mCf", l), ("mdec", a)], [("mCb", l, 1)])
                            tt("pool", mCf[l][:], mCf[l][:], mdec[:, a, 0, :].unsqueeze(2).to_broadcast([64, 4, 65]), ALU.mult,
                               [("mCf", l), ("mdec", a)], [("mCf", l)])
                            yield
                            P.phase = "mlstm"
                            ao, aok = aoR.next()
                            aov = ao[:, 0:260].rearrange("p (h c) -> p h c", c=65)
                            for h in range(4):
                                mm(aov[:, h, :], mPT[:, h, :], mV[:, a, h, :], True, False, ["mPT", "mV"], [aok])
                                mm(aov[:, h, :], mq0T[a][:, h, :], mCb[l][0][:, h, :], False, False, [("mq0T", a), ("mCb", l, 0)], [aok])
                                mm(aov[:, h, :], mq1T[a][:, h, :], mCb[l][1][:, h, :], False, True, [("mq1T", a), ("mCb", l, 1)], [aok])
                            tt("dve", mt1[:, 0:4], aov[:, :, 64], meb[:, a, :], ALU.mult, [aok, ("meb", a)], ["mt1"])
                            stt("dve", mt1[:, 0:4], mt1[:, 0:4], -1.0, mt1[:, 0:4], ALU.mult, ALU.max, ["mt1"], ["mt1"])
                            P.emit("dve", lambda e: e.tensor_scalar_max(out=mt1[:, 0:4], in0=mt1[:, 0:4], scalar1=1.0), ["mt1"], ["mt1"])
                            recip(mt1[:, 0:4], mt1[:, 0:4], ["mt1"], ["mt1"])
                            tt("dve", mt1[:, 0:4], mt1[:, 0:4], meb[:, a, :], ALU.mult, ["mt1", ("meb", a)], ["mt1"])
                            tt("dve", mhm[:].rearrange("p (h d) -> p h d", d=64), aov[:, :, 0:64],
                               mt1[:, 0:4].unsqueeze(2).to_broadcast([128, 4, 64]), ALU.mult, [aok, "mt1"], ["mhm"])
                            yield
                            P.phase = "mlstm"
                            pd, pdk = aoR.next()
                            pdv = pd[0:64, 0:260].rearrange("p (h c) -> p h c", c=65)
                            for h in range(4):
                                mm(pdv[:, h, :], mkt[a][64:128, h * 64:(h + 1) * 64], mV[64:128, a, h, :], True, True, [("mkt", a), "mV"], [pdk])
                            tt("dve", mCf[l][:], mCf[l][:], pdv, ALU.add, [("mCf", l), pdk], [("mCf", l)])
                            tt("dve", mCb[l][0][:], mCf[l][:], mdec[:, a, 1, :].unsqueeze(2).to_broadcast([64, 4, 65]), ALU.mult,
                               [("mCf", l), ("mdec", a)], [("mCb", l, 0)])
                            tt("pool", mCf[l][:], mCf[l][:], mdec[:, a, 1, :].unsqueeze(2).to_broadcast([64, 4, 65]), ALU.mult,
                               [("mCf", l), ("mdec", a)], [("mCf", l)])
                            yield
                            P.phase = "mlstm"
                            tt("pool", sqtmp[:, 0:256], mhm[:], mhm[:], ALU.mult, ["mhm"], ["sqtmp"])
                            P.emit("dve", lambda e: e.tensor_reduce(out=nrm_ss[:, 0:4], in_=sqtmp[:, 0:256].rearrange("p (h d) -> p h d", d=64),
                                                                     axis=AX.X, op=ALU.add), ["sqtmp"], ["nrm_ss"])
                            act(nrm_ss[:, 0:4], nrm_ss[:, 0:4], AF.Ln, ["nrm_ss"], ["nrm_ss"], bias=EPS, scale=1.0 / 64)
                            act(nrm_ss[:, 0:4], nrm_ss[:, 0:4], AF.Exp, ["nrm_ss"], ["nrm_ss"], scale=-0.5)
                            tt("dve", mhm[:].rearrange("p (h d) -> p h d", d=64), mhm[:].rearrange("p (h d) -> p h d", d=64),
                               nrm_ss[:, 0:4].unsqueeze(2).to_broadcast([128, 4, 64]), ALU.mult, ["mhm", "nrm_ss"], ["mhm"])
                            tt("pool", mhm[:], mhm[:], rp(l, RP_MHG, 256), ALU.mult, ["mhm", "rowp"], ["mhm"])
                            tt("dve", ymls[:], mhm[:], mso[:, a, :], ALU.mult, ["mhm", ("mso", a)], ["ymls"])
                            tp, tpk = tpR.next()
                            for k in range(2):
                                tr(tp[:, k, :], ymls[:, k * 128:(k + 1) * 128], ["ymls"], [tpk])
                            cp("act", yT[:, 4:6, a * 128:(a + 1) * 128], tp[:, 0:2, :], [tpk], [("yT", 2)])
                            yield

                    gens = [[gen_fox(), max(1, (4 * nblk + 4) // (5 * NSUB))], [gen_swa(), 1], [gen_mlstm(), 1]]
                    while gens:
                        for ge in list(gens):
                            for _ in range(ge[1]):
                                try:
                                    next(ge[0])
                                except StopIteration:
                                    gens.remove(ge)
                                    break

                    if stop_at <= 6:
                        raise _Stop()
                    P.phase = "merge"
                    ych0 = [0, 2, 4, 6]
                    for b, nm in enumerate(["w_fox_out", "w_conv_out", "w_mlstm_out", "w_swa_out"]):
                        Wb, wbk = WS.next(nm, hold=2)
                        nkc = 4 if b == 3 else 2
                        for half in range(2):
                            Wg, wgk = WS.next("w_in")
                            for a in range(NSUB):
                                pg, pgk = proj_tok(Wg, wgk, 512, a, denseR)
                                s_, s_k = sg[a % 2], "sg%d" % (a % 2)
                                act(s_[:], pg[:, 0:512], AF.Sigmoid, [pgk], [s_k])
                                po, pok = denseR.next()
                                for kc in range(nkc):
                                    mm(po[:, 0:512], yT[:, ych0[b] + kc, a * 128:(a + 1) * 128], Wb[:, kc, half * 512:(half + 1) * 512],
                                       kc == 0, kc == nkc - 1, [("yT", b), wbk], [pok])
                                mdst = merged[:, a, half * 512:(half + 1) * 512]
                                if b == 0:
                                    tt("dve", mdst, po[:, 0:512], s_[:], ALU.mult, [pok, s_k], [("merged", a)])
                                else:
                                    tt("dve", mtmp[:], po[:, 0:512], s_[:], ALU.mult, [pok, s_k], ["mtmp"])
                                    tt("pool", mdst, mdst, mtmp[:], ALU.add, [("merged", a), "mtmp"], [("merged", a)])
                    for a in range(NSUB):
                        cp("act", hb[:], merged[:, a, :], [("merged", a)], ["hb"])
                        tp, tpk = tpR.next()
                        for kc in range(8):
                            tr(tp[:, kc, :], hb[:, kc * 128:(kc + 1) * 128], ["hb"], [tpk])
                        cp("act" if a % 2 else "dve", hT[:, :, a * 128:(a + 1) * 128], tp[:, :, :], [tpk], [("hT", a)])
                    P.phase = "merge_out"
                    for half in range(2):
                        Wm, wmk = WS.next("w_merge_out")
                        for a in range(NSUB):
                            ps, pk = proj_tok(Wm, wmk, 512, a, denseR)
                            xs = xt[:, a, half * 512:(half + 1) * 512]
                            tt("dve", xs, ps[:, 0:512], xs, ALU.add, [pk, ("xt", a)], [("xt", a)])

                    if stop_at <= 7:
                        raise _Stop()
                    P.phase = "ffn"
                    resid_norm(l, 1)
                    for c in range(6):
                        ncol = min(512, DFF - c * 512)
                        Wg, wgk = WS.next("w_gate", hold=1)
                        Wu, wuk = WS.next("w_up")
                        for cb_ in range(ncol // 128):
                            ffc = c * 4 + cb_
                            pg, pgk = denseR.next()
                            for kc in range(8):
                                mm(pg[:, 0:T], Wg[:, kc, cb_ * 128:(cb_ + 1) * 128], hT[:, kc, :], kc == 0, kc == 7, HT_ALL + [wgk], [pgk])
                            pu, puk = denseR.next()
                            for kc in range(8):
                                mm(pu[:, 0:T], Wu[:, kc, cb_ * 128:(cb_ + 1) * 128], hT[:, kc, :], kc == 0, kc == 7, HT_ALL + [wuk], [puk])
                            s_, s_k = sg[ffc % 2], "sg%d" % (ffc % 2)
                            act(s_[:, 0:T], pg[:, 0:T], AF.Silu, [pgk], [s_k])
                            tt("dve", actT[:, ffc, :], pu[:, 0:T], s_[:, 0:T], ALU.mult, [puk, s_k], [("actT", ffc)])
                    P.phase = "ffn_down"
                    ACT_ALL = [("actT", f) for f in range(22)]
                    for half in range(2):
                        accs = []
                        for a in range(NSUB):
                            accs.append(denseR.next())
                        for c in range(3):
                            Wd, wdk = WS.next("w_down")
                            k0 = c * 8
                            nk = min(8, 22 - k0)
                            for a in range(NSUB):
                                ps, pk = accs[a]
                                for k in range(nk):
                                    mm(ps[:, 0:512], actT[:, k0 + k, a * 128:(a + 1) * 128], Wd[:, k, :], (k0 + k) == 0, (k0 + k) == 21,
                                       ACT_ALL + [wdk], [pk])
                        for a in range(NSUB):
                            ps, pk = accs[a]
                            xs = xt[:, a, half * 512:(half + 1) * 512]
                            tt("dve", xs, ps[:, 0:512], xs, ALU.add, [pk, ("xt", a)], [("xt", a)])
                for a in range(NSUB):
                    dma("sp", out_d[tok0 + a * 128: tok0 + (a + 1) * 128, :], xt[:, a, :], [("xt", a)], ["out"], "out")
        try:
            main_loop()
        except _Stop:
            for a in range(NSUB):
                dma("sp", out_d[a * 128:(a + 1) * 128, :], xt[:, a, :], [("xt", a)], ["out"], "out")
        P.emit("sp", lambda e: e.nop(), ["out"], ())
        P.finalize(nc)
        _LAST["P"] = P
    return nc


def _t5_bucket(n):
    max_exact = 16
    nf = np.maximum(n, 1).astype(np.float32)
    large = max_exact + (np.log(nf / max_exact) / math.log(128 / max_exact) * (32 - max_exact)).astype(np.int32)
    large = np.minimum(large, 31)
    return np.where(n < max_exact, n, large)


def host_consts(inp):
    f32 = np.float32
    s = np.arange(128)[:, None]
    t = np.arange(128)[None, :]
    cst = np.zeros((128, 6, 128), f32)
    cst[:, 0] = np.eye(128, dtype=f32)
    cst[:, 1] = (s <= t)
    cst[:, 2] = (s <= t) & (s // 64 == t // 64)
    cst[:, 3] = np.where(s <= t, 0.0, NEG)
    cst[:, 4] = 1.0
    cst[:, 5] = cst[:, 2]
    j = np.arange(128)[:, None]
    i = np.arange(128)[None, :]
    swam = np.zeros((128, 2, 128), f32)
    d_prev = i + 128 - j
    d_cur = i - j
    swam[:, 0] = np.where((d_prev >= 0) & (d_prev < 128), 0.0, NEG)
    swam[:, 1] = np.where((d_cur >= 0) & (d_cur < 128), 0.0, NEG)
    rel = np.asarray(inp["rel_bias"], f32)
    swab = np.zeros((128, 2, 8, 128), f32)
    swab[:, 0] = rel[_t5_bucket(np.maximum(d_prev, 0))].transpose(0, 2, 1)
    swab[:, 1] = rel[_t5_bucket(np.maximum(d_cur, 0))].transpose(0, 2, 1)
    gT = np.zeros((128, 32), f32)
    for l in range(DEPTH):
        gT[:, l * 16:l * 16 + 8] = np.asarray(inp["attn_norm"][l], f32).reshape(8, 128).T
        gT[:, l * 16 + 8:l * 16 + 16] = np.asarray(inp["ffn_norm"][l], f32).reshape(8, 128).T
    rowp = np.zeros((1, DEPTH * ROWP), f32)
    for l in range(DEPTH):
        o = l * ROWP
        rowp[0, o + RP_FQG:o + RP_FQG + 64] = inp["fox_q_gain"][l]
        rowp[0, o + RP_FKG:o + RP_FKG + 64] = inp["fox_k_gain"][l]
        rowp[0, o + RP_SQG:o + RP_SQG + 64] = inp["swa_q_gain"][l]
        rowp[0, o + RP_SKG:o + RP_SKG + 64] = inp["swa_k_gain"][l]
        rowp[0, o + RP_MHG:o + RP_MHG + 256] = inp["mlstm_h_gain"][l]
        rowp[0, o + RP_FFB:o + RP_FFB + 4] = inp["fox_f_bias"][l]
        rowp[0, o + RP_MIB:o + RP_MIB + 4] = inp["mlstm_i_bias"][l]
        rowp[0, o + RP_MFB:o + RP_MFB + 4] = inp["mlstm_f_bias"][l]
        rowp[0, o + RP_SNK:o + RP_SNK + 8] = inp["swa_sinks"][l]
    convw = np.zeros((128, 12), f32)
    cw = np.asarray(inp["conv_w"], f32)
    for l in range(DEPTH):
        for cc in range(2):
            for k in range(3):
                convw[:, l * 6 + cc * 3 + k] = cw[l, k, cc * 128:(cc + 1) * 128]
    gcol = np.zeros((128, 8), f32)
    for l in range(DEPTH):
        for i, nm in enumerate(["fox_q_gain", "fox_k_gain", "swa_q_gain", "swa_k_gain"]):
            gcol[:, l * 4 + i] = np.tile(np.asarray(inp[nm][l], f32), 2)
    return {"cst": cst, "swab": swab, "swam": swam, "gT": gT, "rowp": rowp, "convw": convw, "gcol": gcol}


_NC_CACHE = {}


def kernel(**inputs):
    inp = {k: np.asarray(v) for k, v in inputs.items()}
    x = np.ascontiguousarray(inp["x"], dtype=np.float32)
    n_cores = 8
    key = "full"
    if key not in _NC_CACHE:
        _NC_CACHE[key] = build()
    nc = _NC_CACHE[key]
    shared = host_consts(inp)
    for name, r, c in WEIGHTS:
        shared[name] = np.ascontiguousarray(inp[name], dtype=np.float32)
    in_maps = []
    for c in range(n_cores):
        m = dict(shared)
        m["x"] = x[2 * c:2 * c + 2].reshape(2 * SEQ, D)
        in_maps.append(m)
    res = run_bass_kernel_spmd(nc, in_maps, core_ids=list(range(n_cores)))
    out = np.stack([r["out"].reshape(2, SEQ, D) for r in res.results], axis=0).reshape(16, SEQ, D)
    return out.astype(np.float32)
```

```python
import contextlib
import math
import numpy as np
import concourse.bass as bass
import concourse.mybir as mybir
from concourse.bass_utils import run_bass_kernel_spmd

F32 = mybir.dt.float32
BF16 = mybir.dt.bfloat16
ALU = mybir.AluOpType
AF = mybir.ActivationFunctionType
AX = mybir.AxisListType

D = 1024
SEQ = 2048
DEPTH = 2
DFF = 2816
INC = 7436
EPS = 1e-6
NEG = -30000.0
C_FOX, C_CONV, C_ML, C_SWA, C_GATE = 0, 772, 1540, 2572, 3340

COMPUTE = ("pe", "act", "dve", "pool")
ISSUERS = ("pe", "act", "dve", "pool", "sp")


class Op:
    __slots__ = ("eng", "fn", "waits", "marked", "idx", "vc", "dma_sem", "dma_val", "count", "phase")

    def __init__(self, eng, fn):
        self.eng = eng
        self.fn = fn
        self.waits = []
        self.marked = False
        self.idx = -1
        self.vc = None
        self.dma_sem = None
        self.dma_val = 0
        self.count = 0


class Prog:
    def __init__(self):
        self.ops = {e: [] for e in ISSUERS}
        self.last_w = {}
        self.readers = {}
        self.known = {e: {} for e in ISSUERS}
        self.known_dma = {e: {} for e in ISSUERS}
        self.dma_count = {}
        self.phase = "init"
        self.annotate = False

    def _dep(self, op, prod):
        if prod is None or prod is op:
            return
        A = op.eng
        if prod.dma_sem is not None:
            val = self.dma_count[prod.dma_sem]
            if self.known_dma[A].get(prod.dma_sem, 0) >= val:
                return
            self.known_dma[A][prod.dma_sem] = val
            op.waits.append((prod, val))
        else:
            B = prod.eng
            if B == "pe" and A == "pe":
                return
            if self.known[A].get(B, -1) >= prod.idx:
                return
            op.waits.append((prod, None))
            prod.marked = True
        for b, n in prod.vc[0].items():
            if self.known[A].get(b, -1) < n:
                self.known[A][b] = n
        for s, v in prod.vc[1].items():
            if self.known_dma[A].get(s, 0) < v:
                self.known_dma[A][s] = v

    def last(self, eng):
        return self.ops[eng][-1] if self.ops[eng] else None

    def emit(self, eng, fn, reads=(), writes=(), dma_sem=None, after=()):
        op = Op(eng, fn)
        op.phase = self.phase
        op.idx = len(self.ops[eng])
        for r in reads:
            self._dep(op, self.last_w.get(r))
        for w in writes:
            self._dep(op, self.last_w.get(w))
            for rd in self.readers.get(w, ()):
                self._dep(op, rd)
        for a in after:
            self._dep(op, a)
        if dma_sem is not None:
            op.dma_sem = dma_sem
            self.dma_count[dma_sem] = self.dma_count.get(dma_sem, 0) + 16
            op.dma_val = self.dma_count[dma_sem]
        kc = dict(self.known[eng])
        if dma_sem is None:
            kc[eng] = op.idx
            op.vc = (kc, dict(self.known_dma[eng]))
        else:
            kd = dict(self.known_dma[eng])
            kd[dma_sem] = op.dma_val
            op.vc = (kc, kd)
        for r in reads:
            self.readers.setdefault(r, []).append(op)
        for w in writes:
            self.last_w[w] = op
            self.readers[w] = []
        self.ops[eng].append(op)
        return op

    def finalize(self, nc):
        for e in ISSUERS:
            c = 0
            for op in self.ops[e]:
                if op.dma_sem is None and op.marked:
                    c += 1
                op.count = c
        with contextlib.ExitStack() as st:
            esem = {e: st.enter_context(nc.semaphore("s_" + e)) for e in COMPUTE}
            dsem = {k: st.enter_context(nc.semaphore("d_" + str(k))) for k in self.dma_count}
            block = st.enter_context(nc.Block())

            def run(e, engine):
                for op in self.ops[e]:
                    for p, val in op.waits:
                        if p.dma_sem is not None:
                            engine.wait_ge(dsem[p.dma_sem], val)
                        else:
                            engine.wait_ge(esem[p.eng], p.count)
                    ins = op.fn(engine)
                    if self.annotate:
                        ins.annotate(op.phase)
                    if op.dma_sem is not None:
                        ins.then_inc(dsem[op.dma_sem], 16)
                    elif op.marked:
                        ins.then_inc(esem[e], 1)

            @block.tensor
            def _(eng):
                run("pe", eng)

            @block.scalar
            def _(eng):
                run("act", eng)

            @block.vector
            def _(eng):
                run("dve", eng)

            @block.gpsimd
            def _(eng):
                run("pool", eng)

            @block.sync
            def _(eng):
                run("sp", eng)


WEIGHTS = [
    ("w_in", D, INC), ("w_fox_out", 256, D), ("w_conv_out", 256, D), ("w_mlstm_out", 256, D),
    ("w_swa_out", 512, D), ("w_merge_out", D, D), ("w_gate", D, DFF), ("w_up", D, DFF),
    ("w_down", DFF, D),
]
ROWP = 532
RP_FQG, RP_FKG, RP_SQG, RP_SKG, RP_MHG, RP_FFB, RP_MIB, RP_MFB, RP_SNK = 0, 64, 128, 192, 256, 512, 516, 520, 524


class _Stop(Exception):
    pass


_LAST = {}


def build(n_seq=2, n_tiles=8, n_layers=2, NSUB=2, seq_len=SEQ, stop_at=99, do_cast=True, annotate=False):
    T = NSUB * 128
    nc = bass.Bass("TRN2", target_bir_lowering=False)
    NTOK = n_seq * seq_len
    x_d = nc.dram_tensor("x", [NTOK, D], F32, kind="ExternalInput").ap()
    out_d = nc.dram_tensor("out", [NTOK, D], F32, kind="ExternalOutput").ap()
    wf = {}
    wb = {}
    for name, r, c in WEIGHTS:
        wf[name] = nc.dram_tensor(name, [DEPTH, r, c], F32, kind="ExternalInput").ap()
    cst_d = nc.dram_tensor("cst", [128, 6, 128], F32, kind="ExternalInput").ap()
    swab_d = nc.dram_tensor("swab", [128, 2, 8, 128], F32, kind="ExternalInput").ap()
    swam_d = nc.dram_tensor("swam", [128, 2, 128], F32, kind="ExternalInput").ap()
    gT_d = nc.dram_tensor("gT", [128, 32], F32, kind="ExternalInput").ap()
    rowp_d = nc.dram_tensor("rowp", [1, DEPTH * ROWP], F32, kind="ExternalInput").ap()
    convw_d = nc.dram_tensor("convw", [128, 12], F32, kind="ExternalInput").ap()
    gcol_d = nc.dram_tensor("gcol", [128, 8], F32, kind="ExternalInput").ap()

    P = Prog()
    P.annotate = annotate
    st = contextlib.ExitStack()
    with st:
        def sb(name, shape, dt):
            return st.enter_context(nc.sbuf_tensor("s_" + name, shape, dt))

        def psum(name, shape, dt):
            return st.enter_context(nc.psum_tensor("p_" + name, shape, dt))

        def tt(eng, out, in0, in1, op, R, W):
            return P.emit(eng, lambda e: e.tensor_tensor(out=out, in0=in0, in1=in1, op=op), R, W)

        def ts(eng, out, in0, s1, s2, op0, op1, R, W):
            if s2 is None:
                return P.emit(eng, lambda e: e.tensor_scalar(out=out, in0=in0, scalar1=s1, scalar2=None, op0=op0), R, W)
            return P.emit(eng, lambda e: e.tensor_scalar(out=out, in0=in0, scalar1=s1, scalar2=s2, op0=op0, op1=op1), R, W)

        def stt(eng, out, in0, scalar, in1, op0, op1, R, W):
            return P.emit(eng, lambda e: e.scalar_tensor_tensor(out=out, in0=in0, scalar=scalar, in1=in1, op0=op0, op1=op1), R, W)

        def cp(eng, out, in_, R, W):
            if eng == "act":
                return P.emit("act", lambda e: e.copy(out=out, in_=in_), R, W)
            return P.emit(eng, lambda e: e.tensor_copy(out=out, in_=in_), R, W)

        def act(out, in_, func, R, W, bias=None, scale=1.0, accum=None):
            kw = {}
            if bias is not None:
                kw["bias"] = bias
            if accum is not None:
                kw["accum_out"] = accum
            return P.emit("act", lambda e: e.activation(out=out, in_=in_, func=func, scale=scale, **kw), R, W)

        def mm(out, lhsT, rhs, start, stop, R, W):
            return P.emit("pe", lambda e: e.matmul(out, lhsT=lhsT, rhs=rhs, start=start, stop=stop), R, W)

        def tr(out, in_, R, W):
            return P.emit("pe", lambda e: e.transpose(out=out, in_=in_, identity=identb[:]), list(R) + ["identb"], W)

        def mset(eng, ap, val, W):
            return P.emit(eng, lambda e: e.memset(ap, val), (), W)

        def recip(out, in_, R, W):
            return P.emit("dve", lambda e: e.reciprocal(out=out, in_=in_), R, W)

        def dma(eng, out, in_, R, W, sem):
            return P.emit(eng, lambda e: e.dma_start(out=out, in_=in_), R, W, dma_sem=sem)

        class Rot:
            def __init__(self, items):
                self.items = items
                self.i = 0

            def next(self):
                it = self.items[self.i % len(self.items)]
                self.i += 1
                return it

        mmR = Rot([(psum("mm%d" % i, [128, 512], F32), "mm%d" % i) for i in range(2)])
        tpR = Rot([(psum("tp%d" % i, [128, 8, 128], BF16), "tp%d" % i) for i in range(2)])
        scR = Rot([(psum("sc%d" % i, [128, 512], F32), "sc%d" % i) for i in range(2)])
        aoR = Rot([(psum("ao%d" % i, [128, 512], F32), "ao%d" % i) for i in range(2)])
        denseR = Rot(mmR.items + scR.items + aoR.items)
        foxR = Rot([mmR.items[1], scR.items[0]])
        attR = Rot([scR.items[1]])
        projR = Rot(mmR.items + scR.items)

        cst = sb("cst", [128, 6, 128], F32)
        identb = sb("identb", [128, 128], BF16)
        m01b = sb("m01b", [128, 128], BF16)
        chunkind = sb("chunkind", [128, 2, 64], F32)
        bm = sb("bm", [128, 2, 8, 128], F32)
        swam = sb("swam", [128, 2, 128], F32)
        gT = sb("gT", [128, 32], F32)
        rowp = sb("rowp", [128, DEPTH * ROWP], F32)
        convw = sb("convw", [128, 12], F32)
        gcol = sb("gcol", [128, 8], F32)
        expsink = sb("expsink", [128, DEPTH, 8], F32)
        tri, triblk, cmaskT, ones = cst[:, 1, :], cst[:, 2, :], cst[:, 3, :], cst[:, 4, :]

        dma("sp", cst[:], cst_d, (), ["cst"], "c0")
        dma("sp", bm[:], swab_d, (), ["bm"], "c1")
        dma("sp", swam[:], swam_d, (), ["swam"], "c2")
        dma("sp", gT[:], gT_d, (), ["gT"], "c3")
        dma("sp", rowp[:], rowp_d.partition_broadcast(128), (), ["rowp"], "c4")
        dma("sp", convw[:], convw_d, (), ["convw"], "c5")
        dma("sp", gcol[:], gcol_d, (), ["gcol"], "c6")
        cp("dve", identb[:], cst[:, 0, :], ["cst"], ["identb"])
        cp("dve", m01b[:], cst[:, 5, :], ["cst"], ["m01b"])
        mset("pool", chunkind[:], 0.0, ["chunkind"])
        mset("pool", chunkind[0:64, 0, :], 1.0, ["chunkind"])
        mset("pool", chunkind[64:128, 1, :], 1.0, ["chunkind"])
        for blk in range(2):
            tt("dve", bm[:, blk, :, :], bm[:, blk, :, :], swam[:, blk, :].unsqueeze(1).to_broadcast([128, 8, 128]),
               ALU.add, ["bm", "swam"], ["bm"])
        for l in range(DEPTH):
            act(expsink[:, l, :], rowp[:, l * ROWP + RP_SNK: l * ROWP + RP_SNK + 8], AF.Exp, ["rowp"], ["expsink"])

        NSLOT = 5
        CAST_AHEAD = 8
        wslots = [sb("wslot%d" % i, [128, 8, 512], BF16) for i in range(NSLOT)]

        def layer_chunks(l):
            ch = []

            def add(name, r0, nrows, c0, ncol):
                ch.append((name, l, r0, nrows, c0, ncol, nrows // 128))

            def win(c0, ncol):
                add("w_in", 0, D, c0, ncol)

            win(C_FOX, 512)
            win(C_FOX + 512, 260)
            win(C_SWA + 512, 256)
            win(C_SWA, 512)
            win(C_ML + 512, 264)
            win(C_ML + 776, 256)
            win(C_ML, 512)
            win(C_CONV, 512)
            win(C_CONV + 512, 256)
            for b_, (nm, rows) in enumerate([("w_fox_out", 256), ("w_conv_out", 256), ("w_mlstm_out", 256), ("w_swa_out", 512)]):
                add(nm, 0, rows, 0, 1024)
                win(C_GATE + b_ * 1024, 512)
                win(C_GATE + b_ * 1024 + 512, 512)
            for h in range(2):
                add("w_merge_out", 0, D, h * 512, 512)
            for c in range(6):
                ncol = min(512, DFF - c * 512)
                add("w_gate", 0, D, c * 512, ncol)
                add("w_up", 0, D, c * 512, ncol)
            for h in range(2):
                for c in range(3):
                    k0 = c * 8
                    nk = min(8, 22 - k0)
                    add("w_down", k0 * 128, nk * 128, h * 512, 512)
            return ch

        first_pass = []
        for l in range(n_layers):
            first_pass.extend(layer_chunks(l))
        NFP = len(first_pass)
        scr = [nc.dram_tensor("scr%d" % k, [128, ch[6] * ch[5]], BF16, kind="Internal").ap() for k, ch in enumerate(first_pass)]
        n_pass = n_seq * n_tiles

        class WStream:
            def __init__(self):
                self.issued = 0
                self.taken = 0
                self.holds = {}
                self.cast_next = 0
                self.total = NFP * n_pass

            def _cast_upto(self, k_end):
                while self.cast_next < min(k_end, NFP):
                    k = self.cast_next
                    name, l, r0, nr, c0, nc_, nk = first_pass[k]
                    if do_cast:
                        dma("pool", scr[k].rearrange("p (k c) -> k p c", c=nc_),
                            wf[name][l, r0:r0 + nr, c0:c0 + nc_].rearrange("(k p) c -> k p c", p=128), (), [("wbc", k)], "cast%d" % k)
                    self.cast_next += 1

            def _view(self, n):
                name, l, r0, nr, c0, ncol, nk = first_pass[n % NFP]
                slot = n % NSLOT
                return wslots[slot][:].rearrange("p k c -> p (k c)")[:, 0:nk * ncol].rearrange("p (k c) -> p k c", c=ncol)

            def _issue(self, n):
                k = n % NFP
                name, l, r0, nr, c0, ncol, nk = first_pass[k]
                self._cast_upto(k + 1 + (CAST_AHEAD if n < NFP else 0))
                flat = wslots[n % NSLOT][:].rearrange("p k c -> p (k c)")[:, 0:nk * ncol]
                dma("sp", flat, scr[k], [("wbc", k)] if do_cast else [], [("ws", n % NSLOT)], "ws%d" % (n % NSLOT))

            def next(self, expect, hold=0):
                m = self.taken
                while self.issued < self.total and self.issued < m + NSLOT:
                    k = self.issued
                    prev = k - NSLOT
                    if prev >= 0 and not (prev + 1 + self.holds[prev] <= m):
                        break
                    self._issue(k)
                    self.issued += 1
                n = self.taken
                assert self.issued > n
                self.holds[n] = hold
                self.taken += 1
                assert first_pass[n % NFP][0] == expect, (first_pass[n % NFP][0], expect)
                return self._view(n), ("ws", n % NSLOT)

        WS = WStream()

        xt = sb("xt", [128, NSUB, D], F32)
        hT = sb("hT", [128, 8, T], BF16)
        hb = sb("hb", [128, D], BF16)
        small = sb("small", [128, 64], F32)
        stages = [sb("stage%d" % i, [128, 512], F32) for i in range(3)]
        stR = Rot([(stages[i], "stage%d" % i) for i in range(3)])
        sqtmp2 = [sb("sqtmp%d" % i, [128, 512], F32) for i in range(2)]
        nrm_ss2 = [sb("nrm_ss%d" % i, [128, 8], F32) for i in range(2)]
        sqtmp, nrm_ss = sqtmp2[0], nrm_ss2[0]
        hn_ctr = [0]
        qkn_l = [sb("qkn%d" % i, [128, 512], BF16) for i in range(NSUB)]
        fqT = sb("fqT", [128, 2, T], BF16)
        fkT = [sb("fkT%d" % l, [128, 2, seq_len], BF16) for l in range(n_layers)]
        fV = [sb("fV%d" % l, [128, seq_len // 128, 4, 65], BF16) for l in range(n_layers)]
        fnegc = [sb("fnegc%d" % l, [128, seq_len // 128, 4], F32) for l in range(n_layers)]
        fncar = [sb("fncar%d" % l, [128, NSUB + 1, 4], F32) for l in range(n_layers)]
        fsp = sb("fsp", [128, NSUB, 4], F32)
        fxf = sb("fxf", [128, 4], F32)
        fD = sb("fD", [128, 128], F32)
        fcbc = [sb("fcbc%d" % i, [128, T], F32) for i in range(4)]
        fcbm = [sb("fcbm%d" % i, [128, T], F32) for i in range(4)]
        atmp = [sb("atmp%d" % i, [128, 512], F32) for i in range(2)]
        apT = [sb("apT%d" % i, [128, 512], BF16) for i in range(2)]
        frec = sb("frec", [128, 8], F32)
        yfox = sb("yfox", [128, NSUB, 256], BF16)
        sqT = [sb("sqT%d" % i, [64, 8, 128], BF16) for i in range(NSUB)]
        swtmp = [sb("swtmp%d" % i, [128, 512], F32) for i in range(2)]
        swpT = [sb("swpT%d" % i, [128, 512], BF16) for i in range(2)]
        skT = [sb("skT%d" % l, [64, NSUB + 1, 2, 128], BF16) for l in range(n_layers)]
        sV = [sb("sV%d" % l, [128, NSUB + 1, 2, 65], BF16) for l in range(n_layers)]
        skst = sb("skst", [128, 128], F32)
        skn = sb("skn", [128, 128], BF16)
        yswa = [sb("yswa%d" % i, [128, 512], BF16) for i in range(2)]
        sden = sb("sden", [128, 4], F32)
        mV = sb("mV", [128, NSUB, 4, 65], BF16)
        mif = sb("mif", [128, NSUB, 8], F32)
        msp = sb("msp", [128, NSUB, 4], F32)
        mea = sb("mea", [128, NSUB, 4], F32)
        meb = sb("meb", [128, NSUB, 4], F32)
        mdec = sb("mdec", [64, NSUB, 2, 4], F32)
        mso = sb("mso", [128, NSUB, 256], F32)
        mkt = [sb("mkt%d" % i, [128, 256], BF16) for i in range(NSUB)]
        mqb_l = [sb("mqb%d" % i, [128, 256], BF16) for i in range(NSUB)]
        mqT = [sb("mqT%d" % i, [64, 4, 128], BF16) for i in range(NSUB)]
        mq0T = [sb("mq0T%d" % i, [64, 4, 128], BF16) for i in range(NSUB)]
        mq1T = [sb("mq1T%d" % i, [64, 4, 128], BF16) for i in range(NSUB)]
        mkT = [sb("mkT%d" % i, [64, 4, 128], BF16) for i in range(NSUB)]
        mPT = sb("mPT", [128, 4, 128], BF16)
        mCf = [sb("mCf%d" % l, [64, 4, 65], F32) for l in range(n_layers)]
        mCb = [[sb("mCb%d_%d" % (l, i), [64, 4, 65], BF16) for i in range(2)] for l in range(n_layers)]
        mt1 = sb("mt1", [128, 8], F32)
        mp2 = sb("mp2", [128, 16], F32)
        mhm = sb("mhm", [128, 256], F32)
        ymls = sb("ymls", [128, 256], BF16)
        cu = sb("cu", [128, 2, T], F32)
        cbt = sb("cbt", [128, 2, T], F32)
        cz = [sb("cz%d" % l, [128, 2, T + 2], F32) for l in range(n_layers)]
        cy = sb("cy", [128, T], F32)
        yT = sb("yT", [128, 10, T], BF16)
        sg = [sb("sg%d" % i, [128, 512], F32) for i in range(2)]
        mtmp = sqtmp2[1]
        merged = sb("merged", [128, NSUB, D], F32)
        actT = sb("actT", [128, 22, T], BF16)

        def rp(l, off, n):
            return rowp[:, l * ROWP + off: l * ROWP + off + n]

        for a in range(NSUB):
            mset("pool", mq0T[a][:], 0.0, [("mq0T", a)])
            mset("pool", mq1T[a][:], 0.0, [("mq1T", a)])
        mset("pool", mV[:], 1.0, ["mV"])
        for l in range(n_layers):
            mset("pool", fV[l][:], 1.0, [("fV", l)])
            mset("pool", sV[l][:], 1.0, [("sV", l)])

        def head_norm(src, nh, gain_views, out_bf, srcR, outW, out_scale=1.0):
            i = hn_ctr[0] % 2
            hn_ctr[0] += 1
            sq, sqk = sqtmp2[i], "sqtmp" + "AB"[i]
            ss, ssk = nrm_ss2[i], "nrm_ss" + "AB"[i]
            w = nh * 64
            tt("pool", sq[:, 0:w], src, src, ALU.mult, srcR, [sqk])
            P.emit("dve", lambda e: e.tensor_reduce(out=ss[:, 0:nh], in_=sq[:, 0:w].rearrange("p (h d) -> p h d", d=64),
                                                     axis=AX.X, op=ALU.add), [sqk], [ssk])
            act(ss[:, 0:nh], ss[:, 0:nh], AF.Ln, [ssk], [ssk], bias=EPS, scale=1.0 / 64)
            act(ss[:, 0:nh], ss[:, 0:nh], AF.Exp, [ssk], [ssk], scale=-0.5)
            tt("dve", out_bf[:, 0:w].rearrange("p (h d) -> p h d", d=64), src.rearrange("p (h d) -> p h d", d=64),
               ss[:, 0:nh].unsqueeze(2).to_broadcast([128, nh, 64]), ALU.mult, list(srcR) + [ssk], outW)

        def resid_norm(l, which):
            for a in range(NSUB):
                mset("pool", small[:, 0:1], 0.0, ["small0"])
                act(hb[:], xt[:, a, :], AF.Square, [("xt", a), "small0"], ["hb", "small0"], accum=small[:, 0:1])
                act(small[:, 1:2], small[:, 0:1], AF.Ln, ["small0"], ["small1"], bias=EPS, scale=1.0 / D)
                act(small[:, 2:3], small[:, 1:2], AF.Exp, ["small1"], ["small2"], scale=-0.5)
                ts("dve", hb[:], xt[:, a, :], small[:, 2:3], None, ALU.mult, None, [("xt", a), "small2"], ["hb"])
                tp, tpk = tpR.next()
                for kc in range(8):
                    tr(tp[:, kc, :], hb[:, kc * 128:(kc + 1) * 128], ["hb"], [tpk])
                g0 = l * 16 + which * 8
                tt("dve", hT[:, :, a * 128:(a + 1) * 128], tp[:, :, :],
                   gT[:, g0:g0 + 8].unsqueeze(2).to_broadcast([128, 8, 128]), ALU.mult, [tpk, "gT"], [("hT", a)])

        HT_ALL = [("hT", a) for a in range(NSUB)]

        def proj_tok(W, wk, ncol, a, rot=None):
            ps, pk = (rot or projR).next()
            for kc in range(8):
                mm(ps[:, 0:ncol], hT[:, kc, a * 128:(a + 1) * 128], W[:, kc, :], kc == 0, kc == 7, [("hT", a), wk], [pk])
            return ps, pk

        def main_loop():
          for s in range(n_seq):
            for t in range(n_tiles):
                tok0 = s * seq_len + t * T
                blk0 = t * NSUB
                first_tile = (t == 0)
                for a in range(NSUB):
                    dma("sp", xt[:, a, :], x_d[tok0 + a * 128: tok0 + (a + 1) * 128, :], (), [("xt", a)], "xt")
                for l in range(n_layers):
                    if first_tile:
                        mset("pool", fncar[l][:, 0, :], 0.0, [("fncar", l)])
                        mset("pool", mCf[l][:], 0.0, [("mCf", l)])
                        mset("pool", mCb[l][0][:], 0.0, [("mCb", l, 0)])
                        mset("pool", cz[l][:, :, 0:2], 0.0, [("cz", l)])
                    if stop_at <= 0:
                        raise _Stop()
                    deferred = []

                    def flush():
                        while deferred:
                            deferred.pop(0)()
                    P.phase = "norm1"
                    resid_norm(l, 0)

                    if stop_at <= 1:
                        raise _Stop()
                    def fox_prologue(h):
                        cb, cbk = fcbc[h], "fcbc%d" % h
                        cbm, cbmk = fcbm[h], "fcbm%d" % h
                        pc, pck = aoR.next()
                        for a in range(NSUB):
                            ts("dve", fD[:], ones, fsp[:, a, h:h + 1], None, ALU.mult, None, ["cst", ("fsp", a)], ["fD"])
                            mm(pc[:, a * 128:(a + 1) * 128], fD[:], tri, True, True, ["fD", "cst"], [pck])
                        for a in range(NSUB):
                            act(cb[:, a * 128:(a + 1) * 128], pc[:, a * 128:(a + 1) * 128], AF.Identity, [pck, ("fncar", l)], [cbk],
                                bias=fncar[l][:, a, h:h + 1])
                        tt("pool", cbm[:].rearrange("p (a t) -> p a t", t=128), cb[:].rearrange("p (a t) -> p a t", t=128),
                           cmaskT.unsqueeze(1).to_broadcast([128, NSUB, 128]), ALU.subtract, [cbk, "cst"], [cbmk])

                    P.phase = "fox_proj"
                    W, wk = WS.next("w_in")
                    for a in range(NSUB):
                        ps, pk = proj_tok(W, wk, 512, a)
                        stg, sk = stR.next()
                        cp("act", stg[:], ps[:, 0:512], [pk], [sk])
                        if stop_at <= 1.1:
                            raise _Stop()
                        qkn, qknk = qkn_l[a], "qkn%d" % a
                        head_norm(stg[:], 8, None, qkn, [sk], [qknk])

                        def fox_tr(a=a, qkn=qkn, qknk=qknk):
                            tp, tpk = tpR.next()
                            for k in range(4):
                                tr(tp[:, k, :], qkn[:, k * 128:(k + 1) * 128], [qknk], [tpk])
                            act(fqT[:, :, a * 128:(a + 1) * 128], tp[:, 0:2, :], AF.Copy, [tpk, "gcol"], ["fqT"], scale=gcol[:, l * 4 + 0:l * 4 + 1])
                            act(fkT[l][:, :, (blk0 + a) * 128:(blk0 + a + 1) * 128], tp[:, 2:4, :], AF.Copy, [tpk, "gcol"], [("fkT", l)],
                                scale=gcol[:, l * 4 + 1:l * 4 + 2])
                        deferred.append(fox_tr)
                    if stop_at <= 1.3:
                        raise _Stop()
                    W, wk = WS.next("w_in")
                    for a in range(NSUB):
                        ps, pk = proj_tok(W, wk, 260, a)
                        cp("dve", fV[l][:, blk0 + a, :, 0:64], ps[:, 0:256].rearrange("p (h d) -> p h d", d=64), [pk], [("fV", l)])
                        if stop_at <= 1.4:
                            raise _Stop()
                        tt("dve", fxf[:], ps[:, 256:260], rp(l, RP_FFB, 4), ALU.add, [pk, "rowp"], ["fxf"])
                        act(fxf[:], fxf[:], AF.Exp, ["fxf"], ["fxf"], scale=-1.0)
                        act(fsp[:, a, :], fxf[:], AF.Ln, ["fxf"], [("fsp", a)], bias=1.0)
                        if stop_at <= 1.5:
                            raise _Stop()

                        def fox_cum(a=a):
                            p2, p2k = aoR.next()
                            mm(p2[:, 0:4], tri, fsp[:, a, :], True, True, ["cst", ("fsp", a)], [p2k])
                            mm(p2[:, 4:8], ones, fsp[:, a, :], True, True, ["cst", ("fsp", a)], [p2k])
                            tt("dve", fnegc[l][:, blk0 + a, :], p2[:, 0:4], fncar[l][:, a, :], ALU.add, [p2k, ("fncar", l)], [("fnegc", l)])
                            tt("dve", fncar[l][:, a + 1, :], p2[:, 4:8], fncar[l][:, a, :], ALU.add, [p2k, ("fncar", l)], [("fncar", l)])
                        deferred.append(fox_cum)

                    late = [(lambda h=h: fox_prologue(h)) for h in range(4)]
                    if stop_at <= 2:
                        flush()
                        raise _Stop()
                    P.phase = "swa_proj"
                    W, wk = WS.next("w_in")
                    for a in range(NSUB):
                        ps, pk = proj_tok(W, wk, 256, a)
                        cp("act", skst[:], ps[:, 0:128], [pk], ["skst"])
                        cp("act", sV[l][:, a + 1, :, 0:64], ps[:, 128:256].rearrange("p (h d) -> p h d", d=64), [pk], [("sV", l)])
                        head_norm(skst[:], 2, None, skn, ["skst"], ["skn"])
                        tp, tpk = tpR.next()
                        for g in range(2):
                            tr(tp[0:64, g, :], skn[:, g * 64:(g + 1) * 64], ["skn"], [tpk])
                        ts("dve", skT[l][:, a + 1, :, :], tp[0:64, 0:2, :], gcol[0:64, l * 4 + 3:l * 4 + 4], None, ALU.mult, None, [tpk, "gcol"], [("skT", l)])
                    W, wk = WS.next("w_in")
                    for a in range(NSUB):
                        ps, pk = proj_tok(W, wk, 512, a)
                        if a == 0:
                            flush()
                        stg, sk = stR.next()
                        cp("act", stg[:], ps[:, 0:512], [pk], [sk])
                        qkn, qknk = qkn_l[a], "qkn%d" % a
                        head_norm(stg[:], 8, None, qkn, [sk], [qknk])

                        def swa_tr(a=a, qkn=qkn, qknk=qknk):
                            qb_, qbk = sqT[a], "sqT%d" % a
                            for half in range(2):
                                tp, tpk = tpR.next()
                                for k in range(4):
                                    hh = half * 4 + k
                                    tr(tp[0:64, k, :], qkn[:, hh * 64:(hh + 1) * 64], [qknk], [tpk])
                                if half:
                                    act(qb_[:, 4:8, :], tp[0:64, 0:4, :], AF.Copy, [tpk, "gcol"], [qbk], scale=gcol[0:64, l * 4 + 2:l * 4 + 3])
                                else:
                                    ts("dve", qb_[:, 0:4, :], tp[0:64, 0:4, :], gcol[0:64, l * 4 + 2:l * 4 + 3], None, ALU.mult, None,
                                       [tpk, "gcol"], [qbk])
                        deferred.append(swa_tr)

                    P.phase = "mlstm_proj"
                    W, wk = WS.next("w_in")
                    for a in range(NSUB):
                        ps, pk = proj_tok(W, wk, 264, a)
                        cp("dve", mV[:, a, :, 0:64], ps[:, 0:256].rearrange("p (h d) -> p h d", d=64), [pk], ["mV"])
                        tt("dve", mif[:, a, 0:4], ps[:, 256:260], rp(l, RP_MIB, 4), ALU.add, [pk, "rowp"], [("mif", a)])
                        tt("dve", mif[:, a, 4:8], ps[:, 260:264], rp(l, RP_MFB, 4), ALU.add, [pk, "rowp"], [("mif", a)])
                        act(mt1[:, 0:4], mif[:, a, 4:8], AF.Exp, [("mif", a)], ["mt1"], scale=-1.0)
                        act(msp[:, a, :], mt1[:, 0:4], AF.Ln, ["mt1"], [("msp", a)], bias=1.0)

                        def ml_cum(a=a):
                            p2, p2k = aoR.next()
                            mm(p2[:, 0:4], triblk, msp[:, a, :], True, True, ["cst", ("msp", a)], [p2k])
                            for c in range(2):
                                mm(p2[0:64, 8 + 4 * c:12 + 4 * c], chunkind[:, c, :], msp[:, a, :], True, True, ["chunkind", ("msp", a)], [p2k])
                            cp("dve", mp2[:, 0:4], p2[:, 0:4], [p2k], ["mp2"])
                            cp("dve", mp2[0:64, 8:16], p2[0:64, 8:16], [p2k], ["mp2"])
                            tt("dve", mt1[:, 4:8], mp2[:, 0:4], mif[:, a, 0:4], ALU.add, ["mp2", ("mif", a)], ["mt1"])
                            act(mea[:, a, :], mt1[:, 4:8], AF.Exp, ["mt1"], [("mea", a)])
                            act(meb[:, a, :], mp2[:, 0:4], AF.Exp, ["mp2"], [("meb", a)], scale=-1.0)
                            act(mdec[:, a, :, :], mp2[0:64, 8:16].rearrange("p (c h) -> p c h", h=4), AF.Exp, ["mp2"], [("mdec", a)], scale=-1.0)
                        deferred.append(ml_cum)
                    W, wk = WS.next("w_in")
                    for a in range(NSUB):
                        ps, pk = proj_tok(W, wk, 256, a)
                        act(mso[:, a, :], ps[:, 0:256], AF.Sigmoid, [pk], [("mso", a)])
                    flush()
                    W, wk = WS.next("w_in")
                    for a in range(NSUB):
                        ps, pk = proj_tok(W, wk, 512, a)
                        ts("dve", mqb_l[a][:], ps[:, 0:256], 0.125, None, ALU.mult, None, [pk], [("mqb", a)])
                        tt("dve", mkt[a][:].rearrange("p (h d) -> p h d", d=64), ps[:, 256:512].rearrange("p (h d) -> p h d", d=64),
                           mea[:, a, :].unsqueeze(2).to_broadcast([128, 4, 64]), ALU.mult, [pk, ("mea", a)], [("mkt", a)])

                        def ml_tr(a=a):
                            tq, tqk = tpR.next()
                            for h in range(4):
                                tr(tq[0:64, h, :], mqb_l[a][:, h * 64:(h + 1) * 64], [("mqb", a)], [tqk])
                            tk, tkk = tpR.next()
                            for h in range(4):
                                tr(tk[0:64, h, :], mkt[a][:, h * 64:(h + 1) * 64], [("mkt", a)], [tkk])
                            cp("dve", mqT[a][:], tq[0:64, 0:4, :], [tqk], [("mqT", a)])
                            cp("dve", mq0T[a][:, :, 0:64], tq[0:64, 0:4, 0:64], [tqk], [("mq0T", a)])
                            cp("dve", mq1T[a][:, :, 64:128], tq[0:64, 0:4, 64:128], [tqk], [("mq1T", a)])
                            cp("act", mkT[a][:], tk[0:64, 0:4, :], [tkk], [("mkT", a)])
                        deferred.append(ml_tr)

                    if stop_at <= 5:
                        raise _Stop()
                    P.phase = "conv"
                    W, wk = WS.next("w_in")
                    for cb_ in range(4):
                        ps, pk = projR.next()
                        for kc in range(8):
                            mm(ps[:, 0:T], W[:, kc, cb_ * 128:(cb_ + 1) * 128], hT[:, kc, :], kc == 0, kc == 7, HT_ALL + [wk], [pk])
                        dst = cu if cb_ < 2 else cbt
                        cp("act", dst[:, cb_ % 2, :], ps[:, 0:T], [pk], ["cu" if cb_ < 2 else "cbt"])
                    flush()
                    for f_ in late:
                        f_()
                    W, wk = WS.next("w_in")
                    for cc in range(2):
                        ps, pk = projR.next()
                        for kc in range(8):
                            mm(ps[:, 0:T], W[:, kc, cc * 128:(cc + 1) * 128], hT[:, kc, :], kc == 0, kc == 7, HT_ALL + [wk], [pk])
                        z = cz[l]
                        tt("dve", z[:, cc, 2:T + 2], ps[:, 0:T], cu[:, cc, :], ALU.mult, [pk, "cu"], [("cz", l)])
                        w0 = convw[:, l * 6 + cc * 3 + 0: l * 6 + cc * 3 + 1]
                        w1 = convw[:, l * 6 + cc * 3 + 1: l * 6 + cc * 3 + 2]
                        w2 = convw[:, l * 6 + cc * 3 + 2: l * 6 + cc * 3 + 3]
                        ts("dve", cy[:], z[:, cc, 2:T + 2], w2, None, ALU.mult, None, [("cz", l), "convw"], ["cy"])
                        stt("dve", cy[:], z[:, cc, 1:T + 1], w1, cy[:], ALU.mult, ALU.add, [("cz", l), "convw", "cy"], ["cy"])
                        stt("dve", cy[:], z[:, cc, 0:T], w0, cy[:], ALU.mult, ALU.add, [("cz", l), "convw", "cy"], ["cy"])
                        tt("dve", yT[:, 2 + cc, :], cy[:], cbt[:, cc, :], ALU.mult, ["cy", "cbt"], [("yT", 1)])
                        cp("pool", z[:, cc, 0:2], z[:, cc, T:T + 2], [("cz", l)], [("cz", l)])


                    flush()
                    nblk = blk0 + NSUB

                    def gen_fox():
                        P.phase = "fox_attn"
                        acc, acck = mmR.items[0]
                        accv = acc[:, 0:NSUB * 65].rearrange("p (a c) -> p a c", c=65)
                        units = [(h, j) for h in range(4) for j in range(nblk)]
                        scs = {}

                        def s_qk(n):
                            h, j = units[n]
                            hp, hbase = h // 2, (h % 2) * 64
                            c0 = max(0, j - blk0) * 128
                            sc, sck = foxR.next()
                            scs[n] = (sc, sck)
                            mm(sc[:, c0:T], fkT[l][hbase:hbase + 64, hp, j * 128:(j + 1) * 128], fqT[hbase:hbase + 64, hp, c0:T],
                               True, True, [("fkT", l), "fqT"], [sck])

                        def s_dve(n):
                            h, j = units[n]
                            cb, cbk, cbm, cbmk = fcbc[h], "fcbc%d" % h, fcbm[h], "fcbm%d" % h
                            c0 = max(0, j - blk0) * 128
                            sc, sck = scs.pop(n)
                            bi = n % 2
                            tmp, tmpk = atmp[bi], "atmp%d" % bi
                            if j >= blk0:
                                stt("dve", tmp[:, c0:c0 + 128], sc[:, c0:c0 + 128], 0.125, cbm[:, c0:c0 + 128], ALU.mult, ALU.subtract,
                                    [sck, cbmk], [tmpk])
                                if c0 + 128 < T:
                                    stt("dve", tmp[:, c0 + 128:T], sc[:, c0 + 128:T], 0.125, cb[:, c0 + 128:T], ALU.mult, ALU.subtract,
                                        [sck, cbk], [tmpk])
                            else:
                                stt("dve", tmp[:, c0:T], sc[:, c0:T], 0.125, cb[:, c0:T], ALU.mult, ALU.subtract, [sck, cbk], [tmpk])

                        def s_act(n):
                            h, j = units[n]
                            c0 = max(0, j - blk0) * 128
                            bi = n % 2
                            act(apT[bi][:, c0:T], atmp[bi][:, c0:T], AF.Exp, ["atmp%d" % bi, ("fnegc", l)], ["apT%d" % bi],
                                bias=fnegc[l][:, j, h:h + 1])

                        def s_pv(n):
                            h, j = units[n]
                            a0 = max(0, j - blk0)
                            bi = n % 2
                            pT, pTk = apT[bi], "apT%d" % bi
                            for a in range(a0, NSUB):
                                mm(accv[:, a, :], pT[:, a * 128:(a + 1) * 128], fV[l][:, j, h, :], (j == 0 and a == 0),
                                   (j == nblk - 1 and a == NSUB - 1), [pTk, ("fV", l)], [acck])
                            if j == nblk - 1:
                                recip(frec[:, 0:NSUB], accv[:, :, 64], [acck], ["frec"])
                                tt("dve", yfox[:, :, h * 64:(h + 1) * 64], accv[:, :, 0:64],
                                   frec[:, 0:NSUB].unsqueeze(2).to_broadcast([128, NSUB, 64]), ALU.mult, [acck, "frec"], ["yfox"])

                        NU = len(units)
                        for step in range(NU + 3):
                            P.phase = "fox_attn"
                            if 0 <= step - 3 < NU:
                                s_pv(step - 3)
                            if 0 <= step - 2 < NU:
                                s_act(step - 2)
                            if 0 <= step - 1 < NU:
                                s_dve(step - 1)
                            if step < NU:
                                s_qk(step)
                            yield
                        P.phase = "fox_attn"
                        cp("pool", fncar[l][:, 0, :], fncar[l][:, NSUB, :], [("fncar", l)], [("fncar", l)])
                        for a in range(NSUB):
                            tp, tpk = tpR.next()
                            for k in range(2):
                                tr(tp[:, k, :], yfox[:, a, k * 128:(k + 1) * 128], ["yfox"], [tpk])
                            cp("act", yT[:, 0:2, a * 128:(a + 1) * 128], tp[:, 0:2, :], [tpk], [("yT", 0)])

                    def gen_swa():
                        for a in range(NSUB):
                            qb_, qbk = sqT[a], "sqT%d" % a
                            has_prev = not (first_tile and a == 0)
                            yb, ybk = yswa[a % 2], "yswa%d" % (a % 2)
                            for g in range(2):
                                P.phase = "swa"
                                pTs = []
                                for which in ([0, 1] if has_prev else [1]):
                                    blk = a + which
                                    sc, sck = attR.next()
                                    mm(sc[:], skT[l][:, blk, g, :], qb_[:, 4 * g:4 * g + 4, :].rearrange("p h t -> p (h t)"),
                                       True, True, [("skT", l), qbk], [sck])
                                    tmp, tmpk, pT, pTk = swtmp[which], "swtmp%d" % which, swpT[which], "swpT%d" % which
                                    stt("dve", tmp[:], sc[:], 0.125, bm[:, which, 4 * g:4 * g + 4, :].rearrange("p h t -> p (h t)"),
                                        ALU.mult, ALU.add, [sck, "bm"], [tmpk])
                                    act(pT[:], tmp[:], AF.Exp, [tmpk], [pTk])
                                    pTs.append((pT, pTk, blk))
                                ao, aok = aoR.next()
                                aov = ao[:, 0:260].rearrange("p (h c) -> p h c", c=65)
                                for hh in range(4):
                                    for i, (pT, pTk, blk) in enumerate(pTs):
                                        mm(aov[:, hh, :], pT[:, hh * 128:(hh + 1) * 128], sV[l][:, blk, g, :], i == 0, i == len(pTs) - 1,
                                           [pTk, ("sV", l)], [aok])
                                tt("dve", sden[:], aov[:, :, 64], expsink[:, l, 4 * g:4 * g + 4], ALU.add, [aok, "expsink"], ["sden"])
                                recip(sden[:], sden[:], ["sden"], ["sden"])
                                tt("dve", yb[:, g * 256:(g + 1) * 256].rearrange("p (h d) -> p h d", d=64), aov[:, :, 0:64],
                                   sden[:].unsqueeze(2).to_broadcast([128, 4, 64]), ALU.mult, [aok, "sden"], [ybk])
                                yield
                            P.phase = "swa"
                            tp, tpk = tpR.next()
                            for k in range(4):
                                tr(tp[:, k, :], yb[:, k * 128:(k + 1) * 128], [ybk], [tpk])
                            cp("act", yT[:, 6:10, a * 128:(a + 1) * 128], tp[:, 0:4, :], [tpk], [("yT", 3)])
                            yield
                        P.phase = "swa"
                        cp("pool", skT[l][:, 0, :, :], skT[l][:, NSUB, :, :], [("skT", l)], [("skT", l)])
                        cp("pool", sV[l][:, 0, :, :], sV[l][:, NSUB, :, :], [("sV", l)], [("sV", l)])

                    def gen_mlstm():
                        for a in range(NSUB):
                            P.phase = "mlstm"
                            sc, sck = attR.next()
                            scv = sc[:].rearrange("p (h t) -> p h t", t=128)
                            for h in range(4):
                                mm(scv[:, h, :], mkT[a][:, h, :], mqT[a][:, h, :], True, True, [("mkT", a), ("mqT", a)], [sck])
                            tt("dve", mPT[:], scv, m01b[:].unsqueeze(1).to_broadcast([128, 4, 128]), ALU.mult, [sck, "m01b"], ["mPT"])
                            yield
                            P.phase = "mlstm"
                            pd, pdk = aoR.next()
                            pdv = pd[0:64, 0:260].rearrange("p (h c) -> p h c", c=65)
                            for h in range(4):
                                mm(pdv[:, h, :], mkt[a][0:64, h * 64:(h + 1) * 64], mV[0:64, a, h, :], True, True, [("mkt", a), "mV"], [pdk])
                            tt("dve", mCf[l][:], mCf[l][:], pdv, ALU.add, [("mCf", l), pdk], [("mCf", l)])
                            tt("dve", mCb[l][1][:], mCf[l][:], mdec[:, a, 0, :].unsqueeze(2).to_broadcast([64, 4, 65]), ALU.mult,
                               [("mCf", l), ("mdec", a)], [("mCb", l, 1)])
                            tt("pool", mCf[l][:], mCf[l][:], mdec[:, a, 0, :].unsqueeze(2).to_broadcast([64, 4, 65]), ALU.mult,
                               [("mCf", l), ("mdec", a)], [("mCf", l)])
                            yield
                            P.phase = "mlstm"
                            ao, aok = aoR.next()
                            aov = ao[:, 0:260].rearrange("p (h c) -> p h c", c=65)
                            for h in range(4):
                                mm(aov[:, h, :], mPT[:, h, :], mV[:, a, h, :], True, False, ["mPT", "mV"], [aok])
                                mm(aov[:, h, :], mq0T[a][:, h, :], mCb[l][0][:, h, :], False, False, [("mq0T", a), ("mCb", l, 0)], [aok])
                                mm(aov[:, h, :], mq1T[a][:, h, :], mCb[l][1][:, h, :], False, True, [("mq1T", a), ("mCb", l, 1)], [aok])
                            tt("dve", mt1[:, 0:4], aov[:, :, 64], meb[:, a, :], ALU.mult, [aok, ("meb", a)], ["mt1"])
                            stt("dve", mt1[:, 0:4], mt1[:, 0:4], -1.0, mt1[:, 0:4], ALU.mult, ALU.max, ["mt1"], ["mt1"])
                            P.emit("dve", lambda e: e.tensor_scalar_max(out=mt1[:, 0:4], in0=mt1[:, 0:4], scalar1=1.0), ["mt1"], ["mt1"])
                            recip(mt1[:, 0:4], mt1[:, 0:4], ["mt1"], ["mt1"])
                            tt("dve", mt1[:, 0:4], mt1[:, 0:4], meb[:, a, :], ALU.mult, ["mt1", ("meb", a)], ["mt1"])
                            tt("dve", mhm[:].rearrange("p (h d) -> p h d", d=64), aov[:, :, 0:64],
                               mt1[:, 0:4].unsqueeze(2).to_broadcast([128, 4, 64]), ALU.mult, [aok, "mt1"], ["mhm"])
                            yield
                            P.phase = "mlstm"
                            pd, pdk = aoR.next()
                            pdv = pd[0:64, 0:260].rearrange("p (h c) -> p h c", c=65)
                            for h in range(4):
                                mm(pdv[:, h, :], mkt[a][64:128, h * 64:(h + 1) * 64], mV[64:128, a, h, :], True, True, [("mkt", a), "mV"], [pdk])
                            tt("dve", mCf[l][:], mCf[l][:], pdv, ALU.add, [("mCf", l), pdk], [("mCf", l)])
                            tt("dve", mCb[l][0][:], mCf[l][:], mdec[:, a, 1, :].unsqueeze(2).to_broadcast([64, 4, 65]), ALU.mult,
                               [("mCf", l), ("mdec", a)], [("mCb", l, 0)])
                            tt("pool", mCf[l][:], mCf[l][:], mdec[:, a, 1, :].unsqueeze(2).to_broadcast([64, 4, 65]), ALU.mult,
                               [("mCf", l), ("mdec", a)], [("mCf", l)])
                            yield
                            P.phase = "mlstm"
                            tt("pool", sqtmp[:, 0:256], mhm[:], mhm[:], ALU.mult, ["mhm"], ["sqtmpA"])
                            P.emit("dve", lambda e: e.tensor_reduce(out=nrm_ss[:, 0:4], in_=sqtmp[:, 0:256].rearrange("p (h d) -> p h d", d=64),
                                                                     axis=AX.X, op=ALU.add), ["sqtmpA"], ["nrm_ssA"])
                            act(nrm_ss[:, 0:4], nrm_ss[:, 0:4], AF.Ln, ["nrm_ssA"], ["nrm_ssA"], bias=EPS, scale=1.0 / 64)
                            act(nrm_ss[:, 0:4], nrm_ss[:, 0:4], AF.Exp, ["nrm_ssA"], ["nrm_ssA"], scale=-0.5)
                            tt("dve", mhm[:].rearrange("p (h d) -> p h d", d=64), mhm[:].rearrange("p (h d) -> p h d", d=64),
                               nrm_ss[:, 0:4].unsqueeze(2).to_broadcast([128, 4, 64]), ALU.mult, ["mhm", "nrm_ssA"], ["mhm"])
                            tt("pool", mhm[:], mhm[:], rp(l, RP_MHG, 256), ALU.mult, ["mhm", "rowp"], ["mhm"])
                            tt("dve", ymls[:], mhm[:], mso[:, a, :], ALU.mult, ["mhm", ("mso", a)], ["ymls"])
                            tp, tpk = tpR.next()
                            for k in range(2):
                                tr(tp[:, k, :], ymls[:, k * 128:(k + 1) * 128], ["ymls"], [tpk])
                            cp("act", yT[:, 4:6, a * 128:(a + 1) * 128], tp[:, 0:2, :], [tpk], [("yT", 2)])
                            yield

                    gens = [[gen_fox(), max(1, (4 * nblk + 4) // (5 * NSUB))], [gen_swa(), 1], [gen_mlstm(), 1]]
                    while gens:
                        for ge in list(gens):
                            for _ in range(ge[1]):
                                try:
                                    next(ge[0])
                                except StopIteration:
                                    gens.remove(ge)
                                    break

                    if stop_at <= 6:
                        raise _Stop()
                    P.phase = "merge"
                    ych0 = [0, 2, 4, 6]
                    for b, nm in enumerate(["w_fox_out", "w_conv_out", "w_mlstm_out", "w_swa_out"]):
                        Wb, wbk = WS.next(nm, hold=2)
                        nkc = 4 if b == 3 else 2
                        for half in range(2):
                            Wg, wgk = WS.next("w_in")
                            for a in range(NSUB):
                                pg, pgk = proj_tok(Wg, wgk, 512, a, denseR)
                                s_, s_k = sg[a % 2], "sg%d" % (a % 2)
                                act(s_[:], pg[:, 0:512], AF.Sigmoid, [pgk], [s_k])
                                po, pok = denseR.next()
                                for kc in range(nkc):
                                    mm(po[:, 0:512], yT[:, ych0[b] + kc, a * 128:(a + 1) * 128], Wb[:, kc, half * 512:(half + 1) * 512],
                                       kc == 0, kc == nkc - 1, [("yT", b), wbk], [pok])
                                mdst = merged[:, a, half * 512:(half + 1) * 512]
                                if b == 0:
                                    tt("dve", mdst, po[:, 0:512], s_[:], ALU.mult, [pok, s_k], [("merged", a)])
                                else:
                                    tt("dve", mtmp[:], po[:, 0:512], s_[:], ALU.mult, [pok, s_k], ["sqtmpB"])
                                    tt("pool", mdst, mdst, mtmp[:], ALU.add, [("merged", a), "sqtmpB"], [("merged", a)])
                    for a in range(NSUB):
                        cp("act", hb[:], merged[:, a, :], [("merged", a)], ["hb"])
                        tp, tpk = tpR.next()
                        for kc in range(8):
                            tr(tp[:, kc, :], hb[:, kc * 128:(kc + 1) * 128], ["hb"], [tpk])
                        cp("act" if a % 2 else "dve", hT[:, :, a * 128:(a + 1) * 128], tp[:, :, :], [tpk], [("hT", a)])
                    P.phase = "merge_out"
                    for half in range(2):
                        Wm, wmk = WS.next("w_merge_out")
                        for a in range(NSUB):
                            ps, pk = proj_tok(Wm, wmk, 512, a, denseR)
                            xs = xt[:, a, half * 512:(half + 1) * 512]
                            tt("dve", xs, ps[:, 0:512], xs, ALU.add, [pk, ("xt", a)], [("xt", a)])

                    if stop_at <= 7:
                        raise _Stop()
                    P.phase = "ffn"
                    resid_norm(l, 1)
                    for c in range(6):
                        ncol = min(512, DFF - c * 512)
                        Wg, wgk = WS.next("w_gate", hold=1)
                        Wu, wuk = WS.next("w_up")
                        for cb_ in range(ncol // 128):
                            ffc = c * 4 + cb_
                            pg, pgk = denseR.next()
                            for kc in range(8):
                                mm(pg[:, 0:T], Wg[:, kc, cb_ * 128:(cb_ + 1) * 128], hT[:, kc, :], kc == 0, kc == 7, HT_ALL + [wgk], [pgk])
                            pu, puk = denseR.next()
                            for kc in range(8):
                                mm(pu[:, 0:T], Wu[:, kc, cb_ * 128:(cb_ + 1) * 128], hT[:, kc, :], kc == 0, kc == 7, HT_ALL + [wuk], [puk])
                            s_, s_k = sg[ffc % 2], "sg%d" % (ffc % 2)
                            act(s_[:, 0:T], pg[:, 0:T], AF.Silu, [pgk], [s_k])
                            tt("dve", actT[:, ffc, :], pu[:, 0:T], s_[:, 0:T], ALU.mult, [puk, s_k], [("actT", ffc)])
                    P.phase = "ffn_down"
                    ACT_ALL = [("actT", f) for f in range(22)]
                    for half in range(2):
                        accs = []
                        for a in range(NSUB):
                            accs.append(denseR.next())
                        for c in range(3):
                            Wd, wdk = WS.next("w_down")
                            k0 = c * 8
                            nk = min(8, 22 - k0)
                            for a in range(NSUB):
                                ps, pk = accs[a]
                                for k in range(nk):
                                    mm(ps[:, 0:512], actT[:, k0 + k, a * 128:(a + 1) * 128], Wd[:, k, :], (k0 + k) == 0, (k0 + k) == 21,
                                       ACT_ALL + [wdk], [pk])
                        for a in range(NSUB):
                            ps, pk = accs[a]
                            xs = xt[:, a, half * 512:(half + 1) * 512]
                            tt("dve", xs, ps[:, 0:512], xs, ALU.add, [pk, ("xt", a)], [("xt", a)])
                for a in range(NSUB):
                    dma("sp", out_d[tok0 + a * 128: tok0 + (a + 1) * 128, :], xt[:, a, :], [("xt", a)], ["out"], "out")
        try:
            main_loop()
        except _Stop:
            for a in range(NSUB):
                dma("sp", out_d[a * 128:(a + 1) * 128, :], xt[:, a, :], [("xt", a)], ["out"], "out")
        P.emit("sp", lambda e: e.nop(), ["out"], ())
        P.finalize(nc)
        _LAST["P"] = P
    return nc


def _t5_bucket(n):
    max_exact = 16
    nf = np.maximum(n, 1).astype(np.float32)
    large = max_exact + (np.log(nf / max_exact) / math.log(128 / max_exact) * (32 - max_exact)).astype(np.int32)
    large = np.minimum(large, 31)
    return np.where(n < max_exact, n, large)


def host_consts(inp):
    f32 = np.float32
    s = np.arange(128)[:, None]
    t = np.arange(128)[None, :]
    cst = np.zeros((128, 6, 128), f32)
    cst[:, 0] = np.eye(128, dtype=f32)
    cst[:, 1] = (s <= t)
    cst[:, 2] = (s <= t) & (s // 64 == t // 64)
    cst[:, 3] = np.where(s <= t, 0.0, NEG)
    cst[:, 4] = 1.0
    cst[:, 5] = cst[:, 2]
    j = np.arange(128)[:, None]
    i = np.arange(128)[None, :]
    swam = np.zeros((128, 2, 128), f32)
    d_prev = i + 128 - j
    d_cur = i - j
    swam[:, 0] = np.where((d_prev >= 0) & (d_prev < 128), 0.0, NEG)
    swam[:, 1] = np.where((d_cur >= 0) & (d_cur < 128), 0.0, NEG)
    rel = np.asarray(inp["rel_bias"], f32)
    swab = np.zeros((128, 2, 8, 128), f32)
    swab[:, 0] = rel[_t5_bucket(np.maximum(d_prev, 0))].transpose(0, 2, 1)
    swab[:, 1] = rel[_t5_bucket(np.maximum(d_cur, 0))].transpose(0, 2, 1)
    gT = np.zeros((128, 32), f32)
    for l in range(DEPTH):
        gT[:, l * 16:l * 16 + 8] = np.asarray(inp["attn_norm"][l], f32).reshape(8, 128).T
        gT[:, l * 16 + 8:l * 16 + 16] = np.asarray(inp["ffn_norm"][l], f32).reshape(8, 128).T
    rowp = np.zeros((1, DEPTH * ROWP), f32)
    for l in range(DEPTH):
        o = l * ROWP
        rowp[0, o + RP_FQG:o + RP_FQG + 64] = inp["fox_q_gain"][l]
        rowp[0, o + RP_FKG:o + RP_FKG + 64] = inp["fox_k_gain"][l]
        rowp[0, o + RP_SQG:o + RP_SQG + 64] = inp["swa_q_gain"][l]
        rowp[0, o + RP_SKG:o + RP_SKG + 64] = inp["swa_k_gain"][l]
        rowp[0, o + RP_MHG:o + RP_MHG + 256] = inp["mlstm_h_gain"][l]
        rowp[0, o + RP_FFB:o + RP_FFB + 4] = inp["fox_f_bias"][l]
        rowp[0, o + RP_MIB:o + RP_MIB + 4] = inp["mlstm_i_bias"][l]
        rowp[0, o + RP_MFB:o + RP_MFB + 4] = inp["mlstm_f_bias"][l]
        rowp[0, o + RP_SNK:o + RP_SNK + 8] = inp["swa_sinks"][l]
    convw = np.zeros((128, 12), f32)
    cw = np.asarray(inp["conv_w"], f32)
    for l in range(DEPTH):
        for cc in range(2):
            for k in range(3):
                convw[:, l * 6 + cc * 3 + k] = cw[l, k, cc * 128:(cc + 1) * 128]
    gcol = np.zeros((128, 8), f32)
    for l in range(DEPTH):
        for i, nm in enumerate(["fox_q_gain", "fox_k_gain", "swa_q_gain", "swa_k_gain"]):
            gcol[:, l * 4 + i] = np.tile(np.asarray(inp[nm][l], f32), 2)
    return {"cst": cst, "swab": swab, "swam": swam, "gT": gT, "rowp": rowp, "convw": convw, "gcol": gcol}


_NC_CACHE = {}


def kernel(**inputs):
    inp = {k: np.asarray(v) for k, v in inputs.items()}
    x = np.ascontiguousarray(inp["x"], dtype=np.float32)
    n_cores = 8
    key = "full"
    if key not in _NC_CACHE:
        _NC_CACHE[key] = build()
    nc = _NC_CACHE[key]
    shared = host_consts(inp)
    for name, r, c in WEIGHTS:
        shared[name] = np.ascontiguousarray(inp[name], dtype=np.float32)
    in_maps = []
    for c in range(n_cores):
        m = dict(shared)
        m["x"] = x[2 * c:2 * c + 2].reshape(2 * SEQ, D)
        in_maps.append(m)
    res = run_bass_kernel_spmd(nc, in_maps, core_ids=list(range(n_cores)))
    out = np.stack([r["out"].reshape(2, SEQ, D) for r in res.results], axis=0).reshape(16, SEQ, D)
    return out.astype(np.float32)
```
